# Optimizing a Trainium2 kernel written in Bass

```python
import math
import jax, jax.numpy as jnp
from jax import lax
import numpy as np

D_MODEL = 2048
BATCH = 4
SEQ = 4096
DEPTH = 2

HEAD_DIM = 128
Q_BLOCK = 128
NEG_INF = -1e30
RMS_EPS = 1e-6
N_EVEN = (DEPTH + 1) // 2
N_ODD = DEPTH // 2

NUM_BUCKETS = 32
T5_MAX_DIST = 2048
BIAS_HEADS = 8

NSA_HEADS = 8
NSA_KV_HEADS = 2
NSA_GROUP = NSA_HEADS // NSA_KV_HEADS
CMP_LEN = 32
CMP_STRIDE = 16
CMP_HIDDEN = HEAD_DIM
SLC_LEN = 64
N_SEL = 16
WIN = 512
SEL_QB = 64
FORCE_SCORE = 1e9

MLA_HEADS = 8
Q_LORA = 512
KV_LORA = 512
QK_NOPE = 128
QK_ROPE = 64
V_DIM = 128
ROPE_THETA = 10000.0

DIL_PATTERNS = ((128, 1), (512, 4), (2048, 16))
DIL_HEADS = 8

NSA_Q_W = NSA_HEADS * HEAD_DIM
NSA_KV_W = 6 * NSA_KV_HEADS * HEAD_DIM
NSA_G_W = 3 * NSA_HEADS
AB_IN_W = NSA_Q_W + NSA_KV_W + NSA_G_W + Q_LORA + KV_LORA + QK_ROPE
AB_OUT_W = NSA_HEADS * HEAD_DIM + MLA_HEADS * V_DIM
C_IN_W = len(DIL_PATTERNS) * 3 * DIL_HEADS * HEAD_DIM
C_OUT_W = DIL_HEADS * HEAD_DIM

D_FF = 5632
N_EXPERTS = 8
MOE_TOP_K = 2
D_FF_EXPERT = 7168
MOE_BLOCK = 256

kernel_name = 'hybrid_nsa_mla_dilated_moe_adaln'

F32 = jnp.float32


def rms_norm(x, g):
    xf = x.astype(F32)
    y = xf * lax.rsqrt(jnp.mean(xf * xf, axis=-1, keepdims=True) + RMS_EPS)
    return (y * g.astype(F32)).astype(x.dtype)


def t5_bucket(dist):
    n = jnp.maximum(dist, 0)
    max_exact = NUM_BUCKETS // 2
    nf = jnp.maximum(n, 1).astype(F32)
    large = max_exact + (jnp.log(nf / max_exact) / math.log(T5_MAX_DIST / max_exact)
                         * (NUM_BUCKETS - max_exact)).astype(jnp.int32)
    large = jnp.minimum(large, NUM_BUCKETS - 1)
    return jnp.where(n < max_exact, n, large)


def swiglu(x, w_gate, w_up, w_down):
    return jnp.dot(jax.nn.silu(jnp.dot(x, w_gate)) * jnp.dot(x, w_up), w_down)


def banded_attention(q, k, v, max_back, dist_scale, bias_tab):
    B, L, G, R, D = q.shape
    nb = -(-max_back // Q_BLOCK)
    nq = -(-L // Q_BLOCK)
    pad = nq * Q_BLOCK - L
    kw = (nb + 1) * Q_BLOCK
    qp = jnp.pad(q, ((0, 0), (0, pad), (0, 0), (0, 0), (0, 0))).reshape(B, nq, Q_BLOCK, G, R, D)
    kp = jnp.pad(k, ((0, 0), (nb * Q_BLOCK, pad), (0, 0), (0, 0)))
    vp = jnp.pad(v, ((0, 0), (nb * Q_BLOCK, pad), (0, 0), (0, 0)))
    kidx = jnp.arange(nq)[:, None] * Q_BLOCK + jnp.arange(kw)[None, :]
    kb = kp[:, kidx]
    vb = vp[:, kidx]
    rel = jnp.arange(Q_BLOCK)[:, None] + nb * Q_BLOCK - jnp.arange(kw)[None, :]
    mask = ((rel >= 0) & (rel <= max_back))[None] & ((kidx - nb * Q_BLOCK) >= 0)[:, None, :]
    bias = bias_tab[t5_bucket(rel * dist_scale)].astype(F32).reshape(Q_BLOCK, kw, G, R).transpose(2, 3, 0, 1)
    s = jnp.einsum('bnqgrd,bnkgd->bngrqk', qp, kb, preferred_element_type=F32) * (D ** -0.5) + bias
    s = jnp.where(mask[None, :, None, None], s, NEG_INF)
    m = jnp.max(s, axis=-1, keepdims=True)
    p = jnp.exp(s - m)
    l = jnp.sum(p, axis=-1, keepdims=True)
    o = jnp.einsum('bngrqk,bnkgd->bnqgrd', p / l, vb.astype(F32))
    lse = (m + jnp.log(l))[..., 0].transpose(0, 1, 4, 2, 3)
    o = o.reshape(B, nq * Q_BLOCK, G, R, D)[:, :L]
    lse = lse.reshape(B, nq * Q_BLOCK, G, R)[:, :L]
    return o, lse


def nsa_compress(blocks, pos, w1, w2):
    w1 = w1.reshape(CMP_LEN, HEAD_DIM, CMP_HIDDEN)
    hid = jax.nn.gelu(jnp.einsum('bnlgd,lde->bnge', blocks + pos[None, None, :, None, :], w1))
    return jnp.dot(hid, w2)


def nsa_mixer(q, kv, gate_logits, rel_bias, pos_k, w1_k, w2_k, pos_v, w1_v, w2_v):
    B, S, _ = q.shape
    G, R, D = NSA_KV_HEADS, NSA_GROUP, HEAD_DIM
    q = q.reshape(B, S, G, R, D)
    kv = kv.reshape(B, S, 6, G, D)
    k_cmp, v_cmp, k_slc, v_slc, k_win, v_win = (kv[:, :, i] for i in range(6))
    scale = D ** -0.5
    tpos = jnp.arange(S)

    n_cmp = (S - CMP_LEN) // CMP_STRIDE + 1
    cidx = jnp.arange(n_cmp)[:, None] * CMP_STRIDE + jnp.arange(CMP_LEN)[None, :]
    kc = nsa_compress(k_cmp[:, cidx], pos_k, w1_k, w2_k)
    vc = nsa_compress(v_cmp[:, cidx], pos_v, w1_v, w2_v)
    rel_c = tpos[:, None] - (jnp.arange(n_cmp) * CMP_STRIDE + CMP_LEN - 1)[None, :]
    bias_c = rel_bias[t5_bucket(rel_c)].astype(F32).reshape(S, n_cmp, G, R).transpose(0, 2, 3, 1)
    s_c = jnp.einsum('bsgrd,bngd->bsgrn', q, kc, preferred_element_type=F32) * scale + bias_c
    s_c = jnp.where((rel_c >= 0)[:, None, None, :], s_c, NEG_INF)
    has_c = (tpos >= CMP_LEN - 1)[None, :, None, None, None]
    p_c = jnp.where(has_c, jax.nn.softmax(s_c, axis=-1), 0.0)
    o_cmp = jnp.einsum('bsgrn,bngd->bsgrd', p_c, vc.astype(F32))

    n_slc = S // SLC_LEN
    ratio = SLC_LEN // CMP_STRIDE
    span = CMP_LEN // CMP_STRIDE
    imp_c = jnp.sum(p_c, axis=3)
    pad_front = span - 1
    pad_back = max(0, ratio * n_slc + ratio - pad_front - n_cmp)
    P = jnp.pad(imp_c, ((0, 0), (0, 0), (0, 0), (pad_front, pad_back)))
    imp_s = sum(P[..., (m - n + pad_front):(m - n + pad_front) + ratio * n_slc:ratio]
                for m in range(ratio) for n in range(span))
    blk_t = tpos // SLC_LEN
    jb = jnp.arange(n_slc)
    valid_s = jb[None, :] <= blk_t[:, None]
    forced = (jb[None, :] == 0) | (jb[None, :] == blk_t[:, None]) | (jb[None, :] == blk_t[:, None] - 1)
    score = jnp.where(forced[None, :, None, :], FORCE_SCORE,
                      jnp.where(valid_s[None, :, None, :], imp_s, -1.0))
    n_top = min(N_SEL, n_slc)
    top_v, top_i = lax.top_k(score, n_top)
    sel_ok = top_v > -0.5

    kb = k_slc.reshape(B, n_slc, SLC_LEN, G, D).transpose(0, 3, 1, 2, 4)
    vb = v_slc.reshape(B, n_slc, SLC_LEN, G, D).transpose(0, 3, 1, 2, 4)
    nqb = S // SEL_QB
    tbl = rel_bias.reshape(NUM_BUCKETS, G, R).transpose(1, 0, 2)
    bi = jnp.arange(B)[:, None, None, None]
    gi = jnp.arange(G)[None, None, :, None]

    def split_q(a):
        return a.reshape((B, nqb, SEL_QB) + a.shape[2:]).swapaxes(0, 1)

    def sel_block(args):
        qb, ib, okb, tb = args
        ks = kb[bi, gi, ib]
        vs = vb[bi, gi, ib]
        kpos = ib[..., None] * SLC_LEN + jnp.arange(SLC_LEN)
        rel = tb[None, :, None, None, None] - kpos
        mask = (rel >= 0) & okb[..., None]
        bias = tbl[gi[..., None], t5_bucket(rel)].astype(F32).transpose(0, 1, 2, 5, 3, 4)
        s = jnp.einsum('bqgrd,bqgnkd->bqgrnk', qb, ks, preferred_element_type=F32) * scale + bias
        s = jnp.where(mask[:, :, :, None], s, NEG_INF)
        shp = s.shape
        p = jax.nn.softmax(s.reshape(shp[:4] + (shp[4] * shp[5],)), axis=-1).reshape(shp)
        return jnp.einsum('bqgrnk,bqgnkd->bqgrd', p, vs.astype(F32))

    o_slc = lax.map(sel_block, (split_q(q), split_q(top_i), split_q(sel_ok), tpos.reshape(nqb, SEL_QB)))
    o_slc = o_slc.swapaxes(0, 1).reshape(B, S, G, R, D)

    o_win, _ = banded_attention(q, k_win, v_win, WIN - 1, 1, rel_bias)

    gates = jax.nn.sigmoid(gate_logits.astype(F32)).reshape(B, S, G, R, 3)
    o = gates[..., 0:1] * o_cmp + gates[..., 1:2] * o_slc + gates[..., 2:3] * o_win
    return o.reshape(B, S, G * R * D).astype(q.dtype)


def apply_rope(x, cos, sin):
    half = x.shape[-1] // 2
    x1, x2 = x[..., :half].astype(F32), x[..., half:].astype(F32)
    return jnp.concatenate([x1 * cos - x2 * sin, x2 * cos + x1 * sin], axis=-1).astype(x.dtype)


def mla_mixer(q_lat, kv_lat, k_pe, positions, q_norm_g, kv_norm_g, w_uq, w_ukv):
    B, S, _ = q_lat.shape
    q = jnp.dot(rms_norm(q_lat, q_norm_g), w_uq).reshape(B, S, MLA_HEADS, QK_NOPE + QK_ROPE)
    q_nope, q_pe = q[..., :QK_NOPE], q[..., QK_NOPE:]
    kvu = jnp.dot(rms_norm(kv_lat, kv_norm_g), w_ukv).reshape(B, S, MLA_HEADS, QK_NOPE + V_DIM)
    k_nope, v = kvu[..., :QK_NOPE], kvu[..., QK_NOPE:]
    inv_freq = ROPE_THETA ** (-jnp.arange(0, QK_ROPE, 2, dtype=F32) / QK_ROPE)
    ang = positions.astype(F32)[..., None] * inv_freq
    cos, sin = jnp.cos(ang), jnp.sin(ang)
    q_pe = apply_rope(q_pe, cos[:, :, None, :], sin[:, :, None, :])
    k_pe = apply_rope(k_pe, cos, sin)
    scale = (QK_NOPE + QK_ROPE) ** -0.5
    nqb = S // Q_BLOCK
    tpos = jnp.arange(S)

    def split_q(a):
        return a.reshape((B, nqb, Q_BLOCK) + a.shape[2:]).swapaxes(0, 1)

    def blk(args):
        qn, qp, tb = args
        s = (jnp.einsum('bqhd,bkhd->bhqk', qn, k_nope, preferred_element_type=F32)
             + jnp.einsum('bqhd,bkd->bhqk', qp, k_pe, preferred_element_type=F32)) * scale
        s = jnp.where(tpos[None, :] <= tb[:, None], s, NEG_INF)
        p = jax.nn.softmax(s, axis=-1)
        return jnp.einsum('bhqk,bkhd->bqhd', p, v.astype(F32))

    o = lax.map(blk, (split_q(q_nope), split_q(q_pe), tpos.reshape(nqb, Q_BLOCK)))
    return o.swapaxes(0, 1).reshape(B, S, MLA_HEADS * V_DIM).astype(q_lat.dtype)


def mixer_ab(h, positions, rel_bias, w_in, w_out, pos_k, w1_k, w2_k, pos_v, w1_v, w2_v,
             q_norm_g, kv_norm_g, w_uq, w_ukv):
    z = jnp.dot(h, w_in)
    cuts = np.cumsum([NSA_Q_W, NSA_KV_W, NSA_G_W, Q_LORA, KV_LORA]).tolist()
    nsa_q, nsa_kv, nsa_g, q_lat, kv_lat, k_pe = jnp.split(z, cuts, axis=-1)
    o_a = nsa_mixer(nsa_q, nsa_kv, nsa_g, rel_bias, pos_k, w1_k, w2_k, pos_v, w1_v, w2_v)
    o_b = mla_mixer(q_lat, kv_lat, k_pe, positions, q_norm_g, kv_norm_g, w_uq, w_ukv)
    return jnp.dot(jnp.concatenate([o_a, o_b], axis=-1), w_out)


def mixer_c(h, rel_bias, w_in, w_out):
    B, S, _ = h.shape
    z = jnp.dot(h, w_in).reshape(B, S, len(DIL_PATTERNS), 3, DIL_HEADS, HEAD_DIM)
    outs, lses = [], []
    for g, (win, dil) in enumerate(DIL_PATTERNS):
        L = S // dil

        def to_sub(a):
            return a.reshape(B, L, dil, DIL_HEADS, HEAD_DIM).swapaxes(1, 2).reshape(B * dil, L, DIL_HEADS, HEAD_DIM)

        o, lse = banded_attention(to_sub(z[:, :, g, 0])[:, :, :, None], to_sub(z[:, :, g, 1]),
                                  to_sub(z[:, :, g, 2]), win // dil, dil, rel_bias)
        outs.append(o.reshape(B, dil, L, DIL_HEADS, HEAD_DIM).swapaxes(1, 2).reshape(B, S, DIL_HEADS, HEAD_DIM))
        lses.append(lse.reshape(B, dil, L, DIL_HEADS).swapaxes(1, 2).reshape(B, S, DIL_HEADS))
    w = jax.nn.softmax(jnp.stack(lses, axis=0), axis=0)
    o = jnp.sum(w[..., None] * jnp.stack(outs, axis=0), axis=0)
    return jnp.dot(o.reshape(B, S, C_OUT_W).astype(h.dtype), w_out)


def moe_swiglu(h, w_router, w_gate, w_up, w_down):
    B, S, D = h.shape
    N = B * S
    A = N * MOE_TOP_K
    hf = h.reshape(N, D)
    logits = jnp.dot(hf, w_router, preferred_element_type=F32)
    top_v, top_e = lax.top_k(logits, MOE_TOP_K)
    gates = jax.nn.softmax(top_v, axis=-1)
    flat_e = top_e.reshape(A)
    flat_tok = jnp.repeat(jnp.arange(N, dtype=jnp.int32), MOE_TOP_K)
    order = jnp.argsort(flat_e)
    se, st, sg = flat_e[order], flat_tok[order], gates.reshape(A)[order]
    counts = jnp.bincount(flat_e, length=N_EXPERTS)
    starts = jnp.cumsum(counts) - counts
    pcounts = (counts + MOE_BLOCK - 1) // MOE_BLOCK * MOE_BLOCK
    pends = jnp.cumsum(pcounts)
    pstarts = pends - pcounts
    dest = pstarts[se] + jnp.arange(A) - starts[se]
    n_blocks = -(-A // MOE_BLOCK) + N_EXPERTS
    slot_tok = jnp.full((n_blocks * MOE_BLOCK,), N, jnp.int32).at[dest].set(st)
    block_e = jnp.minimum(jnp.searchsorted(pends, jnp.arange(n_blocks) * MOE_BLOCK, side='right'), N_EXPERTS - 1)
    xpad = jnp.concatenate([hf, jnp.zeros((1, D), hf.dtype)], axis=0)
    xb = xpad[slot_tok].reshape(n_blocks, MOE_BLOCK, D)

    def expert_block(args):
        xblk, e = args
        return swiglu(xblk, w_gate[e], w_up[e], w_down[e])

    yb = lax.map(expert_block, (xb, block_e)).reshape(n_blocks * MOE_BLOCK, D)
    y = jnp.zeros((N, D), yb.dtype).at[st].add(yb[dest] * sg[:, None].astype(yb.dtype))
    return y.reshape(B, S, D)


def setup_inputs(seed: int = 0) -> dict:
    key = jax.random.key(seed)
    ks = jax.random.split(key, 32)

    def nrm(i, shape, scale):
        return jax.random.normal(ks[i], shape, F32) * scale

    def gain(i, shape):
        return 1.0 + nrm(i, shape, 0.05)

    D = D_MODEL
    offs = jax.random.randint(ks[2], (BATCH, 1), 0, 1024)
    return {
        'x': nrm(0, (BATCH, SEQ, D), 1.0),
        'c': nrm(1, (BATCH, D), 1.0),
        'positions': (offs + jnp.arange(SEQ)[None, :]).astype(jnp.int32),
        'rel_bias': nrm(3, (NUM_BUCKETS, BIAS_HEADS), 0.2),
        'ada_w': nrm(4, (DEPTH, D, 6 * D), D ** -0.5),
        'ada_b': nrm(5, (DEPTH, 6 * D), 0.02),
        'mix_norm_g': gain(6, (DEPTH, D)),
        'ffn_norm_g': gain(7, (DEPTH, D)),
        'ab_w_in': nrm(8, (N_EVEN, D, AB_IN_W), D ** -0.5),
        'ab_w_out': nrm(9, (N_EVEN, AB_OUT_W, D), AB_OUT_W ** -0.5),
        'nsa_cmp_pos_k': nrm(10, (N_EVEN, CMP_LEN, HEAD_DIM), 0.1),
        'nsa_cmp_w1_k': nrm(11, (N_EVEN, CMP_LEN * HEAD_DIM, CMP_HIDDEN), (CMP_LEN * HEAD_DIM) ** -0.5),
        'nsa_cmp_w2_k': nrm(12, (N_EVEN, CMP_HIDDEN, HEAD_DIM), CMP_HIDDEN ** -0.5),
        'nsa_cmp_pos_v': nrm(13, (N_EVEN, CMP_LEN, HEAD_DIM), 0.1),
        'nsa_cmp_w1_v': nrm(14, (N_EVEN, CMP_LEN * HEAD_DIM, CMP_HIDDEN), (CMP_LEN * HEAD_DIM) ** -0.5),
        'nsa_cmp_w2_v': nrm(15, (N_EVEN, CMP_HIDDEN, HEAD_DIM), CMP_HIDDEN ** -0.5),
        'mla_q_norm_g': gain(16, (N_EVEN, Q_LORA)),
        'mla_kv_norm_g': gain(17, (N_EVEN, KV_LORA)),
        'mla_w_uq': nrm(18, (N_EVEN, Q_LORA, MLA_HEADS * (QK_NOPE + QK_ROPE)), Q_LORA ** -0.5),
        'mla_w_ukv': nrm(19, (N_EVEN, KV_LORA, MLA_HEADS * (QK_NOPE + V_DIM)), KV_LORA ** -0.5),
        'ffn_w_gate': nrm(20, (N_EVEN, D, D_FF), D ** -0.5),
        'ffn_w_up': nrm(21, (N_EVEN, D, D_FF), D ** -0.5),
        'ffn_w_down': nrm(22, (N_EVEN, D_FF, D), D_FF ** -0.5),
        'c_w_in': nrm(23, (N_ODD, D, C_IN_W), D ** -0.5),
        'c_w_out': nrm(24, (N_ODD, C_OUT_W, D), C_OUT_W ** -0.5),
        'moe_w_router': nrm(25, (N_ODD, D, N_EXPERTS), D ** -0.5),
        'moe_w_gate': nrm(26, (N_ODD, N_EXPERTS, D, D_FF_EXPERT), D ** -0.5),
        'moe_w_up': nrm(27, (N_ODD, N_EXPERTS, D, D_FF_EXPERT), D ** -0.5),
        'moe_w_down': nrm(28, (N_ODD, N_EXPERTS, D_FF_EXPERT, D), D_FF_EXPERT ** -0.5),
        'final_norm_g': gain(29, (D,)),
    }


def reference(x, c, positions, rel_bias, ada_w, ada_b, mix_norm_g, ffn_norm_g, ab_w_in, ab_w_out,
              nsa_cmp_pos_k, nsa_cmp_w1_k, nsa_cmp_w2_k, nsa_cmp_pos_v, nsa_cmp_w1_v, nsa_cmp_w2_v,
              mla_q_norm_g, mla_kv_norm_g, mla_w_uq, mla_w_ukv, ffn_w_gate, ffn_w_up, ffn_w_down,
              c_w_in, c_w_out, moe_w_router, moe_w_gate, moe_w_up, moe_w_down, final_norm_g):
    cs = jax.nn.silu(c)
    for i in range(DEPTH):
        j = i // 2
        mod = jnp.dot(cs, ada_w[i]) + ada_b[i]
        sh_m, sc_m, g_m, sh_f, sc_f, g_f = (t[:, None, :] for t in jnp.split(mod, 6, axis=-1))
        h = rms_norm(x, mix_norm_g[i]) * (1.0 + sc_m) + sh_m
        if i % 2 == 0:
            y = mixer_ab(h, positions, rel_bias, ab_w_in[j], ab_w_out[j],
                         nsa_cmp_pos_k[j], nsa_cmp_w1_k[j], nsa_cmp_w2_k[j],
                         nsa_cmp_pos_v[j], nsa_cmp_w1_v[j], nsa_cmp_w2_v[j],
                         mla_q_norm_g[j], mla_kv_norm_g[j], mla_w_uq[j], mla_w_ukv[j])
        else:
            y = mixer_c(h, rel_bias, c_w_in[j], c_w_out[j])
        x = x + g_m * y
        h = rms_norm(x, ffn_norm_g[i]) * (1.0 + sc_f) + sh_f
        if i % 2 == 0:
            y = swiglu(h, ffn_w_gate[j], ffn_w_up[j], ffn_w_down[j])
        else:
            y = moe_swiglu(h, moe_w_router[j], moe_w_gate[j], moe_w_up[j], moe_w_down[j])
        x = x + g_f * y
    return rms_norm(x, final_norm_g)
```

```python
import numpy as np
from contextlib import ExitStack
import concourse.bass as bass
import concourse.mybir as mybir
from concourse.bass_utils import run_bass_kernel_spmd

F32 = mybir.dt.float32
BF16 = mybir.dt.bfloat16
I32 = mybir.dt.int32
AF = mybir.ActivationFunctionType
ALU = mybir.AluOpType
AX = mybir.AxisListType

ENG = ['pe', 'act', 'dve', 'pool', 'sp']


class Buf:
    __slots__ = ('name', 'w', 'r')

    def __init__(self, name=''):
        self.name = name
        self.w = None
        self.r = {}


class Prog:
    def __init__(self, nc):
        self.nc = nc
        self.ops = {e: [] for e in ENG}
        self.cnt = {}
        self.seen = {e: {} for e in ENG}
        self.pending = {e: {} for e in ENG}
        self.nslots = 12
        self.rr = {e: 0 for e in ENG}
        self.sems = {}
        self.stack = ExitStack()
        self.nblk = 0

    def _need(self, E, waits, k, v):
        if k == ('e', 'pe') and E == 'pe':
            return
        if waits.get(k, 0) < v:
            waits[k] = v

    def op(self, E, fn, reads=(), writes=(), dma=False):
        waits = dict(self.pending[E])
        self.pending[E] = {}
        for b in reads:
            if b.w is not None:
                self._need(E, waits, *b.w)
        for b in writes:
            if b.w is not None:
                self._need(E, waits, *b.w)
            for k, v in b.r.items():
                self._need(E, waits, k, v)
        if dma:
            s = self.rr[E]
            self.rr[E] = (s + 1) % self.nslots
            key = ('d', E, s)
            prev = self.cnt.get(key, 0)
            if prev:
                self._need(E, waits, key, prev)
            val = prev + 16
            inc = 16
        else:
            key = ('e', E)
            val = self.cnt.get(key, 0) + 1
            inc = 1
        self.cnt[key] = val
        w2 = []
        for k, v in waits.items():
            if self.seen[E].get(k, 0) >= v:
                continue
            self.seen[E][k] = v
            w2.append((k, v))
        self.ops[E].append((w2, fn, key, inc))
        for b in reads:
            if b.r.get(key, 0) < val:
                b.r[key] = val
        for b in writes:
            b.w = (key, val)
            b.r = {}

    def barrier(self):
        for E in ENG:
            for k, v in self.cnt.items():
                if self.pending[E].get(k, 0) < v:
                    self.pending[E][k] = v

    def sem(self, k):
        if k not in self.sems:
            self.sems[k] = self.stack.enter_context(self.nc.semaphore("s_" + "_".join(map(str, k))))
        return self.sems[k]

    def flush(self):
        nc = self.nc
        for E in ENG:
            for (w, fn, key, inc) in self.ops[E]:
                if key is not None:
                    self.sem(key)
                for k, v in w:
                    self.sem(k)
        ops = self.ops
        self.ops = {e: [] for e in ENG}
        sem = self.sems

        def run(E):
            def f(e):
                for (w, fn, key, inc) in ops[E]:
                    for (k, v) in w:
                        e.wait_ge(sem[k], v)
                    if fn is not None:
                        fn(e).then_inc(sem[key], inc)
            return f
        with nc.Block() as block:
            block.tensor(run('pe'))
            block.scalar(run('act'))
            block.vector(run('dve'))
            block.gpsimd(run('pool'))
            block.sync(run('sp'))
        self.nblk += 1

    def finish(self):
        self.barrier()
        for E in ENG:
            waits = self.pending[E]
            self.pending[E] = {}
            w2 = [(k, v) for k, v in waits.items() if self.seen[E].get(k, 0) < v]
            self.ops[E].append((w2, None, None, 0))
        self.flush()
        self.stack.close()

    def mm(self, out, lhsT, rhs, start, stop, R, W):
        self.op('pe', lambda e: e.matmul(out, lhsT=lhsT, rhs=rhs, start=start, stop=stop), R, W)

    def tr(self, out, in_, ident, R, W):
        self.op('pe', lambda e: e.transpose(out=out, in_=in_, identity=ident), R, W)

    def dma(self, q, out, in_, R, W):
        self.op(q, lambda e: e.dma_start(out=out, in_=in_), R, W, dma=True)

    def act(self, out, in_, func, R, W, **kw):
        self.op('act', lambda e: e.activation(out=out, in_=in_, func=func, **kw), R, W)

    def dve(self, name, R, W, *a, **kw):
        self.op('dve', lambda e: getattr(e, name)(*a, **kw), R, W)

    def pool(self, name, R, W, *a, **kw):
        self.op('pool', lambda e: getattr(e, name)(*a, **kw), R, W)


class T:
    def __init__(self, h, name):
        self.h = h
        self.b = Buf(name)

    def __getitem__(self, k):
        return self.h[k]


class Ctx:
    def __init__(self, nc, P):
        self.nc = nc
        self.P = P
        self.st = None
        self.n = 0

    def phase(self):
        return _Phase(self)

    def sb(self, shape, dt, name=None):
        self.n += 1
        name = name or "t"
        return T(self.st.enter_context(self.nc.sbuf_tensor(f"{name}_{self.n}", list(shape), dt)), name)

    def ps(self, shape, dt, name=None):
        self.n += 1
        name = name or "p"
        width = 512 if dt == F32 else 1024
        assert len(shape) == 2 and shape[1] <= width
        full = self.st.enter_context(self.nc.psum_tensor(f"{name}_{self.n}", [128, width], dt))
        return T(full[0:shape[0], 0:shape[1]], name)


class _Phase:
    def __init__(self, c):
        self.c = c

    def __enter__(self):
        self.c.P.barrier()
        self.c.st = ExitStack()
        self.c.st.__enter__()
        return self.c

    def __exit__(self, *a):
        if a[0] is None:
            self.c.P.flush()
        self.c.st.__exit__(*a)
        self.c.st = None
        return False


D = 2048
KC = 16
TG = 512
EPS = 1e-6


def rearr_w(ap):
    return ap.rearrange("(kc p) n -> p kc n", p=128)


def emit_norm_T(C, P, bufs, x_dram_rows, A, B, ident, hT, xdeps=(), tile_cb=None):
    xb, hb, junk, st, pst = bufs
    for tt in range(4):
        x = xb[tt % 2]
        h = hb[tt % 2]
        P.dma('sp', x[:], x_dram_rows[tt * 128:(tt + 1) * 128, :], list(xdeps), [x.b])
        P.act(junk[:], x[:], AF.Square, [x.b], [junk.b, st.b], accum_out=st[:, 0:1])
        P.dve('tensor_scalar', [st.b], [st.b], out=st[:, 1:2], in0=st[:, 0:1], scalar1=1.0 / D, scalar2=EPS, op0=ALU.mult, op1=ALU.add)
        P.act(st[:, 3:4], st[:, 1:2], AF.Sqrt, [st.b], [st.b])
        P.dve('reciprocal', [st.b], [st.b], out=st[:, 2:3], in_=st[:, 3:4])
        P.dve('scalar_tensor_tensor', [x.b, st.b, A.b], [x.b], out=x[:], in0=x[:], scalar=st[:, 2:3], in1=A[:], op0=ALU.mult, op1=ALU.mult)
        P.dve('tensor_tensor', [x.b, B.b], [x.b], out=x[:], in0=x[:], in1=B[:], op=ALU.add)
        P.act(h[:], x[:], AF.Copy, [x.b], [h.b])
        if tile_cb is not None:
            tile_cb(tt, x)
        for half in range(2):
            pt = pst[half]
            for c in range(8):
                cc = half * 8 + c
                P.tr(pt[:, c * 128:(c + 1) * 128], h[:, cc * 128:(cc + 1) * 128], ident[:], [h.b, ident.b], [pt.b])
            dst = hT[:, half * 8:(half + 1) * 8, tt * 128:(tt + 1) * 128]
            src = pt[:].rearrange("p (c t) -> p c t", c=8)
            if half == 0:
                P.act(dst, src, AF.Copy, [pt.b], [hT.b])
            else:
                P.dve('tensor_copy', [pt.b], [hT.b], out=dst, in_=src)


def emit_swiglu(C, P, hT_loader, ngroups, wg, wu, wd, NF, gate, y_dram, yb, nwd=2):
    hT = C.sb([128, KC, TG], BF16, "hT")
    aT = C.sb([128, NF, TG], BF16, "aT")
    wgb = [C.sb([128, KC, 256], BF16, "wgb") for _ in range(2)]
    wub = [C.sb([128, KC, 256], BF16, "wub") for _ in range(2)]
    wdb = [C.sb([128, NF, 256], BF16, "wdb") for _ in range(nwd)]
    sg = [C.sb([128, TG], F32, "sg") for _ in range(2)]
    pg = [C.ps([128, TG], F32, "pg") for _ in range(2)]
    pu = [C.ps([128, TG], F32, "pu") for _ in range(2)]
    pd = [C.ps([128, 256], F32, "pd") for _ in range(2)]
    stg = [C.sb([128, 256], F32, "stg") for _ in range(3)]
    it = 0
    io = 0
    for g in range(ngroups):
        hT_loader(g, hT)
        for fb in range(NF // 2):
            a = wgb[fb % 2]
            b = wub[fb % 2]
            P.dma('pool', a[:], rearr_w(wg[:, fb * 256:(fb + 1) * 256]), [], [a.b])
            P.dma('pool', b[:], rearr_w(wu[:, fb * 256:(fb + 1) * 256]), [], [b.b])
            for j in range(2):
                fc = fb * 2 + j
                p1 = pg[it % 2]
                p2 = pu[it % 2]
                s = sg[it % 2]
                it += 1
                for k in range(KC):
                    P.mm(p1[:], a[:, k, j * 128:(j + 1) * 128], hT[:, k, :], k == 0, k == KC - 1, [a.b, hT.b], [p1.b])
                for k in range(KC):
                    P.mm(p2[:], b[:, k, j * 128:(j + 1) * 128], hT[:, k, :], k == 0, k == KC - 1, [b.b, hT.b], [p2.b])
                P.act(s[:], p1[:], AF.Silu, [p1.b], [s.b])
                P.dve('tensor_tensor', [s.b, p2.b], [aT.b], out=aT[:, fc, :], in0=s[:], in1=p2[:], op=ALU.mult)
        for nb in range(8):
            w = wdb[nb % nwd]
            P.dma('pool', w[:], wd[:, nb * 256:(nb + 1) * 256].rearrange("(fc p) n -> p fc n", p=128), [], [w.b])
            for tt in range(4):
                p = pd[io % 2]
                s = stg[io % 3]
                io += 1
                for fc in range(NF):
                    P.mm(p[:], aT[:, fc, tt * 128:(tt + 1) * 128], w[:, fc, :], fc == 0, fc == NF - 1, [aT.b, w.b], [p.b])
                if gate is not None:
                    P.act(s[:], p[:], AF.Copy, [p.b, gate.b], [s.b], scale=gate[:, g * 4 + tt:g * 4 + tt + 1])
                else:
                    P.act(s[:], p[:], AF.Copy, [p.b], [s.b])
                P.dma('sp', y_dram[(g * 4 + tt) * 128:(g * 4 + tt + 1) * 128, nb * 256:(nb + 1) * 256], s[:], [s.b], [yb])


def build_moe(ntok=16384, NF=56):
    nc = bass.Bass("TRN2", target_bir_lowering=False)
    hTd = nc.dram_tensor("hT", [D, ntok], BF16, kind="ExternalInput").ap()
    gated = nc.dram_tensor("gate", [128, ntok // 128], F32, kind="ExternalInput").ap()
    wg = nc.dram_tensor("wg", [D, NF * 128], F32, kind="ExternalInput").ap()
    wu = nc.dram_tensor("wu", [D, NF * 128], F32, kind="ExternalInput").ap()
    wd = nc.dram_tensor("wd", [NF * 128, D], F32, kind="ExternalInput").ap()
    y = nc.dram_tensor("y", [ntok, D], F32, kind="ExternalOutput").ap()
    P = Prog(nc)
    C = Ctx(nc, P)
    yb = Buf('y')
    with C.phase():
        gate = C.sb([128, ntok // 128], F32, "gate")
        P.dma('sp', gate[:], gated, [], [gate.b])

        def loader(g, hT):
            P.dma('sp', hT[:], hTd[:, g * TG:(g + 1) * TG].rearrange("(kc p) t -> p kc t", p=128), [], [hT.b])
        emit_swiglu(C, P, loader, ntok // TG, wg, wu, wd, NF, gate, y, yb)
    P.finish()
    return nc


def emit_mod_row(C, P, modrow, gn_m, gn_f, outs):
    gn = [C.sb([128, D], F32, "gn") for _ in range(2)]
    P.dma('sp', gn[0][:], gn_m.partition_broadcast(128), [], [gn[0].b])
    P.dma('sp', gn[1][:], gn_f.partition_broadcast(128), [], [gn[1].b])
    for part in range(6):
        dst = outs[part]
        P.dma('sp', dst[:], modrow[:, part * D:(part + 1) * D].partition_broadcast(128), [], [dst.b])
        if part in (1, 4):
            g = gn[0] if part == 1 else gn[1]
            P.dve('scalar_tensor_tensor', [dst.b, g.b], [dst.b], out=dst[:], in0=dst[:], scalar=1.0, in1=g[:], op0=ALU.add, op1=ALU.mult)


def build_mods():
    nc = bass.Bass("TRN2", target_bir_lowering=False)
    cT = nc.dram_tensor("cT", [128, KC, 128], F32, kind="ExternalInput").ap()
    aw = nc.dram_tensor("aw", [2, D, 1536], F32, kind="ExternalInput").ap()
    ab = nc.dram_tensor("ab", [2, 1, 1536], F32, kind="ExternalInput").ap()
    mo = nc.dram_tensor("mod", [2, 128, 1536], F32, kind="ExternalOutput").ap()
    P = Prog(nc)
    C = Ctx(nc, P)
    with C.phase():
        cs = C.sb([128, KC, 128], F32, "cs")
        w = [C.sb([128, KC, 512], F32, "w") for _ in range(2)]
        bb = C.sb([128, 1536], F32, "bb")
        o = [C.sb([128, 1536], F32, "o") for _ in range(2)]
        ps = [C.ps([128, 512], F32, "ps") for _ in range(2)]
        P.dma('sp', cs[:], cT, [], [cs.b])
        P.act(cs[:], cs[:], AF.Silu, [cs.b], [cs.b])
        it = 0
        for l in range(2):
            P.dma('sp', bb[:], ab[l].partition_broadcast(128), [], [bb.b])
            for ch in range(3):
                ww, p = w[it % 2], ps[it % 2]
                it += 1
                P.dma('sp', ww[:], rearr_w(aw[l][:, ch * 512:(ch + 1) * 512]), [], [ww.b])
                for k in range(KC):
                    P.mm(p[:], cs[:, k, :], ww[:, k, :], k == 0, k == KC - 1, [cs.b, ww.b], [p.b])
                P.dve('tensor_tensor', [p.b, bb.b], [o[l].b], out=o[l][:, ch * 512:(ch + 1) * 512], in0=p[:], in1=bb[:, ch * 512:(ch + 1) * 512], op=ALU.add)
            P.dma('sp', mo[l], o[l][:], [o[l].b], [])
    P.finish()
    return nc


def run_mods(inp):
    c = np.asarray(inp['c'], np.float32)
    cT = np.zeros((128, KC, 128), np.float32)
    cT[:, :, :4] = c.reshape(4, KC, 128).transpose(2, 1, 0)
    aw = np.asarray(inp['ada_w'], np.float32)
    ab = np.asarray(inp['ada_b'], np.float32)
    ims = [{"cT": cT, "aw": np.ascontiguousarray(aw[:, :, i * 1536:(i + 1) * 1536]),
            "ab": np.ascontiguousarray(ab[:, None, i * 1536:(i + 1) * 1536])} for i in range(8)]
    res = run_bass_kernel_spmd(build_mods(), ims, core_ids=list(range(8))).results
    return np.concatenate([np.asarray(res[i]["mod"])[:, :4] for i in range(8)], axis=2)


class AttnRes:
    def __init__(self, C):
        self.sp = [C.ps([128, 512], F32, "sp") for _ in range(2)]
        self.po = [C.ps([128, 256], F32, "po") for _ in range(4)]
        self.sbt = [C.sb([128, 512], F32, "sbt") for _ in range(2)]
        self.pt = [C.sb([128, 512], BF16, "pt") for _ in range(3)]
        self.i = 0


def emit_attn(P, ar, QW, scale, rhs_list, kts, R, out_cb, qc_id, VW=129):
    nj = QW // 128
    for ki, kt in enumerate(kts):
        i = ar.i
        ar.i += 1
        ps = ar.sp[i % 2]
        parts = list(zip(kt['lhsT'], rhs_list)) + list(kt.get('extra', []))
        for pi, (l, r) in enumerate(parts):
            P.mm(ps[:, 0:QW], l, r, pi == 0, pi == len(parts) - 1, R, [ps.b])
        pt = ar.pt[i % 3]
        if kt.get('bias') is not None:
            sb = ar.sbt[i % 2]
            P.dve('scalar_tensor_tensor', [ps.b] + R, [sb.b], out=sb[:, 0:QW], in0=ps[:, 0:QW], scalar=float(scale), in1=kt['bias'], op0=ALU.mult, op1=ALU.add)
            P.act(pt[:, 0:QW], sb[:, 0:QW], AF.Exp, [sb.b], [pt.b])
        else:
            P.act(pt[:, 0:QW], ps[:, 0:QW], AF.Exp, [ps.b], [pt.b], scale=float(scale))
        for j in range(nj):
            po = ar.po[j]
            P.mm(po[:, 0:VW], pt[:, j * 128:(j + 1) * 128], kt['v'], ki == 0, ki == len(kts) - 1, [pt.b] + R, [po.b])
    for j in range(nj):
        out_cb(qc_id, j, ar.po[j])


DIL = (1, 4, 16)


def norm_bufs(C):
    xb = [C.sb([128, D], F32, "xb") for _ in range(2)]
    hb = [C.sb([128, D], BF16, "hb") for _ in range(2)]
    junk = C.sb([128, D], BF16, "junk")
    st = C.sb([128, 4], F32, "st")
    pst = [C.ps([128, 1024], BF16, "pst") for _ in range(2)]
    return (xb, hb, junk, st, pst)


def build_l1mix():
    nc = bass.Bass("TRN2", target_bir_lowering=False)
    dt = nc.dram_tensor
    xl = dt("xl", [4096, D], F32, kind="ExternalInput").ap()
    modrow = dt("modrow", [1, 6 * D], F32, kind="ExternalInput").ap()
    gn_m = dt("gn_m", [1, D], F32, kind="ExternalInput").ap()
    gn_f = dt("gn_f", [1, D], F32, kind="ExternalInput").ap()
    w_in = dt("c_w_in", [D, 9216], F32, kind="ExternalInput").ap()
    w_out = dt("c_w_out", [1024, D], F32, kind="ExternalInput").ap()
    wrT = dt("wrT", [1, 8 * D], F32, kind="ExternalInput").ap()
    Fd = dt("Fd", [24, 128, 1024], F32, kind="ExternalInput").ap()
    ctxvd = dt("ctxv", [128, 1], F32, kind="ExternalInput").ap()
    identd = dt("ident", [128, 128], F32, kind="ExternalInput").ap()
    x1m = dt("x1m", [2048, D], F32, kind="ExternalOutput").ap()
    hTf = dt("hTf", [D, 2048], BF16, kind="ExternalOutput").ap()
    gates_o = dt("gates", [2048, 8], F32, kind="ExternalOutput").ap()
    gf_o = dt("gf", [1, D], F32, kind="ExternalOutput").ap()
    QT = dt("QT", [3, 8, 128, 2048], BF16).ap()
    KT = dt("KT", [3, 8, 128, 4096], BF16).ap()
    Vd = dt("Vd", [3, 4096, 1024], BF16).ap()
    ND = dt("ND", [3, 2048, 8, 129], F32).ap()
    bQT, bKT, bV, bND, bX1 = Buf('QT'), Buf('KT'), Buf('V'), Buf('ND'), Buf('x1m')
    P = Prog(nc)
    C = Ctx(nc, P)
    with ExitStack() as keep:
        def sbp(shape, dtp, name):
            C.n += 1
            return T(keep.enter_context(nc.sbuf_tensor(f"{name}_{C.n}", list(shape), dtp)), name)
        mods = [sbp([128, D], F32, f"mod{i}") for i in range(6)]
        ident = sbp([128, 128], BF16, "ident")
        ctxv = sbp([128, 1], F32, "ctxv")
        with C.phase():
            idf = C.sb([128, 128], F32, "idf")
            P.dma('sp', idf[:], identd, [], [idf.b])
            P.dve('tensor_copy', [idf.b], [ident.b], out=ident[:], in_=idf[:])
            P.dma('sp', ctxv[:], ctxvd, [], [ctxv.b])
            emit_mod_row(C, P, modrow, gn_m, gn_f, mods)
            P.dma('sp', gf_o, mods[5][0:1, :], [mods[5].b], [])
        with C.phase():
            nb_ = norm_bufs(C)
            hT = C.sb([128, KC, TG], BF16, "hT")
            wb = [C.sb([128, KC, 512], BF16, "wb") for _ in range(2)]
            ps = [C.ps([128, 512], F32, "psz") for _ in range(2)]
            stg = [C.sb([128, 512], BF16, "stg") for _ in range(3)]
            it = 0
            for gi in range(8):
                tok0 = gi * TG
                emit_norm_T(C, P, nb_, xl[tok0:tok0 + TG, :], mods[1], mods[0], ident, hT)
                for blk in range(18):
                    g, j, hb2 = blk // 6, (blk % 6) // 2, blk % 2
                    if j == 0 and gi < 4:
                        continue
                    w = wb[blk % 2]
                    P.dma('pool', w[:], rearr_w(w_in[:, blk * 512:(blk + 1) * 512]), [], [w.b])
                    for q4 in range(4):
                        p = ps[it % 2]
                        s = stg[it % 3]
                        it += 1
                        if j < 2:
                            h = hb2 * 4 + q4
                            for k in range(KC):
                                P.mm(p[:], w[:, k, q4 * 128:(q4 + 1) * 128], hT[:, k, :], k == 0, k == KC - 1, [w.b, hT.b], [p.b])
                        else:
                            for k in range(KC):
                                P.mm(p[:], hT[:, k, q4 * 128:(q4 + 1) * 128], w[:, k, :], k == 0, k == KC - 1, [w.b, hT.b], [p.b])
                        if it % 2 == 0:
                            P.act(s[:], p[:], AF.Copy, [p.b], [s.b])
                        else:
                            P.dve('tensor_copy', [p.b], [s.b], out=s[:], in_=p[:])
                        if j == 0:
                            P.dma('sp', QT[g, h, :, tok0 - 2048:tok0 - 2048 + TG], s[:], [s.b], [bQT])
                        elif j == 1:
                            P.dma('sp', KT[g, h, :, tok0:tok0 + TG], s[:], [s.b], [bKT])
                        else:
                            P.dma('sp', Vd[g, tok0 + q4 * 128:tok0 + (q4 + 1) * 128, hb2 * 512:(hb2 + 1) * 512], s[:], [s.b], [bV])
        with C.phase():
            ar = AttnRes(C)
            kT = C.sb([128, 4096], BF16, "kT")
            qT = C.sb([128, 2048], BF16, "qT")
            V1 = C.sb([128, 32, 129], BF16, "V1")
            F = C.sb([128, 1024], F32, "F")
            ostg = [C.sb([128, 129], F32, "ostg") for _ in range(4)]
            oi = [0]
            for g in range(3):
                dil = DIL[g]
                TPR = 32 // dil
                nsub = 4096 // dil
                QW = min(512, nsub // 2)
                for h in range(8):
                    P.dma('sp', kT[:], KT[g, h], [bKT], [kT.b])
                    P.dma('sp', qT[:], QT[g, h], [bQT], [qT.b])
                    P.dma('sp', F[:], Fd[g * 8 + h], [], [F.b])
                    P.dve('memset', [], [V1.b], V1[:, :, 128:129], 1.0)
                    vsrc = Vd[g].rearrange("(kt p dl) c -> dl p kt c", p=128, dl=dil)
                    for r in range(dil):
                        P.dma('sp', V1[:, r * TPR:(r + 1) * TPR, 0:128], vsrc[r][:, :, h * 128:(h + 1) * 128], [bV], [V1.b])
                    v4 = V1[:].rearrange("p (r t) c -> p r t c", r=dil)[:, :, 0:TPR // 2, :]
                    P.dve('tensor_scalar', [V1.b, ctxv.b], [V1.b], out=v4, in0=v4, scalar1=ctxv[:, 0:1], scalar2=None, op0=ALU.mult)
                    nd_dst = ND[g].rearrange("(m dl) h c -> dl m h c", dl=dil)
                    for r in range(dil):
                        for q0 in range(nsub // 2, nsub, QW):
                            c0 = q0 * dil + r - 2048
                            rhs = qT[:, c0:c0 + (QW - 1) * dil + 1:dil]
                            kts = []
                            for kt in range(max(0, q0 // 128 - 1), (q0 + QW) // 128):
                                o = (q0 - kt * 128) // 128
                                k0 = kt * 128 * dil + r
                                kts.append(dict(lhsT=[kT[:, k0:k0 + 127 * dil + 1:dil]], v=V1[:, r * TPR + kt, :],
                                                bias=F[:, 128 * (o + 3):128 * (o + 3) + QW]))

                            def cb(qc, j, po, g=g, h=h, r=r, q0=q0, nsub=nsub, nd_dst=nd_dst):
                                s = ostg[oi[0] % 4]
                                oi[0] += 1
                                P.act(s[:], po[:, 0:129], AF.Copy, [po.b], [s.b])
                                m0 = q0 + j * 128 - nsub // 2
                                P.dma('sp', nd_dst[r][m0:m0 + 128, h, :], s[:], [s.b], [bND])
                            emit_attn(P, ar, QW, 128 ** -0.5, [rhs], kts, [kT.b, qT.b, V1.b, F.b], cb, 0)
        with C.phase():
            wo = C.sb([128, 8, D], BF16, "wo")
            P.dma('pool', wo[:], w_out.rearrange("(kc p) n -> p kc n", p=128), [], [wo.b])
            nd = [C.sb([128, 8, 129], F32, "nd") for _ in range(3)]
            rec = C.sb([128, 8], F32, "rec")
            ob = C.sb([128, 8, 128], BF16, "ob")
            oT = C.sb([128, 8, TG], BF16, "oT")
            xr = [C.sb([128, D], F32, "xr") for _ in range(2)]
            tmp = [C.sb([128, 512], F32, "tmp") for _ in range(2)]
            ptr = C.ps([128, 1024], BF16, "ptr")
            py = [C.ps([128, 512], F32, "py") for _ in range(2)]
            it = 0
            for gi in range(4):
                for tt in range(4):
                    t0 = gi * TG + tt * 128
                    for g in range(3):
                        P.dma('sp', nd[g][:], ND[g, t0:t0 + 128], [bND], [nd[g].b])
                    P.dve('tensor_tensor', [nd[0].b, nd[1].b], [nd[0].b], out=nd[0][:], in0=nd[0][:], in1=nd[1][:], op=ALU.add)
                    P.dve('tensor_tensor', [nd[0].b, nd[2].b], [nd[0].b], out=nd[0][:], in0=nd[0][:], in1=nd[2][:], op=ALU.add)
                    P.dve('tensor_scalar', [nd[0].b], [rec.b], out=rec[:], in0=nd[0][:, :, 128], scalar1=1e-30, scalar2=None, op0=ALU.max)
                    P.dve('reciprocal', [rec.b], [rec.b], out=rec[:], in_=rec[:])
                    P.dve('tensor_tensor', [nd[0].b, rec.b], [ob.b], out=ob[:], in0=nd[0][:, :, 0:128], in1=rec[:].unsqueeze(2).to_broadcast([128, 8, 128]), op=ALU.mult)
                    for c in range(8):
                        P.tr(ptr[:, c * 128:(c + 1) * 128], ob[:, c, :], ident[:], [ob.b, ident.b], [ptr.b])
                    P.act(oT[:, :, tt * 128:(tt + 1) * 128], ptr[:].rearrange("p (c t) -> p c t", c=8), AF.Copy, [ptr.b], [oT.b])
                for tt in range(4):
                    t0 = gi * TG + tt * 128
                    x = xr[tt % 2]
                    P.dma('sp', x[:], xl[2048 + t0:2048 + t0 + 128, :], [], [x.b])
                    for nb in range(4):
                        p = py[it % 2]
                        tm = tmp[it % 2]
                        it += 1
                        sl = slice(nb * 512, (nb + 1) * 512)
                        for kc in range(8):
                            P.mm(p[:], oT[:, kc, tt * 128:(tt + 1) * 128], wo[:, kc, sl], kc == 0, kc == 7, [oT.b, wo.b], [p.b])
                        P.dve('tensor_tensor', [p.b, mods[2].b], [tm.b], out=tm[:], in0=p[:], in1=mods[2][:, sl], op=ALU.mult)
                        P.dve('tensor_tensor', [tm.b, x.b], [x.b], out=x[:, sl], in0=x[:, sl], in1=tm[:], op=ALU.add)
                    P.dma('sp', x1m[t0:t0 + 128, :], x[:], [x.b], [bX1])
        with C.phase():
            nb_ = norm_bufs(C)
            hT = C.sb([128, KC, TG], BF16, "hT")
            wr = C.sb([128, 8, D], F32, "wr")
            P.dma('sp', wr[:].rearrange("p e d -> p (e d)"), wrT.partition_broadcast(128), [], [wr.b])
            rj = C.sb([128, D], F32, "rj")
            rj2 = C.sb([128, D], F32, "rj2")
            lg = [C.sb([128, 8], F32, "lg") for _ in range(2)]
            mx = [C.sb([128, 8], F32, "mx") for _ in range(2)]
            ex = [C.sb([128, 8], F32, "ex") for _ in range(2)]
            sm = [C.sb([128, 2], F32, "sm") for _ in range(2)]
            for gi in range(4):
                def tile_cb(tt, x32, gi=gi):
                    l, m, e, s2 = lg[tt % 2], mx[tt % 2], ex[tt % 2], sm[tt % 2]
                    for ee in range(8):
                        P.dve('tensor_tensor', [x32.b, wr.b], [rj.b], out=rj[:], in0=x32[:], in1=wr[:, ee, :], op=ALU.mult)
                        P.act(rj2[:], rj[:], AF.Copy, [rj.b], [rj2.b, l.b], accum_out=l[:, ee:ee + 1])
                    P.dve('max', [l.b], [m.b], out=m[:], in_=l[:])
                    P.dve('tensor_scalar', [m.b], [s2.b], out=s2[:, 0:1], in0=m[:, 0:1], scalar1=-1.0, scalar2=None, op0=ALU.mult)
                    P.act(e[:], l[:], AF.Exp, [l.b, s2.b], [e.b], bias=s2[:, 0:1], scale=1.0)
                    P.dve('scalar_tensor_tensor', [l.b, m.b, e.b], [e.b], out=e[:], in0=l[:], scalar=m[:, 1:2], in1=e[:], op0=ALU.is_ge, op1=ALU.mult)
                    P.dve('tensor_reduce', [e.b], [s2.b], out=s2[:, 1:2], in_=e[:], axis=AX.X, op=ALU.add)
                    P.dve('reciprocal', [s2.b], [s2.b], out=s2[:, 1:2], in_=s2[:, 1:2])
                    P.dve('tensor_scalar', [e.b, s2.b], [e.b], out=e[:], in0=e[:], scalar1=s2[:, 1:2], scalar2=None, op0=ALU.mult)
                    t0 = gi * TG + tt * 128
                    P.dma('sp', gates_o[t0:t0 + 128, :], e[:], [e.b], [])
                emit_norm_T(C, P, nb_, x1m[gi * TG:(gi + 1) * TG, :], mods[4], mods[3], ident, hT, xdeps=[bX1], tile_cb=tile_cb)
                P.dma('sp', hTf[:, gi * TG:(gi + 1) * TG].rearrange("(kc p) t -> p kc t", p=128), hT[:], [hT.b], [])
        P.finish()
    return nc


NEG = -1e30


def t5_bucket_np(dist):
    n = np.maximum(dist, 0)
    nf = np.maximum(n, 1).astype(np.float32)
    large = 16 + (np.log(nf / np.float32(16)) / np.float32(np.log(2048 / 16)) * np.float32(16)).astype(np.int32)
    large = np.minimum(large, 31)
    return np.where(n < 16, n, large)


def toeplitz_table(rel_bias, heads, width, shift, dist_scale, max_back):
    k = np.arange(128)[:, None]
    j = np.arange(width)[None, :]
    d = j - k - shift
    ok = (d >= 0) & (d <= max_back)
    bk = t5_bucket_np(d * dist_scale)
    out = np.empty((len(heads), 128, width), np.float32)
    for i, h in enumerate(heads):
        out[i] = np.where(ok, rel_bias[bk, h], np.float32(NEG))
    return out


def make_Fd(rel_bias):
    return np.concatenate([toeplitz_table(rel_bias, range(8), 1024, 384, dil, 128) for dil in DIL], 0)


BIGM = 30000.0
PI = float(np.pi)


def build_l0(debug=False, upto=5):
    nc = bass.Bass("TRN2", target_bir_lowering=False)

    def din(name, shape, dtp=F32):
        return nc.dram_tensor(name, list(shape), dtp, kind="ExternalInput").ap()

    def dscr(name, shape, dtp):
        return nc.dram_tensor(name, list(shape), dtp, kind="ExternalOutput" if debug else "Internal").ap()
    xl = din("xl", [4096, D])
    modrow = din("modrow", [1, 6 * D])
    gn_m = din("gn_m", [1, D])
    gn_f = din("gn_f", [1, D])
    w_in = din("w_in", [D, 3672])
    w_out = din("w_out", [D, D])
    pos_kv = [din("pos_k", [32, 128]), din("pos_v", [32, 128])]
    w1_kv = [din("w1_k", [4096, 128]), din("w1_v", [4096, 128])]
    w2_kv = [din("w2_k", [128, 128]), din("w2_v", [128, 128])]
    qng = din("qng", [128, 4])
    kvng = din("kvng", [128, 4])
    w_uq = din("w_uq", [512, 1536])
    w_ukv = din("w_ukv", [512, 2048])
    if upto >= 5:
        wg = din("wg", [D, 5632])
        wu = din("wu", [D, 5632])
        wd = din("wd", [5632, D])
    posd = din("pos", [1, 4096], I32)
    invf = din("invf", [64, 2])
    Fwin = din("Fwin", [8, 128, 1408])
    Fslc = din("Fslc", [8, 128, 4864])
    Fmla = din("Fmla", [128, 1024])
    Fcmp = din("Fcmp", [8, 2, 128, 2048])
    vmul = din("vmul", [2048, 64])
    addm = din("addm", [2048, 64])
    Amat = din("Amat", [256, 64])
    Emat = din("Emat", [64, 4096])
    ctxvd = din("ctxv", [128, 1])
    identd = din("ident", [128, 128])
    x1 = nc.dram_tensor("x1", [2048, D], F32, kind="ExternalOutput").ap()
    QTn = dscr("QTn", [8, 128, 2048], BF16)
    KX = dscr("KX", [4, 2, 128, 4096], BF16)
    VX = dscr("VX", [2, 4096, 256], BF16)
    GT = dscr("GT", [2048, 24], F32)
    QN = dscr("QN", [8, 128, 2048], BF16)
    QR = dscr("QR", [8, 64, 2048], BF16)
    KN = dscr("KN", [8, 128, 4096], BF16)
    KR = dscr("KR", [64, 4096], BF16)
    VM = dscr("VM", [4096, 1024], BF16)
    KC_ = dscr("KCc", [2, 128, 256], BF16)
    VC_ = dscr("VCc", [2, 256, 128], BF16)
    OALL = dscr("OALL", [2048, D], BF16)
    X1A = dscr("X1A", [2048, D], F32)
    YF = dscr("YF", [2048, D], F32)
    bS = {k: Buf(k) for k in ("QTn", "KX", "VX", "GT", "QN", "QR", "KN", "KR", "VM", "KC", "VC", "OALL", "X1A", "YF")}
    P = Prog(nc)
    C = Ctx(nc, P)
    with ExitStack() as keep:
        def sbp(shape, dtp, name):
            C.n += 1
            return T(keep.enter_context(nc.sbuf_tensor(f"{name}_{C.n}", list(shape), dtp)), name)
        mods = [sbp([128, D], F32, f"mod{i}") for i in range(6)]
        ident = sbp([128, 128], BF16, "ident")
        identf = sbp([128, 128], F32, "identf")
        ctxv = sbp([128, 1], F32, "ctxv")
        ones = sbp([128, 128], F32, "ones")
        with C.phase():
            P.dma('sp', identf[:], identd, [], [identf.b])
            P.dve('tensor_copy', [identf.b], [ident.b], out=ident[:], in_=identf[:])
            P.dma('sp', ctxv[:], ctxvd, [], [ctxv.b])
            P.dve('memset', [], [ones.b], ones[:], 1.0)
            emit_mod_row(C, P, modrow, gn_m, gn_f, mods)

        with C.phase():
            nb_ = norm_bufs(C)
            hT = C.sb([128, KC, TG], BF16, "hT")
            wb = [C.sb([128, KC, 512], BF16, "wb") for _ in range(2)]
            ps = [C.ps([128, 512], F32, "psz") for _ in range(2)]
            pq = C.ps([128, 512], F32, "pq")
            pr = [C.ps([128, 512], F32, "pr") for _ in range(2)]
            stg = [C.sb([128, 512], BF16, "stg") for _ in range(3)]
            gst = [C.sb([128, 24], F32, "gst") for _ in range(2)]
            wuq = C.sb([128, 4, 1536], BF16, "wuq")
            wuqs = C.sb([128, 4, 8, 64], BF16, "wuqs")
            wkn = C.sb([128, 4, 8, 128], BF16, "wkn")
            wv = C.sb([128, 4, 8, 128], BF16, "wv")
            wpe = C.sb([128, KC, 128], BF16, "wpe")
            gq = C.sb([128, 4], F32, "gq")
            gkv = C.sb([128, 4], F32, "gkv")
            ivf = C.sb([64, 2], F32, "ivf")
            _lf = C.sb([128, 4, TG], F32, "latf")
            latf = [_lf, _lf]
            sq = C.sb([128, 4, TG], F32, "sq")
            latb = [C.sb([128, 4, TG], BF16, "latb") for _ in range(2)]
            rbc = [C.sb([128, TG], F32, "rbc") for _ in range(2)]
            rtm = C.sb([128, 4], F32, "rtm")
            posi = C.sb([128, TG], I32, "posi")
            ang = C.sb([64, TG], F32, "ang")
            CC = C.sb([64, TG], F32, "CC")
            SS = C.sb([64, TG], F32, "SS")
            rA = C.sb([64, TG], F32, "rA")
            rB = C.sb([64, TG], F32, "rB")
            P.dma('pool', wuq[:], w_uq.rearrange("(kc p) n -> p kc n", p=128), [], [wuq.b])
            uq4 = w_uq.rearrange("(kc p) (h c) -> p kc h c", p=128, c=192)
            ukv5 = w_ukv.rearrange("(kc p) (h two d) -> p kc h two d", p=128, two=2, d=128)
            for c4 in range(4):
                P.dma('pool', wuqs[:, c4, :, 0:32], uq4[:, c4, :, 160:192], [], [wuqs.b])
                P.dma('pool', wuqs[:, c4, :, 32:64], uq4[:, c4, :, 128:160], [], [wuqs.b])
                P.dma('pool', wkn[:, c4], ukv5[:, c4, :, 0, :], [], [wkn.b])
                P.dma('pool', wv[:, c4], ukv5[:, c4, :, 1, :], [], [wv.b])
            P.dma('pool', wpe[:, :, 0:64], rearr_w(w_in[:, 3608:3672]), [], [wpe.b])
            P.dma('pool', wpe[:, :, 64:96], rearr_w(w_in[:, 3640:3672]), [], [wpe.b])
            P.dma('pool', wpe[:, :, 96:128], rearr_w(w_in[:, 3608:3640]), [], [wpe.b])
            P.dma('sp', gq[:], qng, [], [gq.b])
            P.dma('sp', gkv[:], kvng, [], [gkv.b])
            P.dma('sp', ivf[:], invf, [], [ivf.b])
            it = 0

            def evac(p, s, M=128, W=TG):
                nonlocal it
                it += 1
                if it % 2 == 0:
                    P.act(s[0:M, 0:W], p[0:M, 0:W], AF.Copy, [p.b], [s.b])
                else:
                    P.dve('tensor_copy', [p.b], [s.b], out=s[0:M, 0:W], in_=p[0:M, 0:W])

            for gi in range(8):
                tok0 = gi * TG
                own = gi >= 4
                emit_norm_T(C, P, nb_, xl[tok0:tok0 + TG, :], mods[1], mods[0], ident, hT)
                P.dma('sp', posi[:], posd[:, tok0:tok0 + TG].partition_broadcast(128), [], [posi.b])
                P.dve('tensor_copy', [posi.b], [ang.b], out=ang[:], in_=posi[0:64, :])
                P.dve('tensor_scalar', [ang.b, ivf.b], [ang.b], out=ang[:], in0=ang[:], scalar1=ivf[:, 0:1], scalar2=None, op0=ALU.mult)
                for dst, shift in ((SS, 0.0), (CC, 0.5 * PI)):
                    P.dve('tensor_scalar', [ang.b], [dst.b], out=dst[:], in0=ang[:], scalar1=shift, scalar2=1.0 / (2 * PI), op0=ALU.add, op1=ALU.mult)
                    P.dve('tensor_copy', [dst.b], [posi.b], out=posi[0:64, :], in_=dst[:])
                    P.dve('tensor_copy', [posi.b], [rA.b], out=rA[:], in_=posi[0:64, :])
                    P.dve('tensor_scalar', [ang.b], [dst.b], out=dst[:], in0=ang[:], scalar1=shift, scalar2=None, op0=ALU.add)
                    P.dve('scalar_tensor_tensor', [rA.b, dst.b], [dst.b], out=dst[:], in0=rA[:], scalar=-2 * PI, in1=dst[:], op0=ALU.mult, op1=ALU.add)
                    P.dve('tensor_scalar', [dst.b], [dst.b], out=dst[:], in0=dst[:], scalar1=PI, scalar2=-PI, op0=ALU.min, op1=ALU.max)
                    P.act(dst[:], dst[:], AF.Sin, [dst.b], [dst.b])
                P.dve('tensor_scalar', [SS.b, ivf.b], [SS.b], out=SS[:], in0=SS[:], scalar1=ivf[:, 1:2], scalar2=None, op0=ALU.mult)
                for blk in range(5):
                    c0 = blk * 512
                    ncols = min(512, 3672 - c0)
                    if blk < 2 and not own:
                        continue
                    w = wb[blk % 2]
                    P.dma('pool', w[:, :, 0:ncols], rearr_w(w_in[:, c0:c0 + ncols]), [], [w.b])

                    def fm(m0, M, p):
                        for k in range(KC):
                            P.mm(p[0:M, :], w[:, k, m0:m0 + M], hT[:, k, :], k == 0, k == KC - 1, [w.b, hT.b], [p.b])
                    if blk < 2:
                        for q4 in range(4):
                            p, s = ps[it % 2], stg[it % 3]
                            fm(q4 * 128, 128, p)
                            evac(p, s)
                            P.dma('sp', QTn[blk * 4 + q4, :, tok0 - 2048:tok0 - 2048 + TG], s[:], [s.b], [bS["QTn"]])
                    elif blk == 2:
                        for q4 in range(4):
                            p, s = ps[it % 2], stg[it % 3]
                            fm(q4 * 128, 128, p)
                            evac(p, s)
                            P.dma('sp', KX[q4 // 2, q4 % 2, :, tok0:tok0 + TG], s[:], [s.b], [bS["KX"]])
                    elif blk in (3, 4):
                        kind = blk - 3
                        for g in range(2):
                            p, s = ps[it % 2], stg[it % 3]
                            fm(g * 128, 128, p)
                            evac(p, s)
                            P.dma('sp', KX[2 + kind, g, :, tok0:tok0 + TG], s[:], [s.b], [bS["KX"]])
                        for tt in range(4):
                            p, s = ps[it % 2], stg[it % 3]
                            for k in range(KC):
                                P.mm(p[:, 0:256], hT[:, k, tt * 128:(tt + 1) * 128], w[:, k, 256:512], k == 0, k == KC - 1, [w.b, hT.b], [p.b])
                            evac(p, s, 128, 256)
                            P.dma('sp', VX[kind, tok0 + tt * 128:tok0 + (tt + 1) * 128, :], s[:, 0:256], [s.b], [bS["VX"]])
                if own:
                    w = wb[0]
                    P.dma('pool', w[:, :, 0:24], rearr_w(w_in[:, 2560:2584]), [], [w.b])
                    for tt in range(4):
                        p, gs = ps[it % 2], gst[tt % 2]
                        it += 1
                        for k in range(KC):
                            P.mm(p[:, 0:24], hT[:, k, tt * 128:(tt + 1) * 128], w[:, k, 0:24], k == 0, k == KC - 1, [w.b, hT.b], [p.b])
                        P.act(gs[:], p[:, 0:24], AF.Sigmoid, [p.b], [gs.b])
                        t0 = tok0 - 2048 + tt * 128
                        P.dma('sp', GT[t0:t0 + 128, :], gs[:], [gs.b], [bS["GT"]])
                for li in range(2):
                    if li == 0 and not own:
                        continue
                    w = wb[1 - li]
                    cb0 = 2584 + li * 512
                    P.dma('pool', w[:], rearr_w(w_in[:, cb0:cb0 + 512]), [], [w.b])
                    lf, lb, gg, rb_ = latf[li], latb[li], (gq if li == 0 else gkv), rbc[li]
                    for c4 in range(4):
                        p = ps[it % 2]
                        it += 1
                        for k in range(KC):
                            P.mm(p[:], w[:, k, c4 * 128:(c4 + 1) * 128], hT[:, k, :], k == 0, k == KC - 1, [w.b, hT.b], [p.b])
                        P.act(lf[:, c4, :], p[:], AF.Copy, [p.b], [lf.b])
                    P.act(sq[:], lf[:], AF.Square, [lf.b], [sq.b])
                    for c4 in range(4):
                        P.mm(pq[:], ones[:], sq[:, c4, :], c4 == 0, c4 == 3, [ones.b, sq.b], [pq.b])
                    P.dve('tensor_scalar', [pq.b], [rb_.b], out=rb_[:], in0=pq[:], scalar1=1.0 / 512, scalar2=EPS, op0=ALU.mult, op1=ALU.add)
                    P.act(rb_[:], rb_[:], AF.Sqrt, [rb_.b], [rb_.b])
                    P.dve('reciprocal', [rb_.b], [rb_.b], out=rb_[:], in_=rb_[:])
                    for c4 in range(4):
                        P.dve('tensor_scalar', [lf.b, gg.b], [lb.b], out=lb[:, c4, :], in0=lf[:, c4, :], scalar1=gg[:, c4:c4 + 1], scalar2=None, op0=ALU.mult)
                    if li == 1:
                        for tt in range(4):
                            p = ps[it % 2]
                            it += 1
                            for c4 in range(4):
                                P.mm(p[:, 0:8], sq[:, c4, tt * 128:(tt + 1) * 128], ones[:, 0:8], c4 == 0, c4 == 3, [sq.b, ones.b], [p.b])
                            P.dve('tensor_scalar', [p.b], [rtm.b], out=rtm[:, tt:tt + 1], in0=p[:, 0:1], scalar1=1.0 / 512, scalar2=EPS, op0=ALU.mult, op1=ALU.add)
                        P.act(rtm[:], rtm[:], AF.Sqrt, [rtm.b], [rtm.b])
                        P.dve('reciprocal', [rtm.b], [rtm.b], out=rtm[:], in_=rtm[:])

                def rope(pa, pb, mul, s):
                    P.dve('tensor_tensor', [pa.b, CC.b], [rA.b], out=rA[:], in0=pa[0:64, :], in1=CC[:], op=ALU.mult)
                    P.dve('tensor_tensor', [pb.b, SS.b], [rB.b], out=rB[:], in0=pb[0:64, :], in1=SS[:], op=ALU.mult)
                    if mul is None:
                        P.dve('tensor_tensor', [rA.b, rB.b], [s.b], out=s[0:64, :], in0=rA[:], in1=rB[:], op=ALU.add)
                    else:
                        P.dve('tensor_tensor', [rA.b, rB.b], [rA.b], out=rA[:], in0=rA[:], in1=rB[:], op=ALU.add)
                        P.dve('tensor_tensor', [rA.b, mul.b], [s.b], out=s[0:64, :], in0=rA[:], in1=mul[0:64, :], op=ALU.mult)
                for k in range(KC):
                    P.mm(pr[0][0:64, :], wpe[:, k, 0:64], hT[:, k, :], k == 0, k == KC - 1, [wpe.b, hT.b], [pr[0].b])
                for k in range(KC):
                    P.mm(pr[1][0:64, :], wpe[:, k, 64:128], hT[:, k, :], k == 0, k == KC - 1, [wpe.b, hT.b], [pr[1].b])
                s = stg[it % 3]
                it += 1
                rope(pr[0], pr[1], None, s)
                P.dma('sp', KR[:, tok0:tok0 + TG], s[0:64, :], [s.b], [bS["KR"]])
                if own:
                    lb = latb[0]
                    for h in range(8):
                        p, s = ps[it % 2], stg[it % 3]
                        it += 1
                        for c4 in range(4):
                            P.mm(p[:], wuq[:, c4, h * 192:h * 192 + 128], lb[:, c4, :], c4 == 0, c4 == 3, [wuq.b, lb.b], [p.b])
                        P.dve('tensor_tensor', [p.b, rbc[0].b], [s.b], out=s[:], in0=p[:], in1=rbc[0][:], op=ALU.mult)
                        P.dma('sp', QN[h, :, tok0 - 2048:tok0 - 2048 + TG], s[:], [s.b], [bS["QN"]])
                        for c4 in range(4):
                            P.mm(pr[0][0:64, :], wuq[:, c4, h * 192 + 128:h * 192 + 192], lb[:, c4, :], c4 == 0, c4 == 3, [wuq.b, lb.b], [pr[0].b])
                        for c4 in range(4):
                            P.mm(pr[1][0:64, :], wuqs[:, c4, h, :], lb[:, c4, :], c4 == 0, c4 == 3, [wuqs.b, lb.b], [pr[1].b])
                        s = stg[it % 3]
                        it += 1
                        rope(pr[0], pr[1], rbc[0], s)
                        P.dma('sp', QR[h, :, tok0 - 2048:tok0 - 2048 + TG], s[0:64, :], [s.b], [bS["QR"]])
                lb = latb[1]
                for h in range(8):
                    p, s = ps[it % 2], stg[it % 3]
                    it += 1
                    for c4 in range(4):
                        P.mm(p[:], wkn[:, c4, h, :], lb[:, c4, :], c4 == 0, c4 == 3, [wkn.b, lb.b], [p.b])
                    P.dve('tensor_tensor', [p.b, rbc[1].b], [s.b], out=s[:], in0=p[:], in1=rbc[1][:], op=ALU.mult)
                    P.dma('sp', KN[h, :, tok0:tok0 + TG], s[:], [s.b], [bS["KN"]])
                for tt in range(4):
                    for hb2 in range(2):
                        p, s = ps[it % 2], stg[it % 3]
                        it += 1
                        for c4 in range(4):
                            P.mm(p[:], lb[:, c4, tt * 128:(tt + 1) * 128], wv[:, c4, hb2 * 4:(hb2 + 1) * 4, :].rearrange("p h d -> p (h d)"), c4 == 0, c4 == 3, [wv.b, lb.b], [p.b])
                        P.act(s[:], p[:], AF.Copy, [p.b, rtm.b], [s.b], scale=rtm[:, tt:tt + 1])
                        P.dma('sp', VM[tok0 + tt * 128:tok0 + (tt + 1) * 128, hb2 * 512:(hb2 + 1) * 512], s[:], [s.b], [bS["VM"]])
        build_l0_rest(nc, C, P, locals())
        P.finish()
    return nc


def build_l0_rest(nc, C, P, L):
    KX, VX, GT, QTn, QN, QR, KN, KR, VM = (L[k] for k in ("KX", "VX", "GT", "QTn", "QN", "QR", "KN", "KR", "VM"))
    KC_, VC_, OALL, X1A, YF, bS = (L[k] for k in ("KC_", "VC_", "OALL", "X1A", "YF", "bS"))
    ident, identf, ctxv, mods, xl = L["ident"], L["identf"], L["ctxv"], L["mods"], L["xl"]
    upto = L["upto"]
    if upto < 2:
        return
    with C.phase():
        kT = C.sb([128, 4096], BF16, "kT")
        w1 = C.sb([128, 32, 128], BF16, "w1")
        w2 = C.sb([128, 128], BF16, "w2")
        posq = C.sb([32, 128], F32, "posq")
        posb = C.sb([128, 32, 8], BF16, "posb")
        bia = C.sb([128, 1], F32, "bia")
        u = C.sb([128, 256], F32, "u")
        t1 = C.sb([128, 256], F32, "t1")
        hid = C.sb([128, 256], BF16, "hid")
        so = [C.sb([128, 256], BF16, "so") for _ in range(2)]
        ph = C.ps([128, 256], F32, "ph")
        pb = C.ps([128, 64], F32, "pb")
        po = [C.ps([128, 256], F32, "po") for _ in range(2)]
        for kind in range(2):
            P.dma('pool', w1[:], L["w1_kv"][kind].rearrange("(l d) e -> d l e", d=128), [], [w1.b])
            P.dma('pool', w2[:], L["w2_kv"][kind], [], [w2.b])
            P.dma('sp', posq[:], L["pos_kv"][kind], [], [posq.b])
            P.tr(pb[:, 0:32], posq[:, :], identf[0:32, 0:32], [posq.b, identf.b], [pb.b])
            P.dve('tensor_copy', [pb.b], [posb.b], out=posb[:], in_=pb[:, 0:32].unsqueeze(2).to_broadcast([128, 32, 8]))
            for l in range(32):
                P.mm(pb[:, 32:40], w1[:, l, :], posb[:, l, :], l == 0, l == 31, [w1.b, posb.b], [pb.b])
            P.act(bia[:], pb[:, 32:33], AF.Copy, [pb.b], [bia.b])
            for g in range(2):
                P.dma('sp', kT[:], KX[kind, g], [bS["KX"]], [kT.b])
                for l in range(32):
                    P.mm(ph[:, 0:255], w1[:, l, :], kT[:, l:l + 16 * 254 + 1:16], l == 0, l == 31, [w1.b, kT.b], [ph.b])
                P.dve('memset', [], [u.b], u[:], 0.0)
                P.dve('tensor_scalar', [ph.b, bia.b], [u.b], out=u[:, 0:255], in0=ph[:, 0:255], scalar1=bia[:, 0:1], scalar2=None, op0=ALU.add)
                P.dve('tensor_tensor', [u.b], [t1.b], out=t1[:], in0=u[:], in1=u[:], op=ALU.mult)
                P.dve('tensor_scalar', [t1.b], [t1.b], out=t1[:], in0=t1[:], scalar1=0.044715, scalar2=1.0, op0=ALU.mult, op1=ALU.add)
                P.dve('tensor_tensor', [t1.b, u.b], [t1.b], out=t1[:], in0=t1[:], in1=u[:], op=ALU.mult)
                P.act(t1[:], t1[:], AF.Tanh, [t1.b], [t1.b], scale=0.7978845608028654)
                P.dve('tensor_scalar', [t1.b], [t1.b], out=t1[:], in0=t1[:], scalar1=1.0, scalar2=0.5, op0=ALU.add, op1=ALU.mult)
                P.dve('tensor_tensor', [t1.b, u.b], [hid.b], out=hid[:], in0=t1[:], in1=u[:], op=ALU.mult)
                if kind == 0:
                    p, s_ = po[0], so[0]
                    P.mm(p[:], w2[:], hid[:], True, True, [w2.b, hid.b], [p.b])
                    P.act(s_[:], p[:], AF.Copy, [p.b], [s_.b])
                    P.dma('sp', KC_[g], s_[:], [s_.b], [bS["KC"]])
                else:
                    for nt in range(2):
                        p, s_ = po[nt], so[nt]
                        P.mm(p[:, 0:128], hid[:, nt * 128:(nt + 1) * 128], w2[:], True, True, [w2.b, hid.b], [p.b])
                        P.act(s_[:, 0:128], p[:, 0:128], AF.Copy, [p.b], [s_.b])
                        P.dma('sp', VC_[g, nt * 128:(nt + 1) * 128, :], s_[:, 0:128], [s_.b], [bS["VC"]])

    if upto < 3:
        return
    with C.phase():
        ar = AttnRes(C)
        kT = C.sb([128, 4096], BF16, "kT")
        kR = C.sb([64, 4096], BF16, "kR")
        qT = C.sb([128, 2048], BF16, "qT")
        qR = C.sb([64, 2048], BF16, "qR")
        V1 = C.sb([128, 32, 129], BF16, "V1")
        kc = C.sb([128, 256], BF16, "kc")
        V1c = C.sb([128, 2, 193], BF16, "V1c")
        amf = C.sb([128, 2, 64], F32, "amf")
        Fb = C.sb([128, 4864], F32, "Fb")
        Fm = C.sb([128, 1024], F32, "Fm")
        Em = C.sb([64, 4096], BF16, "Em")
        gt = C.sb([128, 16, 24], F32, "gt")
        acc = [C.sb([128, 16, 128], F32, "acc") for _ in range(4)]
        imp = C.sb([128, 16, 64], F32, "imp")
        negT = C.sb([64, 2048], BF16, "negT")
        rec = [C.sb([128, 2], F32, "rec") for _ in range(4)]
        vm_t = C.sb([128, 64], F32, "vm_t")
        am_t = C.sb([128, 64], F32, "am_t")
        sc_t = C.sb([128, 64], F32, "sc_t")
        sc2 = C.sb([128, 64], F32, "sc2")
        m8 = C.sb([128, 16], F32, "m8")
        ngb = C.sb([128, 64], BF16, "ngb")
        ob = [C.sb([128, 128], BF16, "ob") for _ in range(2)]
        ptn = C.ps([64, 128], BF16, "ptn")
        P.dma('pool', Em[:], L["Emat"], [], [Em.b])
        P.dma('sp', Fm[:], L["Fmla"], [], [Fm.b])
        P.dma('sp', gt[:], GT.rearrange("(t p) c -> p t c", p=128), [bS["GT"]], [gt.b])
        P.dma('sp', amf[:], L["Amat"].rearrange("(t p) c -> p t c", p=128), [], [amf.b])
        R_all = [kT.b, qT.b, V1.b, Fb.b, kc.b, V1c.b, negT.b, Em.b, kR.b, qR.b, Fm.b]
        ri = [0]
        oi = [0]

        def load_v(src_cols):
            P.dve('memset', [], [V1.b], V1[:, :, 128:129], 1.0)
            P.dma('sp', V1[:, :, 0:128], src_cols.rearrange("(kt p) d -> p kt d", p=128), [bS["VX"], bS["VM"]], [V1.b])
            P.dve('tensor_scalar', [V1.b, ctxv.b], [V1.b], out=V1[:, 0:16, :], in0=V1[:, 0:16, :], scalar1=ctxv[:, 0:1], scalar2=None, op0=ALU.mult)

        def branch_cb(a, gcol, first):
            def cb(qc, j, po):
                qt = qc * 4 + j
                r = rec[ri[0] % 4]
                ri[0] += 1
                P.dve('tensor_scalar', [po.b], [r.b], out=r[:, 0:1], in0=po[:, 128:129], scalar1=1e-30, scalar2=None, op0=ALU.max)
                P.dve('reciprocal', [r.b], [r.b], out=r[:, 0:1], in_=r[:, 0:1])
                if gcol is not None:
                    P.dve('tensor_tensor', [r.b, gt.b], [r.b], out=r[:, 1:2], in0=r[:, 0:1], in1=gt[:, qt, gcol:gcol + 1], op=ALU.mult)
                    cf = r[:, 1:2]
                else:
                    cf = r[:, 0:1]
                if first:
                    P.dve('tensor_scalar', [po.b, r.b], [a.b], out=a[:, qt, :], in0=po[:, 0:128], scalar1=cf, scalar2=None, op0=ALU.mult)
                else:
                    P.dve('scalar_tensor_tensor', [po.b, r.b, a.b], [a.b], out=a[:, qt, :], in0=po[:, 0:128], scalar=cf, in1=a[:, qt, :], op0=ALU.mult, op1=ALU.add)
                return r
            return cb

        def store_head(a, col0):
            for qt in range(16):
                o = ob[oi[0] % 2]
                oi[0] += 1
                P.act(o[:], a[:, qt, :], AF.Copy, [a.b], [o.b])
                P.dma('sp', OALL[qt * 128:(qt + 1) * 128, col0:col0 + 128], o[:], [o.b], [bS["OALL"]])

        for g in range(2):
            P.dma('sp', kc[:], KC_[g], [bS["KC"]], [kc.b])
            P.dve('memset', [], [V1c.b], V1c[:, :, 128:129], 1.0)
            P.dma('sp', V1c[:, :, 0:128], VC_[g].rearrange("(t p) d -> p t d", p=128), [bS["VC"]], [V1c.b])
            P.dve('tensor_copy', [amf.b], [V1c.b], out=V1c[:, :, 129:193], in_=amf[:])
            P.dve('tensor_scalar', [V1c.b, ctxv.b], [V1c.b], out=V1c[:, 0, :], in0=V1c[:, 0, :], scalar1=ctxv[:, 0:1], scalar2=None, op0=ALU.mult)
            for r4 in range(4):
                h = g * 4 + r4
                a = acc[r4]
                P.dma('sp', qT[:], QTn[h], [bS["QTn"]], [qT.b])
                P.dma('sp', Fb[:, 0:4096].rearrange("p (t q) -> p t q", t=2), L["Fcmp"][h].rearrange("t p q -> p t q"), [], [Fb.b])
                base_cb = branch_cb(a, h * 3 + 0, True)

                def cb(qc, j, po, base_cb=base_cb, r4=r4):
                    r = base_cb(qc, j, po)
                    qt = qc * 4 + j
                    if r4 == 0:
                        P.dve('tensor_scalar', [po.b, r.b], [imp.b], out=imp[:, qt, :], in0=po[:, 129:193], scalar1=r[:, 0:1], scalar2=None, op0=ALU.mult)
                    else:
                        P.dve('scalar_tensor_tensor', [po.b, r.b, imp.b], [imp.b], out=imp[:, qt, :], in0=po[:, 129:193], scalar=r[:, 0:1], in1=imp[:, qt, :], op0=ALU.mult, op1=ALU.add)
                for qc in range(4):
                    kts = [dict(lhsT=[kc[:, nt * 128:(nt + 1) * 128]], v=V1c[:, nt, :], bias=Fb[:, nt * 2048 + qc * 512:nt * 2048 + (qc + 1) * 512]) for nt in range(2)]
                    emit_attn(P, ar, 512, 128 ** -0.5, [qT[:, qc * 512:(qc + 1) * 512]], kts, R_all, cb, qc, VW=193)
            for qt in range(16):
                P.dma('sp', vm_t[:], L["vmul"][qt * 128:(qt + 1) * 128, :], [], [vm_t.b])
                P.dma('sp', am_t[:], L["addm"][qt * 128:(qt + 1) * 128, :], [], [am_t.b])
                P.dve('tensor_tensor', [imp.b, vm_t.b], [sc_t.b], out=sc_t[:], in0=imp[:, qt, :], in1=vm_t[:], op=ALU.mult)
                P.dve('tensor_tensor', [sc_t.b, am_t.b], [sc_t.b], out=sc_t[:], in0=sc_t[:], in1=am_t[:], op=ALU.add)
                P.dve('max', [sc_t.b], [m8.b], out=m8[:, 0:8], in_=sc_t[:])
                P.dve('match_replace', [sc_t.b, m8.b], [sc2.b], out=sc2[:], in_to_replace=m8[:, 0:8], in_values=sc_t[:], imm_value=-2.0)
                P.dve('max', [sc2.b], [m8.b], out=m8[:, 8:16], in_=sc2[:])
                P.dve('tensor_scalar', [sc_t.b, m8.b], [sc2.b], out=sc2[:], in0=sc_t[:], scalar1=m8[:, 15:16], scalar2=None, op0=ALU.is_ge)
                P.dve('scalar_tensor_tensor', [sc_t.b, sc2.b], [sc2.b], out=sc2[:], in0=sc_t[:], scalar=-0.5, in1=sc2[:], op0=ALU.is_gt, op1=ALU.mult)
                P.dve('tensor_scalar', [sc2.b], [ngb.b], out=ngb[:], in0=sc2[:], scalar1=-1.0, scalar2=BIGM, op0=ALU.add, op1=ALU.mult)
                P.tr(ptn[:, :], ngb[:, :], ident[:], [ngb.b, ident.b], [ptn.b])
                P.act(negT[:, qt * 128:(qt + 1) * 128], ptn[:, :], AF.Copy, [ptn.b], [negT.b])
            for r4 in range(4):
                h = g * 4 + r4
                a = acc[r4]
                P.dma('sp', qT[:], QTn[h], [bS["QTn"]], [qT.b])
                for br in range(2):
                    P.dma('sp', kT[:], KX[2 + br, g], [bS["KX"]], [kT.b])
                    load_v(VX[br][:, g * 128:(g + 1) * 128])
                    if br == 0:
                        P.dma('sp', Fb[:], L["Fslc"][h], [], [Fb.b])
                    else:
                        P.dma('sp', Fb[:, 0:1408], L["Fwin"][h], [], [Fb.b])
                    cb = branch_cb(a, h * 3 + 1 + br, False)
                    for qc in range(4):
                        q0 = 2048 + qc * 512
                        lo = 0 if br == 0 else (q0 - 512) // 128
                        kts = []
                        for kt in range(lo, (q0 + 512) // 128):
                            o = (q0 - kt * 128) // 128
                            d = dict(lhsT=[kT[:, kt * 128:(kt + 1) * 128]], v=V1[:, kt, :], bias=Fb[:, 128 * (o + 3):128 * (o + 3) + 512])
                            if br == 0:
                                d['extra'] = [(Em[:, kt * 128:(kt + 1) * 128], negT[:, qc * 512:(qc + 1) * 512])]
                            kts.append(d)
                        emit_attn(P, ar, 512, 128 ** -0.5, [qT[:, qc * 512:(qc + 1) * 512]], kts, R_all, cb, qc)
                store_head(a, h * 128)
        P.dma('sp', kR[:], KR, [bS["KR"]], [kR.b])
        for h in range(8):
            a = acc[h % 4]
            P.dma('sp', kT[:], KN[h], [bS["KN"]], [kT.b])
            P.dma('sp', qT[:], QN[h], [bS["QN"]], [qT.b])
            P.dma('sp', qR[:], QR[h], [bS["QR"]], [qR.b])
            load_v(VM[:, h * 128:(h + 1) * 128])
            cb = branch_cb(a, None, True)
            for qc in range(4):
                q0 = 2048 + qc * 512
                kts = []
                for kt in range(0, (q0 + 512) // 128):
                    o = (q0 - kt * 128) // 128
                    kts.append(dict(lhsT=[kT[:, kt * 128:(kt + 1) * 128], kR[:, kt * 128:(kt + 1) * 128]], v=V1[:, kt, :],
                                    bias=(Fm[:, 128 * (o + 3):128 * (o + 3) + 512] if o <= 0 else None)))
                emit_attn(P, ar, 512, 192 ** -0.5, [qT[:, qc * 512:(qc + 1) * 512], qR[:, qc * 512:(qc + 1) * 512]], kts, R_all, cb, qc)
            store_head(a, 1024 + h * 128)

    if upto < 4:
        return
    with C.phase():
        wo = C.sb([128, KC, D], BF16, "wo")
        P.dma('pool', wo[:], L["w_out"].rearrange("(kc p) n -> p kc n", p=128), [], [wo.b])
        ot = [C.sb([128, D], BF16, "ot") for _ in range(2)]
        oT = C.sb([128, KC, TG], BF16, "oT")
        xr = [C.sb([128, D], F32, "xr") for _ in range(2)]
        tmp = [C.sb([128, 512], F32, "tmp") for _ in range(2)]
        ptr = [C.ps([128, 1024], BF16, "ptr") for _ in range(2)]
        py = [C.ps([128, 512], F32, "py") for _ in range(2)]
        it = 0
        for gi in range(4):
            for tt in range(4):
                t0 = gi * TG + tt * 128
                o = ot[tt % 2]
                P.dma('sp', o[:], OALL[t0:t0 + 128, :], [bS["OALL"]], [o.b])
                for half in range(2):
                    pt = ptr[half]
                    for c in range(8):
                        cc = half * 8 + c
                        P.tr(pt[:, c * 128:(c + 1) * 128], o[:, cc * 128:(cc + 1) * 128], ident[:], [o.b, ident.b], [pt.b])
                    P.act(oT[:, half * 8:(half + 1) * 8, tt * 128:(tt + 1) * 128], pt[:].rearrange("p (c t) -> p c t", c=8), AF.Copy, [pt.b], [oT.b])
            for tt in range(4):
                t0 = gi * TG + tt * 128
                x = xr[tt % 2]
                P.dma('sp', x[:], xl[2048 + t0:2048 + t0 + 128, :], [], [x.b])
                for nb in range(4):
                    p, tm = py[it % 2], tmp[it % 2]
                    it += 1
                    sl = slice(nb * 512, (nb + 1) * 512)
                    for kc_ in range(KC):
                        P.mm(p[:], oT[:, kc_, tt * 128:(tt + 1) * 128], wo[:, kc_, sl], kc_ == 0, kc_ == KC - 1, [oT.b, wo.b], [p.b])
                    P.dve('tensor_tensor', [p.b, mods[2].b], [tm.b], out=tm[:], in0=p[:], in1=mods[2][:, sl], op=ALU.mult)
                    P.dve('tensor_tensor', [tm.b, x.b], [x.b], out=x[:, sl], in0=x[:, sl], in1=tm[:], op=ALU.add)
                P.dma('sp', X1A[t0:t0 + 128, :], x[:], [x.b], [bS["X1A"]])

    if upto < 5:
        return
    with C.phase():
        nb_ = norm_bufs(C)

        def loader(g, hT):
            emit_norm_T(C, P, nb_, X1A[g * TG:(g + 1) * TG, :], mods[4], mods[3], ident, hT, xdeps=[bS["X1A"]])
        emit_swiglu(C, P, loader, 4, L["wg"], L["wu"], L["wd"], 44, None, YF, bS["YF"], nwd=1)

    with C.phase():
        xa = [C.sb([128, D], F32, "xa") for _ in range(2)]
        yy = [C.sb([128, D], F32, "yy") for _ in range(2)]
        for t in range(16):
            a, y = xa[t % 2], yy[t % 2]
            P.dma('sp', a[:], X1A[t * 128:(t + 1) * 128, :], [bS["X1A"]], [a.b])
            P.dma('sp', y[:], YF[t * 128:(t + 1) * 128, :], [bS["YF"]], [y.b])
            P.dve('tensor_tensor', [y.b, mods[5].b], [y.b], out=y[:], in0=y[:], in1=mods[5][:], op=ALU.mult)
            P.dve('tensor_tensor', [y.b, a.b], [a.b], out=a[:], in0=a[:], in1=y[:], op=ALU.add)
            P.dma('sp', L["x1"][t * 128:(t + 1) * 128, :], a[:], [a.b], [])


def l0_tables(rel_bias, s):
    rb = np.asarray(rel_bias, np.float32)
    t = {}
    t["Fwin"] = toeplitz_table(rb, range(8), 1408, 384, 1, 511)
    t["Fslc"] = toeplitz_table(rb, range(8), 4864, 384, 1, 10 ** 9)
    k = np.arange(128)[:, None]
    j = np.arange(1024)[None, :]
    t["Fmla"] = np.where(j - k - 384 >= 0, np.float32(0), np.float32(NEG)).astype(np.float32)
    n = (np.arange(2)[:, None, None] * 128 + np.arange(128)[None, :, None])
    tl = 2048 + np.arange(2048)[None, None, :]
    rel = tl - (16 * n + 31)
    ok = (rel >= 0) & (n <= 254)
    bk = t5_bucket_np(rel)
    t["Fcmp"] = np.stack([np.where(ok, rb[bk, h], np.float32(NEG)) for h in range(8)], 0).astype(np.float32)
    q = np.arange(2048)[:, None]
    jb = np.arange(64)[None, :]
    blk_t = (2048 + q) // 64
    first = 0 if s == 1 else 32
    valid = (jb <= blk_t) & (jb >= first)
    forced = valid & ((jb == first) | (jb == blk_t) | (jb == blk_t - 1))
    t["vmul"] = (valid & ~forced).astype(np.float32)
    fval = np.where(jb == first, np.float32(3e9), np.where(jb == blk_t, np.float32(2e9), np.float32(1e9)))
    t["addm"] = np.where(forced, fval, np.where(valid, np.float32(0), np.float32(-1))).astype(np.float32)
    A = np.zeros((256, 64), np.float32)
    for jj in range(64):
        for m in range(4):
            for n2 in range(2):
                i = 4 * jj + m - n2
                if 0 <= i < 256:
                    A[i, jj] += 1
    t["Amat"] = A
    t["Emat"] = (np.arange(4096)[None, :] // 64 == np.arange(64)[:, None]).astype(np.float32)
    inv = (np.float32(10000.0) ** (-np.arange(0, 64, 2, dtype=np.float32) / np.float32(64))).astype(np.float32)
    t["invf"] = np.stack([np.concatenate([inv, inv]), np.concatenate([-np.ones(32, np.float32), np.ones(32, np.float32)])], 1).astype(np.float32)
    t["ctxv"] = np.full((128, 1), float(s), np.float32)
    t["ident"] = np.eye(128, dtype=np.float32)
    return t


def l0_inputs(inp, b, s, shared, mods):
    x = inp['x'][b]
    pos = np.asarray(inp['positions'][b]).astype(np.int32)
    if s == 1:
        xl, pl = x, pos
    else:
        xl = np.concatenate([np.zeros((2048, D), np.float32), x[:2048]], 0)
        pl = np.concatenate([np.zeros(2048, np.int32), pos[:2048]])
    d = dict(shared[s])
    d.update({"xl": np.ascontiguousarray(xl), "pos": np.ascontiguousarray(pl[None]), "modrow": np.ascontiguousarray(mods[0, b][None])})
    return d


def l0_shared(inp):
    f = lambda a: np.ascontiguousarray(np.asarray(a, np.float32))
    base = {"gn_m": f(inp['mix_norm_g'][0][None]), "gn_f": f(inp['ffn_norm_g'][0][None]),
            "w_in": f(inp['ab_w_in'][0]), "w_out": f(inp['ab_w_out'][0]),
            "pos_k": f(inp['nsa_cmp_pos_k'][0]), "pos_v": f(inp['nsa_cmp_pos_v'][0]), "w1_k": f(inp['nsa_cmp_w1_k'][0]), "w1_v": f(inp['nsa_cmp_w1_v'][0]),
            "w2_k": f(inp['nsa_cmp_w2_k'][0]), "w2_v": f(inp['nsa_cmp_w2_v'][0]),
            "qng": f(inp['mla_q_norm_g'][0].reshape(4, 128).T), "kvng": f(inp['mla_kv_norm_g'][0].reshape(4, 128).T),
            "w_uq": f(inp['mla_w_uq'][0]), "w_ukv": f(inp['mla_w_ukv'][0]),
            "wg": f(inp['ffn_w_gate'][0]), "wu": f(inp['ffn_w_up'][0]), "wd": f(inp['ffn_w_down'][0])}
    return [{**base, **l0_tables(inp['rel_bias'], s)} for s in range(2)]


def build_final():
    nc = bass.Bass("TRN2", target_bir_lowering=False)
    x1m = nc.dram_tensor("x1m", [2048, D], F32, kind="ExternalInput").ap()
    parts = nc.dram_tensor("parts", [8, 2048, D], F32, kind="ExternalInput").ap()
    gf = nc.dram_tensor("gf", [1, D], F32, kind="ExternalInput").ap()
    fg = nc.dram_tensor("fg", [1, D], F32, kind="ExternalInput").ap()
    out = nc.dram_tensor("out", [2048, D], F32, kind="ExternalOutput").ap()
    P = Prog(nc)
    C = Ctx(nc, P)
    with C.phase():
        gfb = C.sb([128, D], F32, "gfb")
        fgb = C.sb([128, D], F32, "fgb")
        xb = [C.sb([128, D], F32, "xb") for _ in range(2)]
        acc = [C.sb([128, D], F32, "acc") for _ in range(2)]
        pb = [C.sb([128, D], F32, "pb") for _ in range(3)]
        junk = C.sb([128, D], BF16, "junk")
        st = [C.sb([128, 4], F32, "st") for _ in range(2)]
        P.dma('sp', gfb[:], gf.partition_broadcast(128), [], [gfb.b])
        P.dma('sp', fgb[:], fg.partition_broadcast(128), [], [fgb.b])
        k = 0
        for t in range(16):
            x, a, s_ = xb[t % 2], acc[t % 2], st[t % 2]
            rows = slice(t * 128, (t + 1) * 128)
            P.dma('sp', x[:], x1m[rows, :], [], [x.b])
            P.dma('sp', a[:], parts[0, rows, :], [], [a.b])
            for e in range(1, 8):
                p = pb[k % 3]
                k += 1
                P.dma('sp', p[:], parts[e, rows, :], [], [p.b])
                P.dve('tensor_tensor', [a.b, p.b], [a.b], out=a[:], in0=a[:], in1=p[:], op=ALU.add)
            P.dve('tensor_tensor', [a.b, gfb.b], [a.b], out=a[:], in0=a[:], in1=gfb[:], op=ALU.mult)
            P.dve('tensor_tensor', [a.b, x.b], [x.b], out=x[:], in0=x[:], in1=a[:], op=ALU.add)
            P.act(junk[:], x[:], AF.Square, [x.b], [junk.b, s_.b], accum_out=s_[:, 0:1])
            P.dve('tensor_scalar', [s_.b], [s_.b], out=s_[:, 1:2], in0=s_[:, 0:1], scalar1=1.0 / D, scalar2=EPS, op0=ALU.mult, op1=ALU.add)
            P.act(s_[:, 3:4], s_[:, 1:2], AF.Sqrt, [s_.b], [s_.b])
            P.dve('reciprocal', [s_.b], [s_.b], out=s_[:, 2:3], in_=s_[:, 3:4])
            P.dve('scalar_tensor_tensor', [x.b, s_.b, fgb.b], [x.b], out=x[:], in0=x[:], scalar=s_[:, 2:3], in1=fgb[:], op0=ALU.mult, op1=ALU.mult)
            P.dma('sp', out[rows, :], x[:], [x.b], [])
    P.finish()
    return nc


def _launch(nc, ims):
    return run_bass_kernel_spmd(nc, ims, core_ids=list(range(8))).results


def kernel(**inp):
    f32 = lambda a: np.ascontiguousarray(np.asarray(a, np.float32))
    inp = {k: np.asarray(v) for k, v in inp.items()}
    mods = run_mods(inp)
    shared = l0_shared(inp)
    res = _launch(build_l0(), [l0_inputs(inp, i // 2, i % 2, shared, mods) for i in range(8)])
    x1 = np.empty((4, 4096, D), np.float32)
    for i in range(8):
        x1[i // 2, (i % 2) * 2048:(i % 2 + 1) * 2048] = res[i]["x1"]
    del shared, res
    base = {"gn_m": f32(inp['mix_norm_g'][1][None]),
            "gn_f": f32(inp['ffn_norm_g'][1][None]), "c_w_in": f32(inp['c_w_in'][0]), "c_w_out": f32(inp['c_w_out'][0]),
            "wrT": f32(np.asarray(inp['moe_w_router'][0]).T).reshape(1, -1), "Fd": make_Fd(f32(inp['rel_bias'])),
            "ident": np.eye(128, dtype=np.float32)}
    ims = []
    for i in range(8):
        b, s = i // 2, i % 2
        xl = x1[b] if s == 1 else np.concatenate([np.zeros((2048, D), np.float32), x1[b, :2048]], 0)
        ims.append({**base, "xl": np.ascontiguousarray(xl), "modrow": np.ascontiguousarray(mods[1, b][None]),
                    "ctxv": np.full((128, 1), float(s), np.float32)})
    res = _launch(build_l1mix(), ims)
    x1m = [np.asarray(res[i]["x1m"]) for i in range(8)]
    gfs = [np.asarray(res[i]["gf"]) for i in range(8)]
    hT_all = np.ascontiguousarray(np.concatenate([np.asarray(res[i]["hTf"]) for i in range(8)], axis=1))
    gates = np.concatenate([np.asarray(res[i]["gates"]) for i in range(8)], axis=0)
    del ims, base, res, x1
    ims = [{"hT": hT_all, "gate": np.ascontiguousarray(gates[:, e].reshape(128, 128).T),
            "wg": f32(inp['moe_w_gate'][0, e]), "wu": f32(inp['moe_w_up'][0, e]), "wd": f32(inp['moe_w_down'][0, e])} for e in range(8)]
    res = _launch(build_moe(), ims)
    ys = [np.asarray(res[e]["y"]) for e in range(8)]
    del ims, res
    fg = f32(inp['final_norm_g'][None])
    ims = [{"x1m": x1m[i], "parts": np.ascontiguousarray(np.stack([ys[e][i * 2048:(i + 1) * 2048] for e in range(8)], 0)),
            "gf": gfs[i], "fg": fg} for i in range(8)]
    res = _launch(build_final(), ims)
    out = np.empty((4, 4096, D), np.float32)
    for i in range(8):
        out[i // 2, (i % 2) * 2048:(i % 2 + 1) * 2048] = res[i]["out"]
    return out
```

```python
import numpy as np
from contextlib import ExitStack
import concourse.bass as bass
import concourse.mybir as mybir
from concourse.bass_utils import run_bass_kernel_spmd

F32 = mybir.dt.float32
BF16 = mybir.dt.bfloat16
I32 = mybir.dt.int32
AF = mybir.ActivationFunctionType
ALU = mybir.AluOpType
AX = mybir.AxisListType

ENG = ['pe', 'act', 'dve', 'pool', 'sp']


class Buf:
    __slots__ = ('name', 'w', 'r')

    def __init__(self, name=''):
        self.name = name
        self.w = None
        self.r = {}


class Prog:
    def __init__(self, nc):
        self.nc = nc
        self.ops = {e: [] for e in ENG}
        self.cnt = {}
        self.seen = {e: {} for e in ENG}
        self.pending = {e: {} for e in ENG}
        self.nslots = 12
        self.rr = {e: 0 for e in ENG}
        self.sems = {}
        self.stack = ExitStack()
        self.nblk = 0

    def _need(self, E, waits, k, v):
        if k == ('e', 'pe') and E == 'pe':
            return
        if waits.get(k, 0) < v:
            waits[k] = v

    def op(self, E, fn, reads=(), writes=(), dma=False):
        waits = dict(self.pending[E])
        self.pending[E] = {}
        for b in reads:
            if b.w is not None:
                self._need(E, waits, *b.w)
        for b in writes:
            if b.w is not None:
                self._need(E, waits, *b.w)
            for k, v in b.r.items():
                self._need(E, waits, k, v)
        if dma:
            s = self.rr[E]
            self.rr[E] = (s + 1) % self.nslots
            key = ('d', E, s)
            prev = self.cnt.get(key, 0)
            if prev:
                self._need(E, waits, key, prev)
            val = prev + 16
            inc = 16
        else:
            key = ('e', E)
            val = self.cnt.get(key, 0) + 1
            inc = 1
        self.cnt[key] = val
        w2 = []
        for k, v in waits.items():
            if self.seen[E].get(k, 0) >= v:
                continue
            self.seen[E][k] = v
            w2.append((k, v))
        self.ops[E].append((w2, fn, key, inc))
        for b in reads:
            if b.r.get(key, 0) < val:
                b.r[key] = val
        for b in writes:
            b.w = (key, val)
            b.r = {}

    def barrier(self):
        for E in ENG:
            for k, v in self.cnt.items():
                if self.pending[E].get(k, 0) < v:
                    self.pending[E][k] = v

    def sem(self, k):
        if k not in self.sems:
            self.sems[k] = self.stack.enter_context(self.nc.semaphore("s_" + "_".join(map(str, k))))
        return self.sems[k]

    def flush(self):
        nc = self.nc
        for E in ENG:
            for (w, fn, key, inc) in self.ops[E]:
                if key is not None:
                    self.sem(key)
                for k, v in w:
                    self.sem(k)
        ops = self.ops
        self.ops = {e: [] for e in ENG}
        sem = self.sems

        def run(E):
            def f(e):
                for (w, fn, key, inc) in ops[E]:
                    for (k, v) in w:
                        e.wait_ge(sem[k], v)
                    if fn is not None:
                        fn(e).then_inc(sem[key], inc)
            return f
        with nc.Block() as block:
            block.tensor(run('pe'))
            block.scalar(run('act'))
            block.vector(run('dve'))
            block.gpsimd(run('pool'))
            block.sync(run('sp'))
        self.nblk += 1

    def finish(self):
        self.barrier()
        for E in ENG:
            waits = self.pending[E]
            self.pending[E] = {}
            w2 = [(k, v) for k, v in waits.items() if self.seen[E].get(k, 0) < v]
            self.ops[E].append((w2, None, None, 0))
        self.flush()
        self.stack.close()

    def mm(self, out, lhsT, rhs, start, stop, R, W):
        self.op('pe', lambda e: e.matmul(out, lhsT=lhsT, rhs=rhs, start=start, stop=stop), R, W)

    def tr(self, out, in_, ident, R, W):
        self.op('pe', lambda e: e.transpose(out=out, in_=in_, identity=ident), R, W)

    def dma(self, q, out, in_, R, W):
        self.op(q, lambda e: e.dma_start(out=out, in_=in_), R, W, dma=True)

    def act(self, out, in_, func, R, W, **kw):
        self.op('act', lambda e: e.activation(out=out, in_=in_, func=func, **kw), R, W)

    def dve(self, name, R, W, *a, **kw):
        self.op('dve', lambda e: getattr(e, name)(*a, **kw), R, W)

    def pool(self, name, R, W, *a, **kw):
        self.op('pool', lambda e: getattr(e, name)(*a, **kw), R, W)


class T:
    def __init__(self, h, name):
        self.h = h
        self.b = Buf(name)

    def __getitem__(self, k):
        return self.h[k]


class Ctx:
    def __init__(self, nc, P):
        self.nc = nc
        self.P = P
        self.st = None
        self.n = 0

    def phase(self):
        return _Phase(self)

    def sb(self, shape, dt, name=None):
        self.n += 1
        name = name or "t"
        return T(self.st.enter_context(self.nc.sbuf_tensor(f"{name}_{self.n}", list(shape), dt)), name)

    def ps(self, shape, dt, name=None):
        self.n += 1
        name = name or "p"
        width = 512 if dt == F32 else 1024
        assert len(shape) == 2 and shape[1] <= width
        full = self.st.enter_context(self.nc.psum_tensor(f"{name}_{self.n}", [128, width], dt))
        return T(full[0:shape[0], 0:shape[1]], name)


class _Phase:
    def __init__(self, c):
        self.c = c

    def __enter__(self):
        self.c.P.barrier()
        self.c.st = ExitStack()
        self.c.st.__enter__()
        return self.c

    def __exit__(self, *a):
        if a[0] is None:
            self.c.P.flush()
        self.c.st.__exit__(*a)
        self.c.st = None
        return False


D = 2048
KC = 16
TG = 512
EPS = 1e-6


def rearr_w(ap):
    return ap.rearrange("(kc p) n -> p kc n", p=128)


def emit_norm_T(C, P, bufs, x_dram_rows, A, B, ident, hT, xdeps=(), tile_cb=None):
    xb, hb, junk, st, pst = bufs
    for tt in range(4):
        x = xb[tt % 2]
        h = hb[tt % 2]
        P.dma('sp', x[:], x_dram_rows[tt * 128:(tt + 1) * 128, :], list(xdeps), [x.b])
        P.act(junk[:], x[:], AF.Square, [x.b], [junk.b, st.b], accum_out=st[:, 0:1])
        P.dve('tensor_scalar', [st.b], [st.b], out=st[:, 1:2], in0=st[:, 0:1], scalar1=1.0 / D, scalar2=EPS, op0=ALU.mult, op1=ALU.add)
        P.act(st[:, 3:4], st[:, 1:2], AF.Sqrt, [st.b], [st.b])
        P.dve('reciprocal', [st.b], [st.b], out=st[:, 2:3], in_=st[:, 3:4])
        P.dve('scalar_tensor_tensor', [x.b, st.b, A.b], [x.b], out=x[:], in0=x[:], scalar=st[:, 2:3], in1=A[:], op0=ALU.mult, op1=ALU.mult)
        P.dve('tensor_tensor', [x.b, B.b], [x.b], out=x[:], in0=x[:], in1=B[:], op=ALU.add)
        P.act(h[:], x[:], AF.Copy, [x.b], [h.b])
        if tile_cb is not None:
            tile_cb(tt, x)
        for half in range(2):
            pt = pst[half]
            for c in range(8):
                cc = half * 8 + c
                P.tr(pt[:, c * 128:(c + 1) * 128], h[:, cc * 128:(cc + 1) * 128], ident[:], [h.b, ident.b], [pt.b])
            dst = hT[:, half * 8:(half + 1) * 8, tt * 128:(tt + 1) * 128]
            src = pt[:].rearrange("p (c t) -> p c t", c=8)
            if half == 0:
                P.act(dst, src, AF.Copy, [pt.b], [hT.b])
            else:
                P.dve('tensor_copy', [pt.b], [hT.b], out=dst, in_=src)


def emit_swiglu(C, P, hT_loader, ngroups, wg, wu, wd, NF, gate, y_dram, yb, nwd=2):
    hT = C.sb([128, KC, TG], BF16, "hT")
    aT = C.sb([128, NF, TG], BF16, "aT")
    wgb = [C.sb([128, KC, 256], BF16, "wgb") for _ in range(2)]
    wub = [C.sb([128, KC, 256], BF16, "wub") for _ in range(2)]
    wdb = [C.sb([128, NF, 256], BF16, "wdb") for _ in range(nwd)]
    sg = [C.sb([128, TG], F32, "sg") for _ in range(2)]
    pg = [C.ps([128, TG], F32, "pg") for _ in range(2)]
    pu = [C.ps([128, TG], F32, "pu") for _ in range(2)]
    pd = [C.ps([128, 256], F32, "pd") for _ in range(2)]
    stg = [C.sb([128, 256], F32, "stg") for _ in range(3)]
    it = 0
    io = 0
    for g in range(ngroups):
        hT_loader(g, hT)
        for fb in range(NF // 2):
            a = wgb[fb % 2]
            b = wub[fb % 2]
            P.dma('pool', a[:], rearr_w(wg[:, fb * 256:(fb + 1) * 256]), [], [a.b])
            P.dma('pool', b[:], rearr_w(wu[:, fb * 256:(fb + 1) * 256]), [], [b.b])
            for j in range(2):
                fc = fb * 2 + j
                p1 = pg[it % 2]
                p2 = pu[it % 2]
                s = sg[it % 2]
                it += 1
                for k in range(KC):
                    P.mm(p1[:], a[:, k, j * 128:(j + 1) * 128], hT[:, k, :], k == 0, k == KC - 1, [a.b, hT.b], [p1.b])
                for k in range(KC):
                    P.mm(p2[:], b[:, k, j * 128:(j + 1) * 128], hT[:, k, :], k == 0, k == KC - 1, [b.b, hT.b], [p2.b])
                P.act(s[:], p1[:], AF.Silu, [p1.b], [s.b])
                P.dve('tensor_tensor', [s.b, p2.b], [aT.b], out=aT[:, fc, :], in0=s[:], in1=p2[:], op=ALU.mult)
        for nb in range(8):
            w = wdb[nb % nwd]
            P.dma('pool', w[:], wd[:, nb * 256:(nb + 1) * 256].rearrange("(fc p) n -> p fc n", p=128), [], [w.b])
            for tt in range(4):
                p = pd[io % 2]
                s = stg[io % 3]
                io += 1
                for fc in range(NF):
                    P.mm(p[:], aT[:, fc, tt * 128:(tt + 1) * 128], w[:, fc, :], fc == 0, fc == NF - 1, [aT.b, w.b], [p.b])
                if gate is not None:
                    P.act(s[:], p[:], AF.Copy, [p.b, gate.b], [s.b], scale=gate[:, g * 4 + tt:g * 4 + tt + 1])
                else:
                    P.act(s[:], p[:], AF.Copy, [p.b], [s.b])
                P.dma('sp', y_dram[(g * 4 + tt) * 128:(g * 4 + tt + 1) * 128, nb * 256:(nb + 1) * 256], s[:], [s.b], [yb])


def build_moe(ntok=16384, NF=56):
    nc = bass.Bass("TRN2", target_bir_lowering=False)
    hTd = nc.dram_tensor("hT", [D, ntok], BF16, kind="ExternalInput").ap()
    gated = nc.dram_tensor("gate", [128, ntok // 128], F32, kind="ExternalInput").ap()
    wg = nc.dram_tensor("wg", [D, NF * 128], F32, kind="ExternalInput").ap()
    wu = nc.dram_tensor("wu", [D, NF * 128], F32, kind="ExternalInput").ap()
    wd = nc.dram_tensor("wd", [NF * 128, D], F32, kind="ExternalInput").ap()
    y = nc.dram_tensor("y", [ntok, D], F32, kind="ExternalOutput").ap()
    P = Prog(nc)
    C = Ctx(nc, P)
    yb = Buf('y')
    with C.phase():
        gate = C.sb([128, ntok // 128], F32, "gate")
        P.dma('sp', gate[:], gated, [], [gate.b])

        def loader(g, hT):
            P.dma('sp', hT[:], hTd[:, g * TG:(g + 1) * TG].rearrange("(kc p) t -> p kc t", p=128), [], [hT.b])
        emit_swiglu(C, P, loader, ntok // TG, wg, wu, wd, NF, gate, y, yb)
    P.finish()
    return nc


def emit_mod_row(C, P, modrow, gn_m, gn_f, outs):
    gn = [C.sb([128, D], F32, "gn") for _ in range(2)]
    P.dma('sp', gn[0][:], gn_m.partition_broadcast(128), [], [gn[0].b])
    P.dma('sp', gn[1][:], gn_f.partition_broadcast(128), [], [gn[1].b])
    for part in range(6):
        dst = outs[part]
        P.dma('sp', dst[:], modrow[:, part * D:(part + 1) * D].partition_broadcast(128), [], [dst.b])
        if part in (1, 4):
            g = gn[0] if part == 1 else gn[1]
            P.dve('scalar_tensor_tensor', [dst.b, g.b], [dst.b], out=dst[:], in0=dst[:], scalar=1.0, in1=g[:], op0=ALU.add, op1=ALU.mult)


def build_mods():
    nc = bass.Bass("TRN2", target_bir_lowering=False)
    cT = nc.dram_tensor("cT", [128, KC, 128], F32, kind="ExternalInput").ap()
    aw = nc.dram_tensor("aw", [2, D, 1536], F32, kind="ExternalInput").ap()
    ab = nc.dram_tensor("ab", [2, 1, 1536], F32, kind="ExternalInput").ap()
    mo = nc.dram_tensor("mod", [2, 128, 1536], F32, kind="ExternalOutput").ap()
    P = Prog(nc)
    C = Ctx(nc, P)
    with C.phase():
        cs = C.sb([128, KC, 128], F32, "cs")
        w = [C.sb([128, KC, 512], F32, "w") for _ in range(2)]
        bb = C.sb([128, 1536], F32, "bb")
        o = [C.sb([128, 1536], F32, "o") for _ in range(2)]
        ps = [C.ps([128, 512], F32, "ps") for _ in range(2)]
        P.dma('sp', cs[:], cT, [], [cs.b])
        P.act(cs[:], cs[:], AF.Silu, [cs.b], [cs.b])
        it = 0
        for l in range(2):
            P.dma('sp', bb[:], ab[l].partition_broadcast(128), [], [bb.b])
            for ch in range(3):
                ww, p = w[it % 2], ps[it % 2]
                it += 1
                P.dma('sp', ww[:], rearr_w(aw[l][:, ch * 512:(ch + 1) * 512]), [], [ww.b])
                for k in range(KC):
                    P.mm(p[:], cs[:, k, :], ww[:, k, :], k == 0, k == KC - 1, [cs.b, ww.b], [p.b])
                P.dve('tensor_tensor', [p.b, bb.b], [o[l].b], out=o[l][:, ch * 512:(ch + 1) * 512], in0=p[:], in1=bb[:, ch * 512:(ch + 1) * 512], op=ALU.add)
            P.dma('sp', mo[l], o[l][:], [o[l].b], [])
    P.finish()
    return nc


def run_mods(inp):
    c = np.asarray(inp['c'], np.float32)
    cT = np.zeros((128, KC, 128), np.float32)
    cT[:, :, :4] = c.reshape(4, KC, 128).transpose(2, 1, 0)
    aw = np.asarray(inp['ada_w'], np.float32)
    ab = np.asarray(inp['ada_b'], np.float32)
    ims = [{"cT": cT, "aw": np.ascontiguousarray(aw[:, :, i * 1536:(i + 1) * 1536]),
            "ab": np.ascontiguousarray(ab[:, None, i * 1536:(i + 1) * 1536])} for i in range(8)]
    res = run_bass_kernel_spmd(build_mods(), ims, core_ids=list(range(8))).results
    return np.concatenate([np.asarray(res[i]["mod"])[:, :4] for i in range(8)], axis=2)


class AttnRes:
    def __init__(self, C):
        self.sp = [C.ps([128, 512], F32, "sp") for _ in range(2)]
        self.po = [C.ps([128, 256], F32, "po") for _ in range(4)]
        self.sbt = [C.sb([128, 512], F32, "sbt") for _ in range(2)]
        self.pt = [C.sb([128, 512], BF16, "pt") for _ in range(3)]
        self.i = 0


def emit_attn(P, ar, QW, scale, rhs_list, kts, R, out_cb, qc_id, VW=129):
    nj = QW // 128
    n = len(kts)
    base = ar.i
    ar.i += n

    def scores(ki):
        kt = kts[ki]
        ps = ar.sp[(base + ki) % 2]
        parts = list(zip(kt['lhsT'], rhs_list)) + list(kt.get('extra', []))
        for pi, (l, r) in enumerate(parts):
            P.mm(ps[:, 0:QW], l, r, pi == 0, pi == len(parts) - 1, R, [ps.b])

    def probs(ki):
        kt = kts[ki]
        i = base + ki
        ps = ar.sp[i % 2]
        pt = ar.pt[i % 3]
        if kt.get('bias') is not None:
            sb = ar.sbt[i % 2]
            P.dve('scalar_tensor_tensor', [ps.b] + R, [sb.b], out=sb[:, 0:QW], in0=ps[:, 0:QW], scalar=float(scale), in1=kt['bias'], op0=ALU.mult, op1=ALU.add)
            P.act(pt[:, 0:QW], sb[:, 0:QW], AF.Exp, [sb.b], [pt.b])
        else:
            P.act(pt[:, 0:QW], ps[:, 0:QW], AF.Exp, [ps.b], [pt.b], scale=float(scale))

    def pv(ki):
        pt = ar.pt[(base + ki) % 3]
        for j in range(nj):
            po = ar.po[j]
            P.mm(po[:, 0:VW], pt[:, j * 128:(j + 1) * 128], kts[ki]['v'], ki == 0, ki == n - 1, [pt.b] + R, [po.b])

    scores(0)
    for ki in range(n):
        probs(ki)
        if ki + 1 < n:
            scores(ki + 1)
        pv(ki)
    for j in range(nj):
        out_cb(qc_id, j, ar.po[j])


DIL = (1, 4, 16)


def norm_bufs(C):
    xb = [C.sb([128, D], F32, "xb") for _ in range(2)]
    hb = [C.sb([128, D], BF16, "hb") for _ in range(2)]
    junk = C.sb([128, D], BF16, "junk")
    st = C.sb([128, 4], F32, "st")
    pst = [C.ps([128, 1024], BF16, "pst") for _ in range(2)]
    return (xb, hb, junk, st, pst)


def build_l1mix():
    nc = bass.Bass("TRN2", target_bir_lowering=False)
    dt = nc.dram_tensor
    xl = dt("xl", [4096, D], F32, kind="ExternalInput").ap()
    modrow = dt("modrow", [1, 6 * D], F32, kind="ExternalInput").ap()
    gn_m = dt("gn_m", [1, D], F32, kind="ExternalInput").ap()
    gn_f = dt("gn_f", [1, D], F32, kind="ExternalInput").ap()
    w_in = dt("c_w_in", [D, 9216], F32, kind="ExternalInput").ap()
    w_out = dt("c_w_out", [1024, D], F32, kind="ExternalInput").ap()
    wrT = dt("wrT", [1, 8 * D], F32, kind="ExternalInput").ap()
    Fd = dt("Fd", [24, 128, 1024], F32, kind="ExternalInput").ap()
    ctxvd = dt("ctxv", [128, 1], F32, kind="ExternalInput").ap()
    identd = dt("ident", [128, 128], F32, kind="ExternalInput").ap()
    x1m = dt("x1m", [2048, D], F32, kind="ExternalOutput").ap()
    hTf = dt("hTf", [D, 2048], BF16, kind="ExternalOutput").ap()
    gates_o = dt("gates", [2048, 8], F32, kind="ExternalOutput").ap()
    gf_o = dt("gf", [1, D], F32, kind="ExternalOutput").ap()
    QT = dt("QT", [3, 8, 128, 2048], BF16).ap()
    KT = dt("KT", [3, 8, 128, 4096], BF16).ap()
    Vd = dt("Vd", [3, 4096, 1024], BF16).ap()
    ND = dt("ND", [3, 2048, 8, 129], F32).ap()
    bQT, bKT, bV, bND, bX1 = Buf('QT'), Buf('KT'), Buf('V'), Buf('ND'), Buf('x1m')
    P = Prog(nc)
    C = Ctx(nc, P)
    with ExitStack() as keep:
        def sbp(shape, dtp, name):
            C.n += 1
            return T(keep.enter_context(nc.sbuf_tensor(f"{name}_{C.n}", list(shape), dtp)), name)
        mods = [sbp([128, D], F32, f"mod{i}") for i in range(6)]
        ident = sbp([128, 128], BF16, "ident")
        ctxv = sbp([128, 1], F32, "ctxv")
        with C.phase():
            idf = C.sb([128, 128], F32, "idf")
            P.dma('sp', idf[:], identd, [], [idf.b])
            P.dve('tensor_copy', [idf.b], [ident.b], out=ident[:], in_=idf[:])
            P.dma('sp', ctxv[:], ctxvd, [], [ctxv.b])
            emit_mod_row(C, P, modrow, gn_m, gn_f, mods)
            P.dma('sp', gf_o, mods[5][0:1, :], [mods[5].b], [])
        with C.phase():
            nb_ = norm_bufs(C)
            hT = C.sb([128, KC, TG], BF16, "hT")
            wb = [C.sb([128, KC, 512], BF16, "wb") for _ in range(2)]
            ps = [C.ps([128, 512], F32, "psz") for _ in range(2)]
            stg = [C.sb([128, 512], BF16, "stg") for _ in range(3)]
            it = 0
            for gi in range(8):
                tok0 = gi * TG
                emit_norm_T(C, P, nb_, xl[tok0:tok0 + TG, :], mods[1], mods[0], ident, hT)
                for blk in range(18):
                    g, j, hb2 = blk // 6, (blk % 6) // 2, blk % 2
                    if j == 0 and gi < 4:
                        continue
                    w = wb[blk % 2]
                    P.dma('pool', w[:], rearr_w(w_in[:, blk * 512:(blk + 1) * 512]), [], [w.b])
                    for q4 in range(4):
                        p = ps[it % 2]
                        s = stg[it % 3]
                        it += 1
                        if j < 2:
                            h = hb2 * 4 + q4
                            for k in range(KC):
                                P.mm(p[:], w[:, k, q4 * 128:(q4 + 1) * 128], hT[:, k, :], k == 0, k == KC - 1, [w.b, hT.b], [p.b])
                        else:
                            for k in range(KC):
                                P.mm(p[:], hT[:, k, q4 * 128:(q4 + 1) * 128], w[:, k, :], k == 0, k == KC - 1, [w.b, hT.b], [p.b])
                        if it % 2 == 0:
                            P.act(s[:], p[:], AF.Copy, [p.b], [s.b])
                        else:
                            P.dve('tensor_copy', [p.b], [s.b], out=s[:], in_=p[:])
                        if j == 0:
                            P.dma('sp', QT[g, h, :, tok0 - 2048:tok0 - 2048 + TG], s[:], [s.b], [bQT])
                        elif j == 1:
                            P.dma('sp', KT[g, h, :, tok0:tok0 + TG], s[:], [s.b], [bKT])
                        else:
                            P.dma('sp', Vd[g, tok0 + q4 * 128:tok0 + (q4 + 1) * 128, hb2 * 512:(hb2 + 1) * 512], s[:], [s.b], [bV])
        with C.phase():
            ar = AttnRes(C)
            kT = C.sb([128, 4096], BF16, "kT")
            qT = C.sb([128, 2048], BF16, "qT")
            V1 = C.sb([128, 32, 129], BF16, "V1")
            F = C.sb([128, 1024], F32, "F")
            ostg = [C.sb([128, 129], F32, "ostg") for _ in range(4)]
            oi = [0]
            for g in range(3):
                dil = DIL[g]
                TPR = 32 // dil
                nsub = 4096 // dil
                QW = min(512, nsub // 2)
                for h in range(8):
                    P.dma('sp', kT[:], KT[g, h], [bKT], [kT.b])
                    P.dma('sp', qT[:], QT[g, h], [bQT], [qT.b])
                    P.dma('sp', F[:], Fd[g * 8 + h], [], [F.b])
                    P.dve('memset', [], [V1.b], V1[:, :, 128:129], 1.0)
                    vsrc = Vd[g].rearrange("(kt p dl) c -> dl p kt c", p=128, dl=dil)
                    for r in range(dil):
                        P.dma('sp', V1[:, r * TPR:(r + 1) * TPR, 0:128], vsrc[r][:, :, h * 128:(h + 1) * 128], [bV], [V1.b])
                    v4 = V1[:].rearrange("p (r t) c -> p r t c", r=dil)[:, :, 0:TPR // 2, :]
                    P.dve('tensor_scalar', [V1.b, ctxv.b], [V1.b], out=v4, in0=v4, scalar1=ctxv[:, 0:1], scalar2=None, op0=ALU.mult)
                    nd_dst = ND[g].rearrange("(m dl) h c -> dl m h c", dl=dil)
                    for r in range(dil):
                        for q0 in range(nsub // 2, nsub, QW):
                            c0 = q0 * dil + r - 2048
                            rhs = qT[:, c0:c0 + (QW - 1) * dil + 1:dil]
                            kts = []
                            for kt in range(max(0, q0 // 128 - 1), (q0 + QW) // 128):
                                o = (q0 - kt * 128) // 128
                                k0 = kt * 128 * dil + r
                                kts.append(dict(lhsT=[kT[:, k0:k0 + 127 * dil + 1:dil]], v=V1[:, r * TPR + kt, :],
                                                bias=F[:, 128 * (o + 3):128 * (o + 3) + QW]))

                            def cb(qc, j, po, g=g, h=h, r=r, q0=q0, nsub=nsub, nd_dst=nd_dst):
                                s = ostg[oi[0] % 4]
                                oi[0] += 1
                                P.act(s[:], po[:, 0:129], AF.Copy, [po.b], [s.b])
                                m0 = q0 + j * 128 - nsub // 2
                                P.dma('sp', nd_dst[r][m0:m0 + 128, h, :], s[:], [s.b], [bND])
                            emit_attn(P, ar, QW, 128 ** -0.5, [rhs], kts, [kT.b, qT.b, V1.b, F.b], cb, 0)
        with C.phase():
            wo = C.sb([128, 8, D], BF16, "wo")
            P.dma('pool', wo[:], w_out.rearrange("(kc p) n -> p kc n", p=128), [], [wo.b])
            nd = [C.sb([128, 8, 129], F32, "nd") for _ in range(3)]
            rec = C.sb([128, 8], F32, "rec")
            ob = C.sb([128, 8, 128], BF16, "ob")
            oT = C.sb([128, 8, TG], BF16, "oT")
            xr = [C.sb([128, D], F32, "xr") for _ in range(2)]
            tmp = [C.sb([128, 512], F32, "tmp") for _ in range(2)]
            ptr = C.ps([128, 1024], BF16, "ptr")
            py = [C.ps([128, 512], F32, "py") for _ in range(2)]
            it = 0
            for gi in range(4):
                for tt in range(4):
                    t0 = gi * TG + tt * 128
                    for g in range(3):
                        P.dma('sp', nd[g][:], ND[g, t0:t0 + 128], [bND], [nd[g].b])
                    P.dve('tensor_tensor', [nd[0].b, nd[1].b], [nd[0].b], out=nd[0][:], in0=nd[0][:], in1=nd[1][:], op=ALU.add)
                    P.dve('tensor_tensor', [nd[0].b, nd[2].b], [nd[0].b], out=nd[0][:], in0=nd[0][:], in1=nd[2][:], op=ALU.add)
                    P.dve('tensor_scalar', [nd[0].b], [rec.b], out=rec[:], in0=nd[0][:, :, 128], scalar1=1e-30, scalar2=None, op0=ALU.max)
                    P.dve('reciprocal', [rec.b], [rec.b], out=rec[:], in_=rec[:])
                    P.dve('tensor_tensor', [nd[0].b, rec.b], [ob.b], out=ob[:], in0=nd[0][:, :, 0:128], in1=rec[:].unsqueeze(2).to_broadcast([128, 8, 128]), op=ALU.mult)
                    for c in range(8):
                        P.tr(ptr[:, c * 128:(c + 1) * 128], ob[:, c, :], ident[:], [ob.b, ident.b], [ptr.b])
                    P.act(oT[:, :, tt * 128:(tt + 1) * 128], ptr[:].rearrange("p (c t) -> p c t", c=8), AF.Copy, [ptr.b], [oT.b])
                for tt in range(4):
                    t0 = gi * TG + tt * 128
                    x = xr[tt % 2]
                    P.dma('sp', x[:], xl[2048 + t0:2048 + t0 + 128, :], [], [x.b])
                    for nb in range(4):
                        p = py[it % 2]
                        tm = tmp[it % 2]
                        it += 1
                        sl = slice(nb * 512, (nb + 1) * 512)
                        for kc in range(8):
                            P.mm(p[:], oT[:, kc, tt * 128:(tt + 1) * 128], wo[:, kc, sl], kc == 0, kc == 7, [oT.b, wo.b], [p.b])
                        P.dve('tensor_tensor', [p.b, mods[2].b], [tm.b], out=tm[:], in0=p[:], in1=mods[2][:, sl], op=ALU.mult)
                        P.dve('tensor_tensor', [tm.b, x.b], [x.b], out=x[:, sl], in0=x[:, sl], in1=tm[:], op=ALU.add)
                    P.dma('sp', x1m[t0:t0 + 128, :], x[:], [x.b], [bX1])
        with C.phase():
            nb_ = norm_bufs(C)
            hT = C.sb([128, KC, TG], BF16, "hT")
            wr = C.sb([128, 8, D], F32, "wr")
            P.dma('sp', wr[:].rearrange("p e d -> p (e d)"), wrT.partition_broadcast(128), [], [wr.b])
            rj = C.sb([128, D], F32, "rj")
            rj2 = C.sb([128, D], F32, "rj2")
            lg = [C.sb([128, 8], F32, "lg") for _ in range(2)]
            mx = [C.sb([128, 8], F32, "mx") for _ in range(2)]
            ex = [C.sb([128, 8], F32, "ex") for _ in range(2)]
            sm = [C.sb([128, 2], F32, "sm") for _ in range(2)]
            for gi in range(4):
                def tile_cb(tt, x32, gi=gi):
                    l, m, e, s2 = lg[tt % 2], mx[tt % 2], ex[tt % 2], sm[tt % 2]
                    for ee in range(8):
                        P.dve('tensor_tensor', [x32.b, wr.b], [rj.b], out=rj[:], in0=x32[:], in1=wr[:, ee, :], op=ALU.mult)
                        P.act(rj2[:], rj[:], AF.Copy, [rj.b], [rj2.b, l.b], accum_out=l[:, ee:ee + 1])
                    P.dve('max', [l.b], [m.b], out=m[:], in_=l[:])
                    P.dve('tensor_scalar', [m.b], [s2.b], out=s2[:, 0:1], in0=m[:, 0:1], scalar1=-1.0, scalar2=None, op0=ALU.mult)
                    P.act(e[:], l[:], AF.Exp, [l.b, s2.b], [e.b], bias=s2[:, 0:1], scale=1.0)
                    P.dve('scalar_tensor_tensor', [l.b, m.b, e.b], [e.b], out=e[:], in0=l[:], scalar=m[:, 1:2], in1=e[:], op0=ALU.is_ge, op1=ALU.mult)
                    P.dve('tensor_reduce', [e.b], [s2.b], out=s2[:, 1:2], in_=e[:], axis=AX.X, op=ALU.add)
                    P.dve('reciprocal', [s2.b], [s2.b], out=s2[:, 1:2], in_=s2[:, 1:2])
                    P.dve('tensor_scalar', [e.b, s2.b], [e.b], out=e[:], in0=e[:], scalar1=s2[:, 1:2], scalar2=None, op0=ALU.mult)
                    t0 = gi * TG + tt * 128
                    P.dma('sp', gates_o[t0:t0 + 128, :], e[:], [e.b], [])
                emit_norm_T(C, P, nb_, x1m[gi * TG:(gi + 1) * TG, :], mods[4], mods[3], ident, hT, xdeps=[bX1], tile_cb=tile_cb)
                P.dma('sp', hTf[:, gi * TG:(gi + 1) * TG].rearrange("(kc p) t -> p kc t", p=128), hT[:], [hT.b], [])
        P.finish()
    return nc


NEG = -1e30


def t5_bucket_np(dist):
    n = np.maximum(dist, 0)
    nf = np.maximum(n, 1).astype(np.float32)
    large = 16 + (np.log(nf / np.float32(16)) / np.float32(np.log(2048 / 16)) * np.float32(16)).astype(np.int32)
    large = np.minimum(large, 31)
    return np.where(n < 16, n, large)


def toeplitz_table(rel_bias, heads, width, shift, dist_scale, max_back):
    k = np.arange(128)[:, None]
    j = np.arange(width)[None, :]
    d = j - k - shift
    ok = (d >= 0) & (d <= max_back)
    bk = t5_bucket_np(d * dist_scale)
    out = np.empty((len(heads), 128, width), np.float32)
    for i, h in enumerate(heads):
        out[i] = np.where(ok, rel_bias[bk, h], np.float32(NEG))
    return out


def make_Fd(rel_bias):
    return np.concatenate([toeplitz_table(rel_bias, range(8), 1024, 384, dil, 128) for dil in DIL], 0)


BIGM = 30000.0
PI = float(np.pi)


def build_l0(debug=False, upto=5):
    nc = bass.Bass("TRN2", target_bir_lowering=False)

    def din(name, shape, dtp=F32):
        return nc.dram_tensor(name, list(shape), dtp, kind="ExternalInput").ap()

    def dscr(name, shape, dtp):
        return nc.dram_tensor(name, list(shape), dtp, kind="ExternalOutput" if debug else "Internal").ap()
    xl = din("xl", [4096, D])
    modrow = din("modrow", [1, 6 * D])
    gn_m = din("gn_m", [1, D])
    gn_f = din("gn_f", [1, D])
    w_in = din("w_in", [D, 3672])
    w_out = din("w_out", [D, D])
    pos_kv = [din("pos_k", [32, 128]), din("pos_v", [32, 128])]
    w1_kv = [din("w1_k", [4096, 128]), din("w1_v", [4096, 128])]
    w2_kv = [din("w2_k", [128, 128]), din("w2_v", [128, 128])]
    qng = din("qng", [128, 4])
    kvng = din("kvng", [128, 4])
    w_uq = din("w_uq", [512, 1536])
    w_ukv = din("w_ukv", [512, 2048])
    if upto >= 5:
        wg = din("wg", [D, 5632])
        wu = din("wu", [D, 5632])
        wd = din("wd", [5632, D])
    posd = din("pos", [1, 4096], I32)
    invf = din("invf", [64, 2])
    Fwin = din("Fwin", [8, 128, 1408])
    Fslc = din("Fslc", [8, 128, 4864])
    Fmla = din("Fmla", [128, 1024])
    Fcmp = din("Fcmp", [8, 2, 128, 2048])
    vmul = din("vmul", [2048, 64])
    addm = din("addm", [2048, 64])
    Amat = din("Amat", [256, 64])
    Emat = din("Emat", [64, 4096])
    ctxvd = din("ctxv", [128, 1])
    identd = din("ident", [128, 128])
    x1 = nc.dram_tensor("x1", [2048, D], F32, kind="ExternalOutput").ap()
    QTn = dscr("QTn", [8, 128, 2048], BF16)
    KX = dscr("KX", [4, 2, 128, 4096], BF16)
    VX = dscr("VX", [2, 4096, 256], BF16)
    GT = dscr("GT", [2048, 24], F32)
    QN = dscr("QN", [8, 128, 2048], BF16)
    QR = dscr("QR", [8, 64, 2048], BF16)
    KN = dscr("KN", [8, 128, 4096], BF16)
    KR = dscr("KR", [64, 4096], BF16)
    VM = dscr("VM", [4096, 1024], BF16)
    KC_ = dscr("KCc", [2, 128, 256], BF16)
    VC_ = dscr("VCc", [2, 256, 128], BF16)
    OALL = dscr("OALL", [2048, D], BF16)
    X1A = dscr("X1A", [2048, D], F32)
    YF = dscr("YF", [2048, D], F32)
    bS = {k: Buf(k) for k in ("QTn", "KX", "VX", "GT", "QN", "QR", "KN", "KR", "VM", "KC", "VC", "OALL", "X1A", "YF")}
    P = Prog(nc)
    C = Ctx(nc, P)
    with ExitStack() as keep:
        def sbp(shape, dtp, name):
            C.n += 1
            return T(keep.enter_context(nc.sbuf_tensor(f"{name}_{C.n}", list(shape), dtp)), name)
        mods = [sbp([128, D], F32, f"mod{i}") for i in range(6)]
        ident = sbp([128, 128], BF16, "ident")
        identf = sbp([128, 128], F32, "identf")
        ctxv = sbp([128, 1], F32, "ctxv")
        ones = sbp([128, 128], F32, "ones")
        with C.phase():
            P.dma('sp', identf[:], identd, [], [identf.b])
            P.dve('tensor_copy', [identf.b], [ident.b], out=ident[:], in_=identf[:])
            P.dma('sp', ctxv[:], ctxvd, [], [ctxv.b])
            P.dve('memset', [], [ones.b], ones[:], 1.0)
            emit_mod_row(C, P, modrow, gn_m, gn_f, mods)

        with C.phase():
            nb_ = norm_bufs(C)
            hT = C.sb([128, KC, TG], BF16, "hT")
            wb = [C.sb([128, KC, 512], BF16, "wb") for _ in range(2)]
            ps = [C.ps([128, 512], F32, "psz") for _ in range(2)]
            pq = C.ps([128, 512], F32, "pq")
            pr = [C.ps([128, 512], F32, "pr") for _ in range(2)]
            stg = [C.sb([128, 512], BF16, "stg") for _ in range(3)]
            gst = [C.sb([128, 24], F32, "gst") for _ in range(2)]
            wuq = C.sb([128, 4, 1536], BF16, "wuq")
            wuqs = C.sb([128, 4, 8, 64], BF16, "wuqs")
            wkn = C.sb([128, 4, 8, 128], BF16, "wkn")
            wv = C.sb([128, 4, 8, 128], BF16, "wv")
            wpe = C.sb([128, KC, 128], BF16, "wpe")
            gq = C.sb([128, 4], F32, "gq")
            gkv = C.sb([128, 4], F32, "gkv")
            ivf = C.sb([64, 2], F32, "ivf")
            _lf = C.sb([128, 4, TG], F32, "latf")
            latf = [_lf, _lf]
            sq = C.sb([128, 4, TG], F32, "sq")
            latb = [C.sb([128, 4, TG], BF16, "latb") for _ in range(2)]
            rbc = [C.sb([128, TG], F32, "rbc") for _ in range(2)]
            rtm = C.sb([128, 4], F32, "rtm")
            posi = C.sb([128, TG], I32, "posi")
            ang = C.sb([64, TG], F32, "ang")
            CC = C.sb([64, TG], F32, "CC")
            SS = C.sb([64, TG], F32, "SS")
            rA = C.sb([64, TG], F32, "rA")
            rB = C.sb([64, TG], F32, "rB")
            P.dma('pool', wuq[:], w_uq.rearrange("(kc p) n -> p kc n", p=128), [], [wuq.b])
            uq4 = w_uq.rearrange("(kc p) (h c) -> p kc h c", p=128, c=192)
            ukv5 = w_ukv.rearrange("(kc p) (h two d) -> p kc h two d", p=128, two=2, d=128)
            for c4 in range(4):
                P.dma('pool', wuqs[:, c4, :, 0:32], uq4[:, c4, :, 160:192], [], [wuqs.b])
                P.dma('pool', wuqs[:, c4, :, 32:64], uq4[:, c4, :, 128:160], [], [wuqs.b])
                P.dma('pool', wkn[:, c4], ukv5[:, c4, :, 0, :], [], [wkn.b])
                P.dma('pool', wv[:, c4], ukv5[:, c4, :, 1, :], [], [wv.b])
            P.dma('pool', wpe[:, :, 0:64], rearr_w(w_in[:, 3608:3672]), [], [wpe.b])
            P.dma('pool', wpe[:, :, 64:96], rearr_w(w_in[:, 3640:3672]), [], [wpe.b])
            P.dma('pool', wpe[:, :, 96:128], rearr_w(w_in[:, 3608:3640]), [], [wpe.b])
            P.dma('sp', gq[:], qng, [], [gq.b])
            P.dma('sp', gkv[:], kvng, [], [gkv.b])
            P.dma('sp', ivf[:], invf, [], [ivf.b])
            it = 0

            def evac(p, s, M=128, W=TG):
                nonlocal it
                it += 1
                if it % 2 == 0:
                    P.act(s[0:M, 0:W], p[0:M, 0:W], AF.Copy, [p.b], [s.b])
                else:
                    P.dve('tensor_copy', [p.b], [s.b], out=s[0:M, 0:W], in_=p[0:M, 0:W])

            for gi in range(8):
                tok0 = gi * TG
                own = gi >= 4
                emit_norm_T(C, P, nb_, xl[tok0:tok0 + TG, :], mods[1], mods[0], ident, hT)
                P.dma('sp', posi[:], posd[:, tok0:tok0 + TG].partition_broadcast(128), [], [posi.b])
                P.dve('tensor_copy', [posi.b], [ang.b], out=ang[:], in_=posi[0:64, :])
                P.dve('tensor_scalar', [ang.b, ivf.b], [ang.b], out=ang[:], in0=ang[:], scalar1=ivf[:, 0:1], scalar2=None, op0=ALU.mult)
                for dst, shift in ((SS, 0.0), (CC, 0.5 * PI)):
                    P.dve('tensor_scalar', [ang.b], [dst.b], out=dst[:], in0=ang[:], scalar1=shift, scalar2=1.0 / (2 * PI), op0=ALU.add, op1=ALU.mult)
                    P.dve('tensor_copy', [dst.b], [posi.b], out=posi[0:64, :], in_=dst[:])
                    P.dve('tensor_copy', [posi.b], [rA.b], out=rA[:], in_=posi[0:64, :])
                    P.dve('tensor_scalar', [ang.b], [dst.b], out=dst[:], in0=ang[:], scalar1=shift, scalar2=None, op0=ALU.add)
                    P.dve('scalar_tensor_tensor', [rA.b, dst.b], [dst.b], out=dst[:], in0=rA[:], scalar=-2 * PI, in1=dst[:], op0=ALU.mult, op1=ALU.add)
                    P.dve('tensor_scalar', [dst.b], [dst.b], out=dst[:], in0=dst[:], scalar1=PI, scalar2=-PI, op0=ALU.min, op1=ALU.max)
                    P.act(dst[:], dst[:], AF.Sin, [dst.b], [dst.b])
                P.dve('tensor_scalar', [SS.b, ivf.b], [SS.b], out=SS[:], in0=SS[:], scalar1=ivf[:, 1:2], scalar2=None, op0=ALU.mult)
                for blk in range(5):
                    c0 = blk * 512
                    ncols = min(512, 3672 - c0)
                    if blk < 2 and not own:
                        continue
                    w = wb[blk % 2]
                    P.dma('pool', w[:, :, 0:ncols], rearr_w(w_in[:, c0:c0 + ncols]), [], [w.b])

                    def fm(m0, M, p):
                        for k in range(KC):
                            P.mm(p[0:M, :], w[:, k, m0:m0 + M], hT[:, k, :], k == 0, k == KC - 1, [w.b, hT.b], [p.b])
                    if blk < 2:
                        for q4 in range(4):
                            p, s = ps[it % 2], stg[it % 3]
                            fm(q4 * 128, 128, p)
                            evac(p, s)
                            P.dma('sp', QTn[blk * 4 + q4, :, tok0 - 2048:tok0 - 2048 + TG], s[:], [s.b], [bS["QTn"]])
                    elif blk == 2:
                        for q4 in range(4):
                            p, s = ps[it % 2], stg[it % 3]
                            fm(q4 * 128, 128, p)
                            evac(p, s)
                            P.dma('sp', KX[q4 // 2, q4 % 2, :, tok0:tok0 + TG], s[:], [s.b], [bS["KX"]])
                    elif blk in (3, 4):
                        kind = blk - 3
                        for g in range(2):
                            p, s = ps[it % 2], stg[it % 3]
                            fm(g * 128, 128, p)
                            evac(p, s)
                            P.dma('sp', KX[2 + kind, g, :, tok0:tok0 + TG], s[:], [s.b], [bS["KX"]])
                        for tt in range(4):
                            p, s = ps[it % 2], stg[it % 3]
                            for k in range(KC):
                                P.mm(p[:, 0:256], hT[:, k, tt * 128:(tt + 1) * 128], w[:, k, 256:512], k == 0, k == KC - 1, [w.b, hT.b], [p.b])
                            evac(p, s, 128, 256)
                            P.dma('sp', VX[kind, tok0 + tt * 128:tok0 + (tt + 1) * 128, :], s[:, 0:256], [s.b], [bS["VX"]])
                if own:
                    w = wb[0]
                    P.dma('pool', w[:, :, 0:24], rearr_w(w_in[:, 2560:2584]), [], [w.b])
                    for tt in range(4):
                        p, gs = ps[it % 2], gst[tt % 2]
                        it += 1
                        for k in range(KC):
                            P.mm(p[:, 0:24], hT[:, k, tt * 128:(tt + 1) * 128], w[:, k, 0:24], k == 0, k == KC - 1, [w.b, hT.b], [p.b])
                        P.act(gs[:], p[:, 0:24], AF.Sigmoid, [p.b], [gs.b])
                        t0 = tok0 - 2048 + tt * 128
                        P.dma('sp', GT[t0:t0 + 128, :], gs[:], [gs.b], [bS["GT"]])
                for li in range(2):
                    if li == 0 and not own:
                        continue
                    w = wb[1 - li]
                    cb0 = 2584 + li * 512
                    P.dma('pool', w[:], rearr_w(w_in[:, cb0:cb0 + 512]), [], [w.b])
                    lf, lb, gg, rb_ = latf[li], latb[li], (gq if li == 0 else gkv), rbc[li]
                    for c4 in range(4):
                        p = ps[it % 2]
                        it += 1
                        for k in range(KC):
                            P.mm(p[:], w[:, k, c4 * 128:(c4 + 1) * 128], hT[:, k, :], k == 0, k == KC - 1, [w.b, hT.b], [p.b])
                        P.act(lf[:, c4, :], p[:], AF.Copy, [p.b], [lf.b])
                    P.act(sq[:], lf[:], AF.Square, [lf.b], [sq.b])
                    for c4 in range(4):
                        P.mm(pq[:], ones[:], sq[:, c4, :], c4 == 0, c4 == 3, [ones.b, sq.b], [pq.b])
                    P.dve('tensor_scalar', [pq.b], [rb_.b], out=rb_[:], in0=pq[:], scalar1=1.0 / 512, scalar2=EPS, op0=ALU.mult, op1=ALU.add)
                    P.act(rb_[:], rb_[:], AF.Sqrt, [rb_.b], [rb_.b])
                    P.dve('reciprocal', [rb_.b], [rb_.b], out=rb_[:], in_=rb_[:])
                    for c4 in range(4):
                        P.dve('tensor_scalar', [lf.b, gg.b], [lb.b], out=lb[:, c4, :], in0=lf[:, c4, :], scalar1=gg[:, c4:c4 + 1], scalar2=None, op0=ALU.mult)
                    if li == 1:
                        for tt in range(4):
                            p = ps[it % 2]
                            it += 1
                            for c4 in range(4):
                                P.mm(p[:, 0:8], sq[:, c4, tt * 128:(tt + 1) * 128], ones[:, 0:8], c4 == 0, c4 == 3, [sq.b, ones.b], [p.b])
                            P.dve('tensor_scalar', [p.b], [rtm.b], out=rtm[:, tt:tt + 1], in0=p[:, 0:1], scalar1=1.0 / 512, scalar2=EPS, op0=ALU.mult, op1=ALU.add)
                        P.act(rtm[:], rtm[:], AF.Sqrt, [rtm.b], [rtm.b])
                        P.dve('reciprocal', [rtm.b], [rtm.b], out=rtm[:], in_=rtm[:])

                def rope(pa, pb, mul, s):
                    P.dve('tensor_tensor', [pa.b, CC.b], [rA.b], out=rA[:], in0=pa[0:64, :], in1=CC[:], op=ALU.mult)
                    P.dve('tensor_tensor', [pb.b, SS.b], [rB.b], out=rB[:], in0=pb[0:64, :], in1=SS[:], op=ALU.mult)
                    if mul is None:
                        P.dve('tensor_tensor', [rA.b, rB.b], [s.b], out=s[0:64, :], in0=rA[:], in1=rB[:], op=ALU.add)
                    else:
                        P.dve('tensor_tensor', [rA.b, rB.b], [rA.b], out=rA[:], in0=rA[:], in1=rB[:], op=ALU.add)
                        P.dve('tensor_tensor', [rA.b, mul.b], [s.b], out=s[0:64, :], in0=rA[:], in1=mul[0:64, :], op=ALU.mult)
                for k in range(KC):
                    P.mm(pr[0][0:64, :], wpe[:, k, 0:64], hT[:, k, :], k == 0, k == KC - 1, [wpe.b, hT.b], [pr[0].b])
                for k in range(KC):
                    P.mm(pr[1][0:64, :], wpe[:, k, 64:128], hT[:, k, :], k == 0, k == KC - 1, [wpe.b, hT.b], [pr[1].b])
                s = stg[it % 3]
                it += 1
                rope(pr[0], pr[1], None, s)
                P.dma('sp', KR[:, tok0:tok0 + TG], s[0:64, :], [s.b], [bS["KR"]])
                if own:
                    lb = latb[0]
                    for h in range(8):
                        p, s = ps[it % 2], stg[it % 3]
                        it += 1
                        for c4 in range(4):
                            P.mm(p[:], wuq[:, c4, h * 192:h * 192 + 128], lb[:, c4, :], c4 == 0, c4 == 3, [wuq.b, lb.b], [p.b])
                        P.dve('tensor_tensor', [p.b, rbc[0].b], [s.b], out=s[:], in0=p[:], in1=rbc[0][:], op=ALU.mult)
                        P.dma('sp', QN[h, :, tok0 - 2048:tok0 - 2048 + TG], s[:], [s.b], [bS["QN"]])
                        for c4 in range(4):
                            P.mm(pr[0][0:64, :], wuq[:, c4, h * 192 + 128:h * 192 + 192], lb[:, c4, :], c4 == 0, c4 == 3, [wuq.b, lb.b], [pr[0].b])
                        for c4 in range(4):
                            P.mm(pr[1][0:64, :], wuqs[:, c4, h, :], lb[:, c4, :], c4 == 0, c4 == 3, [wuqs.b, lb.b], [pr[1].b])
                        s = stg[it % 3]
                        it += 1
                        rope(pr[0], pr[1], rbc[0], s)
                        P.dma('sp', QR[h, :, tok0 - 2048:tok0 - 2048 + TG], s[0:64, :], [s.b], [bS["QR"]])
                lb = latb[1]
                for h in range(8):
                    p, s = ps[it % 2], stg[it % 3]
                    it += 1
                    for c4 in range(4):
                        P.mm(p[:], wkn[:, c4, h, :], lb[:, c4, :], c4 == 0, c4 == 3, [wkn.b, lb.b], [p.b])
                    P.dve('tensor_tensor', [p.b, rbc[1].b], [s.b], out=s[:], in0=p[:], in1=rbc[1][:], op=ALU.mult)
                    P.dma('sp', KN[h, :, tok0:tok0 + TG], s[:], [s.b], [bS["KN"]])
                for tt in range(4):
                    for hb2 in range(2):
                        p, s = ps[it % 2], stg[it % 3]
                        it += 1
                        for c4 in range(4):
                            P.mm(p[:], lb[:, c4, tt * 128:(tt + 1) * 128], wv[:, c4, hb2 * 4:(hb2 + 1) * 4, :].rearrange("p h d -> p (h d)"), c4 == 0, c4 == 3, [wv.b, lb.b], [p.b])
                        P.act(s[:], p[:], AF.Copy, [p.b, rtm.b], [s.b], scale=rtm[:, tt:tt + 1])
                        P.dma('sp', VM[tok0 + tt * 128:tok0 + (tt + 1) * 128, hb2 * 512:(hb2 + 1) * 512], s[:], [s.b], [bS["VM"]])
        build_l0_rest(nc, C, P, locals())
        P.finish()
    return nc


def build_l0_rest(nc, C, P, L):
    KX, VX, GT, QTn, QN, QR, KN, KR, VM = (L[k] for k in ("KX", "VX", "GT", "QTn", "QN", "QR", "KN", "KR", "VM"))
    KC_, VC_, OALL, X1A, YF, bS = (L[k] for k in ("KC_", "VC_", "OALL", "X1A", "YF", "bS"))
    ident, identf, ctxv, mods, xl = L["ident"], L["identf"], L["ctxv"], L["mods"], L["xl"]
    upto = L["upto"]
    if upto < 2:
        return
    with C.phase():
        kT = C.sb([128, 4096], BF16, "kT")
        w1 = C.sb([128, 32, 128], BF16, "w1")
        w2 = C.sb([128, 128], BF16, "w2")
        posq = C.sb([32, 128], F32, "posq")
        posb = C.sb([128, 32, 8], BF16, "posb")
        bia = C.sb([128, 1], F32, "bia")
        u = C.sb([128, 256], F32, "u")
        t1 = C.sb([128, 256], F32, "t1")
        hid = C.sb([128, 256], BF16, "hid")
        so = [C.sb([128, 256], BF16, "so") for _ in range(2)]
        ph = C.ps([128, 256], F32, "ph")
        pb = C.ps([128, 64], F32, "pb")
        po = [C.ps([128, 256], F32, "po") for _ in range(2)]
        for kind in range(2):
            P.dma('pool', w1[:], L["w1_kv"][kind].rearrange("(l d) e -> d l e", d=128), [], [w1.b])
            P.dma('pool', w2[:], L["w2_kv"][kind], [], [w2.b])
            P.dma('sp', posq[:], L["pos_kv"][kind], [], [posq.b])
            P.tr(pb[:, 0:32], posq[:, :], identf[0:32, 0:32], [posq.b, identf.b], [pb.b])
            P.dve('tensor_copy', [pb.b], [posb.b], out=posb[:], in_=pb[:, 0:32].unsqueeze(2).to_broadcast([128, 32, 8]))
            for l in range(32):
                P.mm(pb[:, 32:40], w1[:, l, :], posb[:, l, :], l == 0, l == 31, [w1.b, posb.b], [pb.b])
            P.act(bia[:], pb[:, 32:33], AF.Copy, [pb.b], [bia.b])
            for g in range(2):
                P.dma('sp', kT[:], KX[kind, g], [bS["KX"]], [kT.b])
                for l in range(32):
                    P.mm(ph[:, 0:255], w1[:, l, :], kT[:, l:l + 16 * 254 + 1:16], l == 0, l == 31, [w1.b, kT.b], [ph.b])
                P.dve('memset', [], [u.b], u[:], 0.0)
                P.dve('tensor_scalar', [ph.b, bia.b], [u.b], out=u[:, 0:255], in0=ph[:, 0:255], scalar1=bia[:, 0:1], scalar2=None, op0=ALU.add)
                P.dve('tensor_tensor', [u.b], [t1.b], out=t1[:], in0=u[:], in1=u[:], op=ALU.mult)
                P.dve('tensor_scalar', [t1.b], [t1.b], out=t1[:], in0=t1[:], scalar1=0.044715, scalar2=1.0, op0=ALU.mult, op1=ALU.add)
                P.dve('tensor_tensor', [t1.b, u.b], [t1.b], out=t1[:], in0=t1[:], in1=u[:], op=ALU.mult)
                P.act(t1[:], t1[:], AF.Tanh, [t1.b], [t1.b], scale=0.7978845608028654)
                P.dve('tensor_scalar', [t1.b], [t1.b], out=t1[:], in0=t1[:], scalar1=1.0, scalar2=0.5, op0=ALU.add, op1=ALU.mult)
                P.dve('tensor_tensor', [t1.b, u.b], [hid.b], out=hid[:], in0=t1[:], in1=u[:], op=ALU.mult)
                if kind == 0:
                    p, s_ = po[0], so[0]
                    P.mm(p[:], w2[:], hid[:], True, True, [w2.b, hid.b], [p.b])
                    P.act(s_[:], p[:], AF.Copy, [p.b], [s_.b])
                    P.dma('sp', KC_[g], s_[:], [s_.b], [bS["KC"]])
                else:
                    for nt in range(2):
                        p, s_ = po[nt], so[nt]
                        P.mm(p[:, 0:128], hid[:, nt * 128:(nt + 1) * 128], w2[:], True, True, [w2.b, hid.b], [p.b])
                        P.act(s_[:, 0:128], p[:, 0:128], AF.Copy, [p.b], [s_.b])
                        P.dma('sp', VC_[g, nt * 128:(nt + 1) * 128, :], s_[:, 0:128], [s_.b], [bS["VC"]])

    if upto < 3:
        return
    with C.phase():
        ar = AttnRes(C)
        kT = C.sb([128, 4096], BF16, "kT")
        kR = C.sb([64, 4096], BF16, "kR")
        qT = C.sb([128, 2048], BF16, "qT")
        qR = C.sb([64, 2048], BF16, "qR")
        V1 = C.sb([128, 32, 129], BF16, "V1")
        kc = C.sb([128, 256], BF16, "kc")
        V1c = C.sb([128, 2, 193], BF16, "V1c")
        amf = C.sb([128, 2, 64], F32, "amf")
        Fb = C.sb([128, 4864], F32, "Fb")
        Fm = C.sb([128, 1024], F32, "Fm")
        Em = C.sb([64, 4096], BF16, "Em")
        gt = C.sb([128, 16, 24], F32, "gt")
        acc = [C.sb([128, 16, 128], F32, "acc") for _ in range(4)]
        imp = C.sb([128, 16, 64], F32, "imp")
        negT = C.sb([64, 2048], BF16, "negT")
        rec = [C.sb([128, 2], F32, "rec") for _ in range(4)]
        vm_t = C.sb([128, 64], F32, "vm_t")
        am_t = C.sb([128, 64], F32, "am_t")
        sc_t = C.sb([128, 64], F32, "sc_t")
        sc2 = C.sb([128, 64], F32, "sc2")
        m8 = C.sb([128, 16], F32, "m8")
        ngb = C.sb([128, 64], BF16, "ngb")
        ob = [C.sb([128, 128], BF16, "ob") for _ in range(2)]
        ptn = C.ps([64, 128], BF16, "ptn")
        P.dma('pool', Em[:], L["Emat"], [], [Em.b])
        P.dma('sp', Fm[:], L["Fmla"], [], [Fm.b])
        P.dma('sp', gt[:], GT.rearrange("(t p) c -> p t c", p=128), [bS["GT"]], [gt.b])
        P.dma('sp', amf[:], L["Amat"].rearrange("(t p) c -> p t c", p=128), [], [amf.b])
        R_all = [kT.b, qT.b, V1.b, Fb.b, kc.b, V1c.b, negT.b, Em.b, kR.b, qR.b, Fm.b]
        ri = [0]
        oi = [0]

        def load_v(src_cols):
            P.dve('memset', [], [V1.b], V1[:, :, 128:129], 1.0)
            P.dma('sp', V1[:, :, 0:128], src_cols.rearrange("(kt p) d -> p kt d", p=128), [bS["VX"], bS["VM"]], [V1.b])
            P.dve('tensor_scalar', [V1.b, ctxv.b], [V1.b], out=V1[:, 0:16, :], in0=V1[:, 0:16, :], scalar1=ctxv[:, 0:1], scalar2=None, op0=ALU.mult)

        def branch_cb(a, gcol, first):
            def cb(qc, j, po):
                qt = qc * 4 + j
                r = rec[ri[0] % 4]
                ri[0] += 1
                P.dve('tensor_scalar', [po.b], [r.b], out=r[:, 0:1], in0=po[:, 128:129], scalar1=1e-30, scalar2=None, op0=ALU.max)
                P.dve('reciprocal', [r.b], [r.b], out=r[:, 0:1], in_=r[:, 0:1])
                if gcol is not None:
                    P.dve('tensor_tensor', [r.b, gt.b], [r.b], out=r[:, 1:2], in0=r[:, 0:1], in1=gt[:, qt, gcol:gcol + 1], op=ALU.mult)
                    cf = r[:, 1:2]
                else:
                    cf = r[:, 0:1]
                if first:
                    P.dve('tensor_scalar', [po.b, r.b], [a.b], out=a[:, qt, :], in0=po[:, 0:128], scalar1=cf, scalar2=None, op0=ALU.mult)
                else:
                    P.dve('scalar_tensor_tensor', [po.b, r.b, a.b], [a.b], out=a[:, qt, :], in0=po[:, 0:128], scalar=cf, in1=a[:, qt, :], op0=ALU.mult, op1=ALU.add)
                return r
            return cb

        def store_head(a, col0):
            for qt in range(16):
                o = ob[oi[0] % 2]
                oi[0] += 1
                P.act(o[:], a[:, qt, :], AF.Copy, [a.b], [o.b])
                P.dma('sp', OALL[qt * 128:(qt + 1) * 128, col0:col0 + 128], o[:], [o.b], [bS["OALL"]])

        for g in range(2):
            P.dma('sp', kc[:], KC_[g], [bS["KC"]], [kc.b])
            P.dve('memset', [], [V1c.b], V1c[:, :, 128:129], 1.0)
            P.dma('sp', V1c[:, :, 0:128], VC_[g].rearrange("(t p) d -> p t d", p=128), [bS["VC"]], [V1c.b])
            P.dve('tensor_copy', [amf.b], [V1c.b], out=V1c[:, :, 129:193], in_=amf[:])
            P.dve('tensor_scalar', [V1c.b, ctxv.b], [V1c.b], out=V1c[:, 0, :], in0=V1c[:, 0, :], scalar1=ctxv[:, 0:1], scalar2=None, op0=ALU.mult)
            for r4 in range(4):
                h = g * 4 + r4
                a = acc[r4]
                P.dma('sp', qT[:], QTn[h], [bS["QTn"]], [qT.b])
                P.dma('sp', Fb[:, 0:4096].rearrange("p (t q) -> p t q", t=2), L["Fcmp"][h].rearrange("t p q -> p t q"), [], [Fb.b])
                base_cb = branch_cb(a, h * 3 + 0, True)

                def cb(qc, j, po, base_cb=base_cb, r4=r4):
                    r = base_cb(qc, j, po)
                    qt = qc * 4 + j
                    if r4 == 0:
                        P.dve('tensor_scalar', [po.b, r.b], [imp.b], out=imp[:, qt, :], in0=po[:, 129:193], scalar1=r[:, 0:1], scalar2=None, op0=ALU.mult)
                    else:
                        P.dve('scalar_tensor_tensor', [po.b, r.b, imp.b], [imp.b], out=imp[:, qt, :], in0=po[:, 129:193], scalar=r[:, 0:1], in1=imp[:, qt, :], op0=ALU.mult, op1=ALU.add)
                for qc in range(4):
                    kts = [dict(lhsT=[kc[:, nt * 128:(nt + 1) * 128]], v=V1c[:, nt, :], bias=Fb[:, nt * 2048 + qc * 512:nt * 2048 + (qc + 1) * 512]) for nt in range(2)]
                    emit_attn(P, ar, 512, 128 ** -0.5, [qT[:, qc * 512:(qc + 1) * 512]], kts, R_all, cb, qc, VW=193)
            for qt in range(16):
                P.dma('sp', vm_t[:], L["vmul"][qt * 128:(qt + 1) * 128, :], [], [vm_t.b])
                P.dma('sp', am_t[:], L["addm"][qt * 128:(qt + 1) * 128, :], [], [am_t.b])
                P.dve('tensor_tensor', [imp.b, vm_t.b], [sc_t.b], out=sc_t[:], in0=imp[:, qt, :], in1=vm_t[:], op=ALU.mult)
                P.dve('tensor_tensor', [sc_t.b, am_t.b], [sc_t.b], out=sc_t[:], in0=sc_t[:], in1=am_t[:], op=ALU.add)
                P.dve('max', [sc_t.b], [m8.b], out=m8[:, 0:8], in_=sc_t[:])
                P.dve('match_replace', [sc_t.b, m8.b], [sc2.b], out=sc2[:], in_to_replace=m8[:, 0:8], in_values=sc_t[:], imm_value=-2.0)
                P.dve('max', [sc2.b], [m8.b], out=m8[:, 8:16], in_=sc2[:])
                P.dve('tensor_scalar', [sc_t.b, m8.b], [sc2.b], out=sc2[:], in0=sc_t[:], scalar1=m8[:, 15:16], scalar2=None, op0=ALU.is_ge)
                P.dve('scalar_tensor_tensor', [sc_t.b, sc2.b], [sc2.b], out=sc2[:], in0=sc_t[:], scalar=-0.5, in1=sc2[:], op0=ALU.is_gt, op1=ALU.mult)
                P.dve('tensor_scalar', [sc2.b], [ngb.b], out=ngb[:], in0=sc2[:], scalar1=-1.0, scalar2=BIGM, op0=ALU.add, op1=ALU.mult)
                P.tr(ptn[:, :], ngb[:, :], ident[:], [ngb.b, ident.b], [ptn.b])
                P.act(negT[:, qt * 128:(qt + 1) * 128], ptn[:, :], AF.Copy, [ptn.b], [negT.b])
            for r4 in range(4):
                h = g * 4 + r4
                a = acc[r4]
                P.dma('sp', qT[:], QTn[h], [bS["QTn"]], [qT.b])
                for br in range(2):
                    P.dma('sp', kT[:], KX[2 + br, g], [bS["KX"]], [kT.b])
                    load_v(VX[br][:, g * 128:(g + 1) * 128])
                    if br == 0:
                        P.dma('sp', Fb[:], L["Fslc"][h], [], [Fb.b])
                    else:
                        P.dma('sp', Fb[:, 0:1408], L["Fwin"][h], [], [Fb.b])
                    cb = branch_cb(a, h * 3 + 1 + br, False)
                    for qc in range(4):
                        q0 = 2048 + qc * 512
                        lo = 0 if br == 0 else (q0 - 512) // 128
                        kts = []
                        for kt in range(lo, (q0 + 512) // 128):
                            o = (q0 - kt * 128) // 128
                            d = dict(lhsT=[kT[:, kt * 128:(kt + 1) * 128]], v=V1[:, kt, :], bias=Fb[:, 128 * (o + 3):128 * (o + 3) + 512])
                            if br == 0:
                                d['extra'] = [(Em[:, kt * 128:(kt + 1) * 128], negT[:, qc * 512:(qc + 1) * 512])]
                            kts.append(d)
                        emit_attn(P, ar, 512, 128 ** -0.5, [qT[:, qc * 512:(qc + 1) * 512]], kts, R_all, cb, qc)
                store_head(a, h * 128)
        P.dma('sp', kR[:], KR, [bS["KR"]], [kR.b])
        for h in range(8):
            a = acc[h % 4]
            P.dma('sp', kT[:], KN[h], [bS["KN"]], [kT.b])
            P.dma('sp', qT[:], QN[h], [bS["QN"]], [qT.b])
            P.dma('sp', qR[:], QR[h], [bS["QR"]], [qR.b])
            load_v(VM[:, h * 128:(h + 1) * 128])
            cb = branch_cb(a, None, True)
            for qc in range(4):
                q0 = 2048 + qc * 512
                kts = []
                for kt in range(0, (q0 + 512) // 128):
                    o = (q0 - kt * 128) // 128
                    kts.append(dict(lhsT=[kT[:, kt * 128:(kt + 1) * 128], kR[:, kt * 128:(kt + 1) * 128]], v=V1[:, kt, :],
                                    bias=(Fm[:, 128 * (o + 3):128 * (o + 3) + 512] if o <= 0 else None)))
                emit_attn(P, ar, 512, 192 ** -0.5, [qT[:, qc * 512:(qc + 1) * 512], qR[:, qc * 512:(qc + 1) * 512]], kts, R_all, cb, qc)
            store_head(a, 1024 + h * 128)

    if upto < 4:
        return
    with C.phase():
        wo = C.sb([128, KC, D], BF16, "wo")
        P.dma('pool', wo[:], L["w_out"].rearrange("(kc p) n -> p kc n", p=128), [], [wo.b])
        ot = [C.sb([128, D], BF16, "ot") for _ in range(2)]
        oT = C.sb([128, KC, TG], BF16, "oT")
        xr = [C.sb([128, D], F32, "xr") for _ in range(2)]
        tmp = [C.sb([128, 512], F32, "tmp") for _ in range(2)]
        ptr = [C.ps([128, 1024], BF16, "ptr") for _ in range(2)]
        py = [C.ps([128, 512], F32, "py") for _ in range(2)]
        it = 0
        for gi in range(4):
            for tt in range(4):
                t0 = gi * TG + tt * 128
                o = ot[tt % 2]
                P.dma('sp', o[:], OALL[t0:t0 + 128, :], [bS["OALL"]], [o.b])
                for half in range(2):
                    pt = ptr[half]
                    for c in range(8):
                        cc = half * 8 + c
                        P.tr(pt[:, c * 128:(c + 1) * 128], o[:, cc * 128:(cc + 1) * 128], ident[:], [o.b, ident.b], [pt.b])
                    P.act(oT[:, half * 8:(half + 1) * 8, tt * 128:(tt + 1) * 128], pt[:].rearrange("p (c t) -> p c t", c=8), AF.Copy, [pt.b], [oT.b])
            for tt in range(4):
                t0 = gi * TG + tt * 128
                x = xr[tt % 2]
                P.dma('sp', x[:], xl[2048 + t0:2048 + t0 + 128, :], [], [x.b])
                for nb in range(4):
                    p, tm = py[it % 2], tmp[it % 2]
                    it += 1
                    sl = slice(nb * 512, (nb + 1) * 512)
                    for kc_ in range(KC):
                        P.mm(p[:], oT[:, kc_, tt * 128:(tt + 1) * 128], wo[:, kc_, sl], kc_ == 0, kc_ == KC - 1, [oT.b, wo.b], [p.b])
                    P.dve('tensor_tensor', [p.b, mods[2].b], [tm.b], out=tm[:], in0=p[:], in1=mods[2][:, sl], op=ALU.mult)
                    P.dve('tensor_tensor', [tm.b, x.b], [x.b], out=x[:, sl], in0=x[:, sl], in1=tm[:], op=ALU.add)
                P.dma('sp', X1A[t0:t0 + 128, :], x[:], [x.b], [bS["X1A"]])

    if upto < 5:
        return
    with C.phase():
        nb_ = norm_bufs(C)

        def loader(g, hT):
            emit_norm_T(C, P, nb_, X1A[g * TG:(g + 1) * TG, :], mods[4], mods[3], ident, hT, xdeps=[bS["X1A"]])
        emit_swiglu(C, P, loader, 4, L["wg"], L["wu"], L["wd"], 44, None, YF, bS["YF"], nwd=1)

    with C.phase():
        xa = [C.sb([128, D], F32, "xa") for _ in range(2)]
        yy = [C.sb([128, D], F32, "yy") for _ in range(2)]
        for t in range(16):
            a, y = xa[t % 2], yy[t % 2]
            P.dma('sp', a[:], X1A[t * 128:(t + 1) * 128, :], [bS["X1A"]], [a.b])
            P.dma('sp', y[:], YF[t * 128:(t + 1) * 128, :], [bS["YF"]], [y.b])
            P.dve('tensor_tensor', [y.b, mods[5].b], [y.b], out=y[:], in0=y[:], in1=mods[5][:], op=ALU.mult)
            P.dve('tensor_tensor', [y.b, a.b], [a.b], out=a[:], in0=a[:], in1=y[:], op=ALU.add)
            P.dma('sp', L["x1"][t * 128:(t + 1) * 128, :], a[:], [a.b], [])


def l0_tables(rel_bias, s):
    rb = np.asarray(rel_bias, np.float32)
    t = {}
    t["Fwin"] = toeplitz_table(rb, range(8), 1408, 384, 1, 511)
    t["Fslc"] = toeplitz_table(rb, range(8), 4864, 384, 1, 10 ** 9)
    k = np.arange(128)[:, None]
    j = np.arange(1024)[None, :]
    t["Fmla"] = np.where(j - k - 384 >= 0, np.float32(0), np.float32(NEG)).astype(np.float32)
    n = (np.arange(2)[:, None, None] * 128 + np.arange(128)[None, :, None])
    tl = 2048 + np.arange(2048)[None, None, :]
    rel = tl - (16 * n + 31)
    ok = (rel >= 0) & (n <= 254)
    bk = t5_bucket_np(rel)
    t["Fcmp"] = np.stack([np.where(ok, rb[bk, h], np.float32(NEG)) for h in range(8)], 0).astype(np.float32)
    q = np.arange(2048)[:, None]
    jb = np.arange(64)[None, :]
    blk_t = (2048 + q) // 64
    first = 0 if s == 1 else 32
    valid = (jb <= blk_t) & (jb >= first)
    forced = valid & ((jb == first) | (jb == blk_t) | (jb == blk_t - 1))
    t["vmul"] = (valid & ~forced).astype(np.float32)
    fval = np.where(jb == first, np.float32(3e9), np.where(jb == blk_t, np.float32(2e9), np.float32(1e9)))
    t["addm"] = np.where(forced, fval, np.where(valid, np.float32(0), np.float32(-1))).astype(np.float32)
    A = np.zeros((256, 64), np.float32)
    for jj in range(64):
        for m in range(4):
            for n2 in range(2):
                i = 4 * jj + m - n2
                if 0 <= i < 256:
                    A[i, jj] += 1
    t["Amat"] = A
    t["Emat"] = (np.arange(4096)[None, :] // 64 == np.arange(64)[:, None]).astype(np.float32)
    inv = (np.float32(10000.0) ** (-np.arange(0, 64, 2, dtype=np.float32) / np.float32(64))).astype(np.float32)
    t["invf"] = np.stack([np.concatenate([inv, inv]), np.concatenate([-np.ones(32, np.float32), np.ones(32, np.float32)])], 1).astype(np.float32)
    t["ctxv"] = np.full((128, 1), float(s), np.float32)
    t["ident"] = np.eye(128, dtype=np.float32)
    return t


def l0_inputs(inp, b, s, shared, mods):
    x = inp['x'][b]
    pos = np.asarray(inp['positions'][b]).astype(np.int32)
    if s == 1:
        xl, pl = x, pos
    else:
        xl = np.concatenate([np.zeros((2048, D), np.float32), x[:2048]], 0)
        pl = np.concatenate([np.zeros(2048, np.int32), pos[:2048]])
    d = dict(shared[s])
    d.update({"xl": np.ascontiguousarray(xl), "pos": np.ascontiguousarray(pl[None]), "modrow": np.ascontiguousarray(mods[0, b][None])})
    return d


def l0_shared(inp):
    f = lambda a: np.ascontiguousarray(np.asarray(a, np.float32))
    base = {"gn_m": f(inp['mix_norm_g'][0][None]), "gn_f": f(inp['ffn_norm_g'][0][None]),
            "w_in": f(inp['ab_w_in'][0]), "w_out": f(inp['ab_w_out'][0]),
            "pos_k": f(inp['nsa_cmp_pos_k'][0]), "pos_v": f(inp['nsa_cmp_pos_v'][0]), "w1_k": f(inp['nsa_cmp_w1_k'][0]), "w1_v": f(inp['nsa_cmp_w1_v'][0]),
            "w2_k": f(inp['nsa_cmp_w2_k'][0]), "w2_v": f(inp['nsa_cmp_w2_v'][0]),
            "qng": f(inp['mla_q_norm_g'][0].reshape(4, 128).T), "kvng": f(inp['mla_kv_norm_g'][0].reshape(4, 128).T),
            "w_uq": f(inp['mla_w_uq'][0]), "w_ukv": f(inp['mla_w_ukv'][0]),
            "wg": f(inp['ffn_w_gate'][0]), "wu": f(inp['ffn_w_up'][0]), "wd": f(inp['ffn_w_down'][0])}
    return [{**base, **l0_tables(inp['rel_bias'], s)} for s in range(2)]


def build_final():
    nc = bass.Bass("TRN2", target_bir_lowering=False)
    x1m = nc.dram_tensor("x1m", [2048, D], F32, kind="ExternalInput").ap()
    parts = nc.dram_tensor("parts", [8, 2048, D], F32, kind="ExternalInput").ap()
    gf = nc.dram_tensor("gf", [1, D], F32, kind="ExternalInput").ap()
    fg = nc.dram_tensor("fg", [1, D], F32, kind="ExternalInput").ap()
    out = nc.dram_tensor("out", [2048, D], F32, kind="ExternalOutput").ap()
    P = Prog(nc)
    C = Ctx(nc, P)
    with C.phase():
        gfb = C.sb([128, D], F32, "gfb")
        fgb = C.sb([128, D], F32, "fgb")
        xb = [C.sb([128, D], F32, "xb") for _ in range(2)]
        acc = [C.sb([128, D], F32, "acc") for _ in range(2)]
        pb = [C.sb([128, D], F32, "pb") for _ in range(3)]
        junk = C.sb([128, D], BF16, "junk")
        st = [C.sb([128, 4], F32, "st") for _ in range(2)]
        P.dma('sp', gfb[:], gf.partition_broadcast(128), [], [gfb.b])
        P.dma('sp', fgb[:], fg.partition_broadcast(128), [], [fgb.b])
        k = 0
        for t in range(16):
            x, a, s_ = xb[t % 2], acc[t % 2], st[t % 2]
            rows = slice(t * 128, (t + 1) * 128)
            P.dma('sp', x[:], x1m[rows, :], [], [x.b])
            P.dma('sp', a[:], parts[0, rows, :], [], [a.b])
            for e in range(1, 8):
                p = pb[k % 3]
                k += 1
                P.dma('sp', p[:], parts[e, rows, :], [], [p.b])
                P.dve('tensor_tensor', [a.b, p.b], [a.b], out=a[:], in0=a[:], in1=p[:], op=ALU.add)
            P.dve('tensor_tensor', [a.b, gfb.b], [a.b], out=a[:], in0=a[:], in1=gfb[:], op=ALU.mult)
            P.dve('tensor_tensor', [a.b, x.b], [x.b], out=x[:], in0=x[:], in1=a[:], op=ALU.add)
            P.act(junk[:], x[:], AF.Square, [x.b], [junk.b, s_.b], accum_out=s_[:, 0:1])
            P.dve('tensor_scalar', [s_.b], [s_.b], out=s_[:, 1:2], in0=s_[:, 0:1], scalar1=1.0 / D, scalar2=EPS, op0=ALU.mult, op1=ALU.add)
            P.act(s_[:, 3:4], s_[:, 1:2], AF.Sqrt, [s_.b], [s_.b])
            P.dve('reciprocal', [s_.b], [s_.b], out=s_[:, 2:3], in_=s_[:, 3:4])
            P.dve('scalar_tensor_tensor', [x.b, s_.b, fgb.b], [x.b], out=x[:], in0=x[:], scalar=s_[:, 2:3], in1=fgb[:], op0=ALU.mult, op1=ALU.mult)
            P.dma('sp', out[rows, :], x[:], [x.b], [])
    P.finish()
    return nc


def _launch(nc, ims):
    return run_bass_kernel_spmd(nc, ims, core_ids=list(range(8))).results


def kernel(**inp):
    f32 = lambda a: np.ascontiguousarray(np.asarray(a, np.float32))
    inp = {k: np.asarray(v) for k, v in inp.items()}
    mods = run_mods(inp)
    shared = l0_shared(inp)
    res = _launch(build_l0(), [l0_inputs(inp, i // 2, i % 2, shared, mods) for i in range(8)])
    x1 = np.empty((4, 4096, D), np.float32)
    for i in range(8):
        x1[i // 2, (i % 2) * 2048:(i % 2 + 1) * 2048] = res[i]["x1"]
    del shared, res
    base = {"gn_m": f32(inp['mix_norm_g'][1][None]),
            "gn_f": f32(inp['ffn_norm_g'][1][None]), "c_w_in": f32(inp['c_w_in'][0]), "c_w_out": f32(inp['c_w_out'][0]),
            "wrT": f32(np.asarray(inp['moe_w_router'][0]).T).reshape(1, -1), "Fd": make_Fd(f32(inp['rel_bias'])),
            "ident": np.eye(128, dtype=np.float32)}
    ims = []
    for i in range(8):
        b, s = i // 2, i % 2
        xl = x1[b] if s == 1 else np.concatenate([np.zeros((2048, D), np.float32), x1[b, :2048]], 0)
        ims.append({**base, "xl": np.ascontiguousarray(xl), "modrow": np.ascontiguousarray(mods[1, b][None]),
                    "ctxv": np.full((128, 1), float(s), np.float32)})
    res = _launch(build_l1mix(), ims)
    x1m = [np.asarray(res[i]["x1m"]) for i in range(8)]
    gfs = [np.asarray(res[i]["gf"]) for i in range(8)]
    hT_all = np.ascontiguousarray(np.concatenate([np.asarray(res[i]["hTf"]) for i in range(8)], axis=1))
    gates = np.concatenate([np.asarray(res[i]["gates"]) for i in range(8)], axis=0)
    del ims, base, res, x1
    ims = [{"hT": hT_all, "gate": np.ascontiguousarray(gates[:, e].reshape(128, 128).T),
            "wg": f32(inp['moe_w_gate'][0, e]), "wu": f32(inp['moe_w_up'][0, e]), "wd": f32(inp['moe_w_down'][0, e])} for e in range(8)]
    res = _launch(build_moe(), ims)
    ys = [np.asarray(res[e]["y"]) for e in range(8)]
    del ims, res
    fg = f32(inp['final_norm_g'][None])
    ims = [{"x1m": x1m[i], "parts": np.ascontiguousarray(np.stack([ys[e][i * 2048:(i + 1) * 2048] for e in range(8)], 0)),
            "gf": gfs[i], "fg": fg} for i in range(8)]
    res = _launch(build_final(), ims)
    out = np.empty((4, 4096, D), np.float32)
    for i in range(8):
        out[i // 2, (i % 2) * 2048:(i % 2 + 1) * 2048] = res[i]["out"]
    return out
```

```python
import numpy as np
from contextlib import ExitStack
import concourse.bass as bass
import concourse.mybir as mybir
from concourse.bass_utils import run_bass_kernel_spmd

F32 = mybir.dt.float32
BF16 = mybir.dt.bfloat16
I32 = mybir.dt.int32
AF = mybir.ActivationFunctionType
ALU = mybir.AluOpType
AX = mybir.AxisListType

ENG = ['pe', 'act', 'dve', 'pool', 'sp']


class Buf:
    __slots__ = ('name', 'w', 'r')

    def __init__(self, name=''):
        self.name = name
        self.w = None
        self.r = {}


class Prog:
    def __init__(self, nc):
        self.nc = nc
        self.ops = {e: [] for e in ENG}
        self.cnt = {}
        self.seen = {e: {} for e in ENG}
        self.pending = {e: {} for e in ENG}
        self.nslots = 12
        self.rr = {e: 0 for e in ENG}
        self.sems = {}
        self.stack = ExitStack()
        self.nblk = 0

    def _need(self, E, waits, k, v):
        if k == ('e', 'pe') and E == 'pe':
            return
        if waits.get(k, 0) < v:
            waits[k] = v

    def op(self, E, fn, reads=(), writes=(), dma=False):
        waits = dict(self.pending[E])
        self.pending[E] = {}
        for b in reads:
            if b.w is not None:
                self._need(E, waits, *b.w)
        for b in writes:
            if b.w is not None:
                self._need(E, waits, *b.w)
            for k, v in b.r.items():
                self._need(E, waits, k, v)
        if dma:
            s = self.rr[E]
            self.rr[E] = (s + 1) % self.nslots
            key = ('d', E, s)
            prev = self.cnt.get(key, 0)
            if prev:
                self._need(E, waits, key, prev)
            val = prev + 16
            inc = 16
        else:
            key = ('e', E)
            val = self.cnt.get(key, 0) + 1
            inc = 1
        self.cnt[key] = val
        w2 = []
        for k, v in waits.items():
            if self.seen[E].get(k, 0) >= v:
                continue
            self.seen[E][k] = v
            w2.append((k, v))
        self.ops[E].append((w2, fn, key, inc))
        for b in reads:
            if b.r.get(key, 0) < val:
                b.r[key] = val
        for b in writes:
            b.w = (key, val)
            b.r = {}

    def barrier(self):
        for E in ENG:
            for k, v in self.cnt.items():
                if self.pending[E].get(k, 0) < v:
                    self.pending[E][k] = v

    def sem(self, k):
        if k not in self.sems:
            self.sems[k] = self.stack.enter_context(self.nc.semaphore("s_" + "_".join(map(str, k))))
        return self.sems[k]

    def flush(self):
        nc = self.nc
        for E in ENG:
            for (w, fn, key, inc) in self.ops[E]:
                if key is not None:
                    self.sem(key)
                for k, v in w:
                    self.sem(k)
        ops = self.ops
        self.ops = {e: [] for e in ENG}
        sem = self.sems

        def run(E):
            def f(e):
                for (w, fn, key, inc) in ops[E]:
                    for (k, v) in w:
                        e.wait_ge(sem[k], v)
                    if fn is not None:
                        fn(e).then_inc(sem[key], inc)
            return f
        with nc.Block() as block:
            block.tensor(run('pe'))
            block.scalar(run('act'))
            block.vector(run('dve'))
            block.gpsimd(run('pool'))
            block.sync(run('sp'))
        self.nblk += 1

    def finish(self):
        self.barrier()
        for E in ENG:
            waits = self.pending[E]
            self.pending[E] = {}
            w2 = [(k, v) for k, v in waits.items() if self.seen[E].get(k, 0) < v]
            self.ops[E].append((w2, None, None, 0))
        self.flush()
        self.stack.close()

    def mm(self, out, lhsT, rhs, start, stop, R, W):
        self.op('pe', lambda e: e.matmul(out, lhsT=lhsT, rhs=rhs, start=start, stop=stop), R, W)

    def tr(self, out, in_, ident, R, W):
        self.op('pe', lambda e: e.transpose(out=out, in_=in_, identity=ident), R, W)

    def dma(self, q, out, in_, R, W):
        self.op(q, lambda e: e.dma_start(out=out, in_=in_), R, W, dma=True)

    def act(self, out, in_, func, R, W, **kw):
        self.op('act', lambda e: e.activation(out=out, in_=in_, func=func, **kw), R, W)

    def dve(self, name, R, W, *a, **kw):
        self.op('dve', lambda e: getattr(e, name)(*a, **kw), R, W)

    def pool(self, name, R, W, *a, **kw):
        self.op('pool', lambda e: getattr(e, name)(*a, **kw), R, W)


class T:
    def __init__(self, h, name):
        self.h = h
        self.b = Buf(name)

    def __getitem__(self, k):
        return self.h[k]


class Ctx:
    def __init__(self, nc, P):
        self.nc = nc
        self.P = P
        self.st = None
        self.n = 0

    def phase(self):
        return _Phase(self)

    def sb(self, shape, dt, name=None):
        self.n += 1
        name = name or "t"
        return T(self.st.enter_context(self.nc.sbuf_tensor(f"{name}_{self.n}", list(shape), dt)), name)

    def ps(self, shape, dt, name=None):
        self.n += 1
        name = name or "p"
        width = 512 if dt == F32 else 1024
        assert len(shape) == 2 and shape[1] <= width
        full = self.st.enter_context(self.nc.psum_tensor(f"{name}_{self.n}", [128, width], dt))
        return T(full[0:shape[0], 0:shape[1]], name)


class _Phase:
    def __init__(self, c):
        self.c = c

    def __enter__(self):
        self.c.P.barrier()
        self.c.st = ExitStack()
        self.c.st.__enter__()
        return self.c

    def __exit__(self, *a):
        if a[0] is None:
            self.c.P.flush()
        self.c.st.__exit__(*a)
        self.c.st = None
        return False


D = 2048
KC = 16
TG = 512
EPS = 1e-6


def rearr_w(ap):
    return ap.rearrange("(kc p) n -> p kc n", p=128)


def emit_norm_T(C, P, bufs, x_dram_rows, A, B, ident, hT, xdeps=(), tile_cb=None):
    xb, hb, junk, st, pst = bufs
    for tt in range(4):
        x = xb[tt % 2]
        h = hb[tt % 2]
        P.dma('sp', x[:], x_dram_rows[tt * 128:(tt + 1) * 128, :], list(xdeps), [x.b])
        P.act(junk[:], x[:], AF.Square, [x.b], [junk.b, st.b], accum_out=st[:, 0:1])
        P.dve('tensor_scalar', [st.b], [st.b], out=st[:, 1:2], in0=st[:, 0:1], scalar1=1.0 / D, scalar2=EPS, op0=ALU.mult, op1=ALU.add)
        P.act(st[:, 3:4], st[:, 1:2], AF.Sqrt, [st.b], [st.b])
        P.dve('reciprocal', [st.b], [st.b], out=st[:, 2:3], in_=st[:, 3:4])
        P.dve('scalar_tensor_tensor', [x.b, st.b, A.b], [x.b], out=x[:], in0=x[:], scalar=st[:, 2:3], in1=A[:], op0=ALU.mult, op1=ALU.mult)
        P.dve('tensor_tensor', [x.b, B.b], [x.b], out=x[:], in0=x[:], in1=B[:], op=ALU.add)
        P.act(h[:], x[:], AF.Copy, [x.b], [h.b])
        if tile_cb is not None:
            tile_cb(tt, x)
        for half in range(2):
            pt = pst[half]
            for c in range(8):
                cc = half * 8 + c
                P.tr(pt[:, c * 128:(c + 1) * 128], h[:, cc * 128:(cc + 1) * 128], ident[:], [h.b, ident.b], [pt.b])
            dst = hT[:, half * 8:(half + 1) * 8, tt * 128:(tt + 1) * 128]
            src = pt[:].rearrange("p (c t) -> p c t", c=8)
            if half == 0:
                P.act(dst, src, AF.Copy, [pt.b], [hT.b])
            else:
                P.dve('tensor_copy', [pt.b], [hT.b], out=dst, in_=src)


def emit_swiglu(C, P, hT_loader, ngroups, wg, wu, wd, NF, gate, y_dram, yb, nwd=2):
    hT = C.sb([128, KC, TG], BF16, "hT")
    aT = C.sb([128, NF, TG], BF16, "aT")
    wgb = [C.sb([128, KC, 256], BF16, "wgb") for _ in range(2)]
    wub = [C.sb([128, KC, 256], BF16, "wub") for _ in range(2)]
    wdb = [C.sb([128, NF, 256], BF16, "wdb") for _ in range(nwd)]
    sg = [C.sb([128, TG], F32, "sg") for _ in range(2)]
    pg = [C.ps([128, TG], F32, "pg") for _ in range(2)]
    pu = [C.ps([128, TG], F32, "pu") for _ in range(2)]
    pd = [C.ps([128, 256], F32, "pd") for _ in range(2)]
    stg = [C.sb([128, 256], F32, "stg") for _ in range(3)]
    it = 0
    io = 0
    for g in range(ngroups):
        hT_loader(g, hT)
        for fb in range(NF // 2):
            a = wgb[fb % 2]
            b = wub[fb % 2]
            P.dma('pool', a[:], rearr_w(wg[:, fb * 256:(fb + 1) * 256]), [], [a.b])
            P.dma('pool', b[:], rearr_w(wu[:, fb * 256:(fb + 1) * 256]), [], [b.b])
            for j in range(2):
                fc = fb * 2 + j
                p1 = pg[it % 2]
                p2 = pu[it % 2]
                s = sg[it % 2]
                it += 1
                for k in range(KC):
                    P.mm(p1[:], a[:, k, j * 128:(j + 1) * 128], hT[:, k, :], k == 0, k == KC - 1, [a.b, hT.b], [p1.b])
                for k in range(KC):
                    P.mm(p2[:], b[:, k, j * 128:(j + 1) * 128], hT[:, k, :], k == 0, k == KC - 1, [b.b, hT.b], [p2.b])
                P.act(s[:], p1[:], AF.Silu, [p1.b], [s.b])
                P.dve('tensor_tensor', [s.b, p2.b], [aT.b], out=aT[:, fc, :], in0=s[:], in1=p2[:], op=ALU.mult)
        for nb in range(8):
            w = wdb[nb % nwd]
            P.dma('pool', w[:], wd[:, nb * 256:(nb + 1) * 256].rearrange("(fc p) n -> p fc n", p=128), [], [w.b])
            for tt in range(4):
                p = pd[io % 2]
                s = stg[io % 3]
                io += 1
                for fc in range(NF):
                    P.mm(p[:], aT[:, fc, tt * 128:(tt + 1) * 128], w[:, fc, :], fc == 0, fc == NF - 1, [aT.b, w.b], [p.b])
                if gate is not None:
                    P.act(s[:], p[:], AF.Copy, [p.b, gate.b], [s.b], scale=gate[:, g * 4 + tt:g * 4 + tt + 1])
                else:
                    P.act(s[:], p[:], AF.Copy, [p.b], [s.b])
                P.dma('sp', y_dram[(g * 4 + tt) * 128:(g * 4 + tt + 1) * 128, nb * 256:(nb + 1) * 256], s[:], [s.b], [yb])


def build_moe(ntok=16384, NF=56):
    nc = bass.Bass("TRN2", target_bir_lowering=False)
    hTd = nc.dram_tensor("hT", [D, ntok], BF16, kind="ExternalInput").ap()
    gated = nc.dram_tensor("gate", [128, ntok // 128], F32, kind="ExternalInput").ap()
    wg = nc.dram_tensor("wg", [D, NF * 128], F32, kind="ExternalInput").ap()
    wu = nc.dram_tensor("wu", [D, NF * 128], F32, kind="ExternalInput").ap()
    wd = nc.dram_tensor("wd", [NF * 128, D], F32, kind="ExternalInput").ap()
    y = nc.dram_tensor("y", [ntok, D], F32, kind="ExternalOutput").ap()
    P = Prog(nc)
    C = Ctx(nc, P)
    yb = Buf('y')
    with C.phase():
        gate = C.sb([128, ntok // 128], F32, "gate")
        P.dma('sp', gate[:], gated, [], [gate.b])

        def loader(g, hT):
            P.dma('sp', hT[:], hTd[:, g * TG:(g + 1) * TG].rearrange("(kc p) t -> p kc t", p=128), [], [hT.b])
        emit_swiglu(C, P, loader, ntok // TG, wg, wu, wd, NF, gate, y, yb)
    P.finish()
    return nc


def emit_mod_row(C, P, modrow, gn_m, gn_f, outs):
    gn = [C.sb([128, D], F32, "gn") for _ in range(2)]
    P.dma('sp', gn[0][:], gn_m.partition_broadcast(128), [], [gn[0].b])
    P.dma('sp', gn[1][:], gn_f.partition_broadcast(128), [], [gn[1].b])
    for part in range(6):
        dst = outs[part]
        P.dma('sp', dst[:], modrow[:, part * D:(part + 1) * D].partition_broadcast(128), [], [dst.b])
        if part in (1, 4):
            g = gn[0] if part == 1 else gn[1]
            P.dve('scalar_tensor_tensor', [dst.b, g.b], [dst.b], out=dst[:], in0=dst[:], scalar=1.0, in1=g[:], op0=ALU.add, op1=ALU.mult)


def build_mods():
    nc = bass.Bass("TRN2", target_bir_lowering=False)
    cT = nc.dram_tensor("cT", [128, KC, 128], F32, kind="ExternalInput").ap()
    aw = nc.dram_tensor("aw", [2, D, 1536], F32, kind="ExternalInput").ap()
    ab = nc.dram_tensor("ab", [2, 1, 1536], F32, kind="ExternalInput").ap()
    mo = nc.dram_tensor("mod", [2, 128, 1536], F32, kind="ExternalOutput").ap()
    P = Prog(nc)
    C = Ctx(nc, P)
    with C.phase():
        cs = C.sb([128, KC, 128], F32, "cs")
        w = [C.sb([128, KC, 512], F32, "w") for _ in range(2)]
        bb = C.sb([128, 1536], F32, "bb")
        o = [C.sb([128, 1536], F32, "o") for _ in range(2)]
        ps = [C.ps([128, 512], F32, "ps") for _ in range(2)]
        P.dma('sp', cs[:], cT, [], [cs.b])
        P.act(cs[:], cs[:], AF.Silu, [cs.b], [cs.b])
        it = 0
        for l in range(2):
            P.dma('sp', bb[:], ab[l].partition_broadcast(128), [], [bb.b])
            for ch in range(3):
                ww, p = w[it % 2], ps[it % 2]
                it += 1
                P.dma('sp', ww[:], rearr_w(aw[l][:, ch * 512:(ch + 1) * 512]), [], [ww.b])
                for k in range(KC):
                    P.mm(p[:], cs[:, k, :], ww[:, k, :], k == 0, k == KC - 1, [cs.b, ww.b], [p.b])
                P.dve('tensor_tensor', [p.b, bb.b], [o[l].b], out=o[l][:, ch * 512:(ch + 1) * 512], in0=p[:], in1=bb[:, ch * 512:(ch + 1) * 512], op=ALU.add)
            P.dma('sp', mo[l], o[l][:], [o[l].b], [])
    P.finish()
    return nc


def run_mods(inp):
    c = np.asarray(inp['c'], np.float32)
    cT = np.zeros((128, KC, 128), np.float32)
    cT[:, :, :4] = c.reshape(4, KC, 128).transpose(2, 1, 0)
    aw = np.asarray(inp['ada_w'], np.float32)
    ab = np.asarray(inp['ada_b'], np.float32)
    ims = [{"cT": cT, "aw": np.ascontiguousarray(aw[:, :, i * 1536:(i + 1) * 1536]),
            "ab": np.ascontiguousarray(ab[:, None, i * 1536:(i + 1) * 1536])} for i in range(8)]
    res = run_bass_kernel_spmd(build_mods(), ims, core_ids=list(range(8))).results
    return np.concatenate([np.asarray(res[i]["mod"])[:, :4] for i in range(8)], axis=2)


class AttnRes:
    def __init__(self, C):
        self.sp = [C.ps([128, 512], F32, "sp") for _ in range(2)]
        self.po = [C.ps([128, 256], F32, "po") for _ in range(4)]
        self.sbt = [C.sb([128, 512], F32, "sbt") for _ in range(2)]
        self.pt = [C.sb([128, 512], BF16, "pt") for _ in range(3)]
        self.i = 0


def emit_attn(P, ar, QW, scale, rhs_list, kts, R, out_cb, qc_id, VW=129):
    nj = QW // 128
    n = len(kts)
    base = ar.i
    ar.i += n

    def scores(ki):
        kt = kts[ki]
        ps = ar.sp[(base + ki) % 2]
        parts = list(zip(kt['lhsT'], rhs_list)) + list(kt.get('extra', []))
        for pi, (l, r) in enumerate(parts):
            P.mm(ps[:, 0:QW], l, r, pi == 0, pi == len(parts) - 1, R, [ps.b])

    def probs(ki):
        kt = kts[ki]
        i = base + ki
        ps = ar.sp[i % 2]
        pt = ar.pt[i % 3]
        if kt.get('bias') is not None:
            sb = ar.sbt[i % 2]
            P.dve('scalar_tensor_tensor', [ps.b] + R, [sb.b], out=sb[:, 0:QW], in0=ps[:, 0:QW], scalar=float(scale), in1=kt['bias'], op0=ALU.mult, op1=ALU.add)
            P.act(pt[:, 0:QW], sb[:, 0:QW], AF.Exp, [sb.b], [pt.b])
        else:
            P.act(pt[:, 0:QW], ps[:, 0:QW], AF.Exp, [ps.b], [pt.b], scale=float(scale))

    def pv(ki):
        pt = ar.pt[(base + ki) % 3]
        for j in range(nj):
            po = ar.po[j]
            P.mm(po[:, 0:VW], pt[:, j * 128:(j + 1) * 128], kts[ki]['v'], ki == 0, ki == n - 1, [pt.b] + R, [po.b])

    scores(0)
    for ki in range(n):
        probs(ki)
        if ki + 1 < n:
            scores(ki + 1)
        pv(ki)
    for j in range(nj):
        out_cb(qc_id, j, ar.po[j])


DIL = (1, 4, 16)


def norm_bufs(C):
    xb = [C.sb([128, D], F32, "xb") for _ in range(2)]
    hb = [C.sb([128, D], BF16, "hb") for _ in range(2)]
    junk = C.sb([128, D], BF16, "junk")
    st = C.sb([128, 4], F32, "st")
    pst = [C.ps([128, 1024], BF16, "pst") for _ in range(2)]
    return (xb, hb, junk, st, pst)


def build_l1mix():
    nc = bass.Bass("TRN2", target_bir_lowering=False)
    dt = nc.dram_tensor
    xl = dt("xl", [4096, D], F32, kind="ExternalInput").ap()
    modrow = dt("modrow", [1, 6 * D], F32, kind="ExternalInput").ap()
    gn_m = dt("gn_m", [1, D], F32, kind="ExternalInput").ap()
    gn_f = dt("gn_f", [1, D], F32, kind="ExternalInput").ap()
    w_in = dt("c_w_in", [D, 9216], F32, kind="ExternalInput").ap()
    w_out = dt("c_w_out", [1024, D], F32, kind="ExternalInput").ap()
    wrT = dt("wrT", [1, 8 * D], F32, kind="ExternalInput").ap()
    Fd = dt("Fd", [24, 128, 1024], F32, kind="ExternalInput").ap()
    ctxvd = dt("ctxv", [128, 1], F32, kind="ExternalInput").ap()
    identd = dt("ident", [128, 128], F32, kind="ExternalInput").ap()
    x1m = dt("x1m", [2048, D], F32, kind="ExternalOutput").ap()
    hTf = dt("hTf", [D, 2048], BF16, kind="ExternalOutput").ap()
    gates_o = dt("gates", [2048, 8], F32, kind="ExternalOutput").ap()
    gf_o = dt("gf", [1, D], F32, kind="ExternalOutput").ap()
    QT = dt("QT", [3, 8, 128, 2048], BF16).ap()
    KT = dt("KT", [3, 8, 128, 4096], BF16).ap()
    Vd = dt("Vd", [3, 4096, 1024], BF16).ap()
    ND = dt("ND", [3, 2048, 8, 129], F32).ap()
    bQT, bKT, bV, bND, bX1 = Buf('QT'), Buf('KT'), Buf('V'), Buf('ND'), Buf('x1m')
    P = Prog(nc)
    C = Ctx(nc, P)
    with ExitStack() as keep:
        def sbp(shape, dtp, name):
            C.n += 1
            return T(keep.enter_context(nc.sbuf_tensor(f"{name}_{C.n}", list(shape), dtp)), name)
        mods = [sbp([128, D], F32, f"mod{i}") for i in range(6)]
        ident = sbp([128, 128], BF16, "ident")
        ctxv = sbp([128, 1], F32, "ctxv")
        with C.phase():
            idf = C.sb([128, 128], F32, "idf")
            P.dma('sp', idf[:], identd, [], [idf.b])
            P.dve('tensor_copy', [idf.b], [ident.b], out=ident[:], in_=idf[:])
            P.dma('sp', ctxv[:], ctxvd, [], [ctxv.b])
            emit_mod_row(C, P, modrow, gn_m, gn_f, mods)
            P.dma('sp', gf_o, mods[5][0:1, :], [mods[5].b], [])
        with C.phase():
            nb_ = norm_bufs(C)
            hT = C.sb([128, KC, TG], BF16, "hT")
            wb = [C.sb([128, KC, 512], BF16, "wb") for _ in range(2)]
            ps = [C.ps([128, 512], F32, "psz") for _ in range(2)]
            stg = [C.sb([128, 512], BF16, "stg") for _ in range(3)]
            it = 0
            for gi in range(8):
                tok0 = gi * TG
                emit_norm_T(C, P, nb_, xl[tok0:tok0 + TG, :], mods[1], mods[0], ident, hT)
                for blk in range(18):
                    g, j, hb2 = blk // 6, (blk % 6) // 2, blk % 2
                    if j == 0 and gi < 4:
                        continue
                    w = wb[blk % 2]
                    P.dma('pool', w[:], rearr_w(w_in[:, blk * 512:(blk + 1) * 512]), [], [w.b])
                    for q4 in range(4):
                        p = ps[it % 2]
                        s = stg[it % 3]
                        it += 1
                        if j < 2:
                            h = hb2 * 4 + q4
                            for k in range(KC):
                                P.mm(p[:], w[:, k, q4 * 128:(q4 + 1) * 128], hT[:, k, :], k == 0, k == KC - 1, [w.b, hT.b], [p.b])
                        else:
                            for k in range(KC):
                                P.mm(p[:], hT[:, k, q4 * 128:(q4 + 1) * 128], w[:, k, :], k == 0, k == KC - 1, [w.b, hT.b], [p.b])
                        if it % 2 == 0:
                            P.act(s[:], p[:], AF.Copy, [p.b], [s.b])
                        else:
                            P.dve('tensor_copy', [p.b], [s.b], out=s[:], in_=p[:])
                        if j == 0:
                            P.dma('sp', QT[g, h, :, tok0 - 2048:tok0 - 2048 + TG], s[:], [s.b], [bQT])
                        elif j == 1:
                            P.dma('sp', KT[g, h, :, tok0:tok0 + TG], s[:], [s.b], [bKT])
                        else:
                            P.dma('sp', Vd[g, tok0 + q4 * 128:tok0 + (q4 + 1) * 128, hb2 * 512:(hb2 + 1) * 512], s[:], [s.b], [bV])
        with C.phase():
            ar = AttnRes(C)
            kT = C.sb([128, 4096], BF16, "kT")
            qT = C.sb([128, 2048], BF16, "qT")
            V1 = C.sb([128, 32, 129], BF16, "V1")
            F = C.sb([128, 1024], F32, "F")
            ostg = [C.sb([128, 129], F32, "ostg") for _ in range(4)]
            oi = [0]
            for g in range(3):
                dil = DIL[g]
                TPR = 32 // dil
                nsub = 4096 // dil
                QW = min(512, nsub // 2)
                for h in range(8):
                    P.dma('sp', kT[:], KT[g, h], [bKT], [kT.b])
                    P.dma('sp', qT[:], QT[g, h], [bQT], [qT.b])
                    P.dma('sp', F[:], Fd[g * 8 + h], [], [F.b])
                    P.dve('memset', [], [V1.b], V1[:, :, 128:129], 1.0)
                    vsrc = Vd[g].rearrange("(kt p dl) c -> dl p kt c", p=128, dl=dil)
                    for r in range(dil):
                        P.dma('sp', V1[:, r * TPR:(r + 1) * TPR, 0:128], vsrc[r][:, :, h * 128:(h + 1) * 128], [bV], [V1.b])
                    v4 = V1[:].rearrange("p (r t) c -> p r t c", r=dil)[:, :, 0:TPR // 2, :]
                    P.dve('tensor_scalar', [V1.b, ctxv.b], [V1.b], out=v4, in0=v4, scalar1=ctxv[:, 0:1], scalar2=None, op0=ALU.mult)
                    nd_dst = ND[g].rearrange("(m dl) h c -> dl m h c", dl=dil)
                    for r in range(dil):
                        for q0 in range(nsub // 2, nsub, QW):
                            c0 = q0 * dil + r - 2048
                            rhs = qT[:, c0:c0 + (QW - 1) * dil + 1:dil]
                            kts = []
                            for kt in range(max(0, q0 // 128 - 1), (q0 + QW) // 128):
                                o = (q0 - kt * 128) // 128
                                k0 = kt * 128 * dil + r
                                kts.append(dict(lhsT=[kT[:, k0:k0 + 127 * dil + 1:dil]], v=V1[:, r * TPR + kt, :],
                                                bias=F[:, 128 * (o + 3):128 * (o + 3) + QW]))

                            def cb(qc, j, po, g=g, h=h, r=r, q0=q0, nsub=nsub, nd_dst=nd_dst):
                                s = ostg[oi[0] % 4]
                                oi[0] += 1
                                P.act(s[:], po[:, 0:129], AF.Copy, [po.b], [s.b])
                                m0 = q0 + j * 128 - nsub // 2
                                P.dma('sp', nd_dst[r][m0:m0 + 128, h, :], s[:], [s.b], [bND])
                            emit_attn(P, ar, QW, 128 ** -0.5, [rhs], kts, [kT.b, qT.b, V1.b, F.b], cb, 0)
        with C.phase():
            wo = C.sb([128, 8, D], BF16, "wo")
            P.dma('pool', wo[:], w_out.rearrange("(kc p) n -> p kc n", p=128), [], [wo.b])
            nd = [C.sb([128, 8, 129], F32, "nd") for _ in range(3)]
            rec = C.sb([128, 8], F32, "rec")
            ob = C.sb([128, 8, 128], BF16, "ob")
            oT = C.sb([128, 8, TG], BF16, "oT")
            xr = [C.sb([128, D], F32, "xr") for _ in range(2)]
            tmp = [C.sb([128, 512], F32, "tmp") for _ in range(2)]
            ptr = C.ps([128, 1024], BF16, "ptr")
            py = [C.ps([128, 512], F32, "py") for _ in range(2)]
            it = 0
            for gi in range(4):
                for tt in range(4):
                    t0 = gi * TG + tt * 128
                    for g in range(3):
                        P.dma('sp', nd[g][:], ND[g, t0:t0 + 128], [bND], [nd[g].b])
                    P.dve('tensor_tensor', [nd[0].b, nd[1].b], [nd[0].b], out=nd[0][:], in0=nd[0][:], in1=nd[1][:], op=ALU.add)
                    P.dve('tensor_tensor', [nd[0].b, nd[2].b], [nd[0].b], out=nd[0][:], in0=nd[0][:], in1=nd[2][:], op=ALU.add)
                    P.dve('tensor_scalar', [nd[0].b], [rec.b], out=rec[:], in0=nd[0][:, :, 128], scalar1=1e-30, scalar2=None, op0=ALU.max)
                    P.dve('reciprocal', [rec.b], [rec.b], out=rec[:], in_=rec[:])
                    P.dve('tensor_tensor', [nd[0].b, rec.b], [ob.b], out=ob[:], in0=nd[0][:, :, 0:128], in1=rec[:].unsqueeze(2).to_broadcast([128, 8, 128]), op=ALU.mult)
                    for c in range(8):
                        P.tr(ptr[:, c * 128:(c + 1) * 128], ob[:, c, :], ident[:], [ob.b, ident.b], [ptr.b])
                    P.act(oT[:, :, tt * 128:(tt + 1) * 128], ptr[:].rearrange("p (c t) -> p c t", c=8), AF.Copy, [ptr.b], [oT.b])
                for tt in range(4):
                    t0 = gi * TG + tt * 128
                    x = xr[tt % 2]
                    P.dma('sp', x[:], xl[2048 + t0:2048 + t0 + 128, :], [], [x.b])
                    for nb in range(4):
                        p = py[it % 2]
                        tm = tmp[it % 2]
                        it += 1
                        sl = slice(nb * 512, (nb + 1) * 512)
                        for kc in range(8):
                            P.mm(p[:], oT[:, kc, tt * 128:(tt + 1) * 128], wo[:, kc, sl], kc == 0, kc == 7, [oT.b, wo.b], [p.b])
                        P.dve('tensor_tensor', [p.b, mods[2].b], [tm.b], out=tm[:], in0=p[:], in1=mods[2][:, sl], op=ALU.mult)
                        P.dve('tensor_tensor', [tm.b, x.b], [x.b], out=x[:, sl], in0=x[:, sl], in1=tm[:], op=ALU.add)
                    P.dma('sp', x1m[t0:t0 + 128, :], x[:], [x.b], [bX1])
        with C.phase():
            nb_ = norm_bufs(C)
            hT = C.sb([128, KC, TG], BF16, "hT")
            wr = C.sb([128, 8, D], F32, "wr")
            P.dma('sp', wr[:].rearrange("p e d -> p (e d)"), wrT.partition_broadcast(128), [], [wr.b])
            rj = C.sb([128, D], F32, "rj")
            rj2 = C.sb([128, D], F32, "rj2")
            lg = [C.sb([128, 8], F32, "lg") for _ in range(2)]
            mx = [C.sb([128, 8], F32, "mx") for _ in range(2)]
            ex = [C.sb([128, 8], F32, "ex") for _ in range(2)]
            sm = [C.sb([128, 2], F32, "sm") for _ in range(2)]
            for gi in range(4):
                def tile_cb(tt, x32, gi=gi):
                    l, m, e, s2 = lg[tt % 2], mx[tt % 2], ex[tt % 2], sm[tt % 2]
                    for ee in range(8):
                        P.dve('tensor_tensor', [x32.b, wr.b], [rj.b], out=rj[:], in0=x32[:], in1=wr[:, ee, :], op=ALU.mult)
                        P.act(rj2[:], rj[:], AF.Copy, [rj.b], [rj2.b, l.b], accum_out=l[:, ee:ee + 1])
                    P.dve('max', [l.b], [m.b], out=m[:], in_=l[:])
                    P.dve('tensor_scalar', [m.b], [s2.b], out=s2[:, 0:1], in0=m[:, 0:1], scalar1=-1.0, scalar2=None, op0=ALU.mult)
                    P.act(e[:], l[:], AF.Exp, [l.b, s2.b], [e.b], bias=s2[:, 0:1], scale=1.0)
                    P.dve('scalar_tensor_tensor', [l.b, m.b, e.b], [e.b], out=e[:], in0=l[:], scalar=m[:, 1:2], in1=e[:], op0=ALU.is_ge, op1=ALU.mult)
                    P.dve('tensor_reduce', [e.b], [s2.b], out=s2[:, 1:2], in_=e[:], axis=AX.X, op=ALU.add)
                    P.dve('reciprocal', [s2.b], [s2.b], out=s2[:, 1:2], in_=s2[:, 1:2])
                    P.dve('tensor_scalar', [e.b, s2.b], [e.b], out=e[:], in0=e[:], scalar1=s2[:, 1:2], scalar2=None, op0=ALU.mult)
                    t0 = gi * TG + tt * 128
                    P.dma('sp', gates_o[t0:t0 + 128, :], e[:], [e.b], [])
                emit_norm_T(C, P, nb_, x1m[gi * TG:(gi + 1) * TG, :], mods[4], mods[3], ident, hT, xdeps=[bX1], tile_cb=tile_cb)
                P.dma('sp', hTf[:, gi * TG:(gi + 1) * TG].rearrange("(kc p) t -> p kc t", p=128), hT[:], [hT.b], [])
        P.finish()
    return nc


NEG = -1e30


def t5_bucket_np(dist):
    n = np.maximum(dist, 0)
    nf = np.maximum(n, 1).astype(np.float32)
    large = 16 + (np.log(nf / np.float32(16)) / np.float32(np.log(2048 / 16)) * np.float32(16)).astype(np.int32)
    large = np.minimum(large, 31)
    return np.where(n < 16, n, large)


def toeplitz_table(rel_bias, heads, width, shift, dist_scale, max_back):
    k = np.arange(128)[:, None]
    j = np.arange(width)[None, :]
    d = j - k - shift
    ok = (d >= 0) & (d <= max_back)
    bk = t5_bucket_np(d * dist_scale)
    out = np.empty((len(heads), 128, width), np.float32)
    for i, h in enumerate(heads):
        out[i] = np.where(ok, rel_bias[bk, h], np.float32(NEG))
    return out


def make_Fd(rel_bias):
    return np.concatenate([toeplitz_table(rel_bias, range(8), 1024, 384, dil, 128) for dil in DIL], 0)


BIGM = 30000.0
PI = float(np.pi)


def build_l0(debug=False, upto=5):
    nc = bass.Bass("TRN2", target_bir_lowering=False)

    def din(name, shape, dtp=F32):
        return nc.dram_tensor(name, list(shape), dtp, kind="ExternalInput").ap()

    def dscr(name, shape, dtp):
        return nc.dram_tensor(name, list(shape), dtp, kind="ExternalOutput" if debug else "Internal").ap()
    xl = din("xl", [4096, D])
    modrow = din("modrow", [1, 6 * D])
    gn_m = din("gn_m", [1, D])
    gn_f = din("gn_f", [1, D])
    w_in = din("w_in", [D, 3672])
    w_out = din("w_out", [D, D])
    pos_kv = [din("pos_k", [32, 128]), din("pos_v", [32, 128])]
    w1_kv = [din("w1_k", [4096, 128]), din("w1_v", [4096, 128])]
    w2_kv = [din("w2_k", [128, 128]), din("w2_v", [128, 128])]
    qng = din("qng", [128, 4])
    kvng = din("kvng", [128, 4])
    w_uq = din("w_uq", [512, 1536])
    w_ukv = din("w_ukv", [512, 2048])
    if upto >= 5:
        wg = din("wg", [D, 5632])
        wu = din("wu", [D, 5632])
        wd = din("wd", [5632, D])
    posd = din("pos", [1, 4096], I32)
    invf = din("invf", [64, 2])
    Fwin = din("Fwin", [8, 128, 1408])
    Fslc = din("Fslc", [8, 128, 4864])
    Fmla = din("Fmla", [128, 1024])
    Fcmp = din("Fcmp", [8, 2, 128, 2048])
    vmul = din("vmul", [2048, 64])
    addm = din("addm", [2048, 64])
    Amat = din("Amat", [256, 64])
    Emat = din("Emat", [64, 4096])
    ctxvd = din("ctxv", [128, 1])
    identd = din("ident", [128, 128])
    x1 = nc.dram_tensor("x1", [2048, D], F32, kind="ExternalOutput").ap()
    QTn = dscr("QTn", [8, 128, 2048], BF16)
    KX = dscr("KX", [4, 2, 128, 4096], BF16)
    VX = dscr("VX", [2, 4096, 256], BF16)
    GT = dscr("GT", [2048, 24], F32)
    QN = dscr("QN", [8, 128, 2048], BF16)
    QR = dscr("QR", [8, 64, 2048], BF16)
    KN = dscr("KN", [8, 128, 4096], BF16)
    KR = dscr("KR", [64, 4096], BF16)
    VM = dscr("VM", [4096, 1024], BF16)
    KC_ = dscr("KCc", [2, 128, 256], BF16)
    VC_ = dscr("VCc", [2, 256, 128], BF16)
    OALL = dscr("OALL", [2048, D], BF16)
    X1A = dscr("X1A", [2048, D], F32)
    YF = dscr("YF", [2048, D], F32)
    bS = {k: Buf(k) for k in ("QTn", "KX", "VX", "GT", "QN", "QR", "KN", "KR", "VM", "KC", "VC", "OALL", "X1A", "YF")}
    P = Prog(nc)
    C = Ctx(nc, P)
    with ExitStack() as keep:
        def sbp(shape, dtp, name):
            C.n += 1
            return T(keep.enter_context(nc.sbuf_tensor(f"{name}_{C.n}", list(shape), dtp)), name)
        mods = [sbp([128, D], F32, f"mod{i}") for i in range(6)]
        ident = sbp([128, 128], BF16, "ident")
        identf = sbp([128, 128], F32, "identf")
        ctxv = sbp([128, 1], F32, "ctxv")
        ones = sbp([128, 128], F32, "ones")
        with C.phase():
            P.dma('sp', identf[:], identd, [], [identf.b])
            P.dve('tensor_copy', [identf.b], [ident.b], out=ident[:], in_=identf[:])
            P.dma('sp', ctxv[:], ctxvd, [], [ctxv.b])
            P.dve('memset', [], [ones.b], ones[:], 1.0)
            emit_mod_row(C, P, modrow, gn_m, gn_f, mods)

        with C.phase():
            nb_ = norm_bufs(C)
            hT = C.sb([128, KC, TG], BF16, "hT")
            wb = [C.sb([128, KC, 512], BF16, "wb") for _ in range(2)]
            ps = [C.ps([128, 512], F32, "psz") for _ in range(2)]
            pq = C.ps([128, 512], F32, "pq")
            pr = [C.ps([128, 512], F32, "pr") for _ in range(2)]
            stg = [C.sb([128, 512], BF16, "stg") for _ in range(3)]
            gst = [C.sb([128, 24], F32, "gst") for _ in range(2)]
            wuq = C.sb([128, 4, 1536], BF16, "wuq")
            wuqs = C.sb([128, 4, 8, 64], BF16, "wuqs")
            wkn = C.sb([128, 4, 8, 128], BF16, "wkn")
            wv = C.sb([128, 4, 8, 128], BF16, "wv")
            wpe = C.sb([128, KC, 128], BF16, "wpe")
            gq = C.sb([128, 4], F32, "gq")
            gkv = C.sb([128, 4], F32, "gkv")
            ivf = C.sb([64, 2], F32, "ivf")
            _lf = C.sb([128, 4, TG], F32, "latf")
            latf = [_lf, _lf]
            sq = C.sb([128, 4, TG], F32, "sq")
            latb = [C.sb([128, 4, TG], BF16, "latb") for _ in range(2)]
            rbc = [C.sb([128, TG], F32, "rbc") for _ in range(2)]
            rtm = C.sb([128, 4], F32, "rtm")
            posi = C.sb([128, TG], I32, "posi")
            ang = C.sb([64, TG], F32, "ang")
            CC = C.sb([64, TG], F32, "CC")
            SS = C.sb([64, TG], F32, "SS")
            rA = C.sb([64, TG], F32, "rA")
            rB = C.sb([64, TG], F32, "rB")
            P.dma('pool', wuq[:], w_uq.rearrange("(kc p) n -> p kc n", p=128), [], [wuq.b])
            uq4 = w_uq.rearrange("(kc p) (h c) -> p kc h c", p=128, c=192)
            ukv5 = w_ukv.rearrange("(kc p) (h two d) -> p kc h two d", p=128, two=2, d=128)
            for c4 in range(4):
                P.dma('pool', wuqs[:, c4, :, 0:32], uq4[:, c4, :, 160:192], [], [wuqs.b])
                P.dma('pool', wuqs[:, c4, :, 32:64], uq4[:, c4, :, 128:160], [], [wuqs.b])
                P.dma('pool', wkn[:, c4], ukv5[:, c4, :, 0, :], [], [wkn.b])
                P.dma('pool', wv[:, c4], ukv5[:, c4, :, 1, :], [], [wv.b])
            P.dma('pool', wpe[:, :, 0:64], rearr_w(w_in[:, 3608:3672]), [], [wpe.b])
            P.dma('pool', wpe[:, :, 64:96], rearr_w(w_in[:, 3640:3672]), [], [wpe.b])
            P.dma('pool', wpe[:, :, 96:128], rearr_w(w_in[:, 3608:3640]), [], [wpe.b])
            P.dma('sp', gq[:], qng, [], [gq.b])
            P.dma('sp', gkv[:], kvng, [], [gkv.b])
            P.dma('sp', ivf[:], invf, [], [ivf.b])
            it = 0

            def evac(p, s, M=128, W=TG):
                nonlocal it
                it += 1
                if it % 2 == 0:
                    P.act(s[0:M, 0:W], p[0:M, 0:W], AF.Copy, [p.b], [s.b])
                else:
                    P.dve('tensor_copy', [p.b], [s.b], out=s[0:M, 0:W], in_=p[0:M, 0:W])

            for gi in range(8):
                tok0 = gi * TG
                own = gi >= 4
                emit_norm_T(C, P, nb_, xl[tok0:tok0 + TG, :], mods[1], mods[0], ident, hT)
                P.dma('sp', posi[:], posd[:, tok0:tok0 + TG].partition_broadcast(128), [], [posi.b])
                P.dve('tensor_copy', [posi.b], [ang.b], out=ang[:], in_=posi[0:64, :])
                P.dve('tensor_scalar', [ang.b, ivf.b], [ang.b], out=ang[:], in0=ang[:], scalar1=ivf[:, 0:1], scalar2=None, op0=ALU.mult)
                for dst, shift in ((SS, 0.0), (CC, 0.5 * PI)):
                    P.dve('tensor_scalar', [ang.b], [dst.b], out=dst[:], in0=ang[:], scalar1=shift, scalar2=1.0 / (2 * PI), op0=ALU.add, op1=ALU.mult)
                    P.dve('tensor_copy', [dst.b], [posi.b], out=posi[0:64, :], in_=dst[:])
                    P.dve('tensor_copy', [posi.b], [rA.b], out=rA[:], in_=posi[0:64, :])
                    P.dve('tensor_scalar', [ang.b], [dst.b], out=dst[:], in0=ang[:], scalar1=shift, scalar2=None, op0=ALU.add)
                    P.dve('scalar_tensor_tensor', [rA.b, dst.b], [dst.b], out=dst[:], in0=rA[:], scalar=-2 * PI, in1=dst[:], op0=ALU.mult, op1=ALU.add)
                    P.dve('tensor_scalar', [dst.b], [dst.b], out=dst[:], in0=dst[:], scalar1=PI, scalar2=-PI, op0=ALU.min, op1=ALU.max)
                    P.act(dst[:], dst[:], AF.Sin, [dst.b], [dst.b])
                P.dve('tensor_scalar', [SS.b, ivf.b], [SS.b], out=SS[:], in0=SS[:], scalar1=ivf[:, 1:2], scalar2=None, op0=ALU.mult)
                for blk in range(5):
                    c0 = blk * 512
                    ncols = min(512, 3672 - c0)
                    if blk < 2 and not own:
                        continue
                    w = wb[blk % 2]
                    P.dma('pool', w[:, :, 0:ncols], rearr_w(w_in[:, c0:c0 + ncols]), [], [w.b])

                    def fm(m0, M, p):
                        for k in range(KC):
                            P.mm(p[0:M, :], w[:, k, m0:m0 + M], hT[:, k, :], k == 0, k == KC - 1, [w.b, hT.b], [p.b])
                    if blk < 2:
                        for q4 in range(4):
                            p, s = ps[it % 2], stg[it % 3]
                            fm(q4 * 128, 128, p)
                            evac(p, s)
                            P.dma('sp', QTn[blk * 4 + q4, :, tok0 - 2048:tok0 - 2048 + TG], s[:], [s.b], [bS["QTn"]])
                    elif blk == 2:
                        for q4 in range(4):
                            p, s = ps[it % 2], stg[it % 3]
                            fm(q4 * 128, 128, p)
                            evac(p, s)
                            P.dma('sp', KX[q4 // 2, q4 % 2, :, tok0:tok0 + TG], s[:], [s.b], [bS["KX"]])
                    elif blk in (3, 4):
                        kind = blk - 3
                        for g in range(2):
                            p, s = ps[it % 2], stg[it % 3]
                            fm(g * 128, 128, p)
                            evac(p, s)
                            P.dma('sp', KX[2 + kind, g, :, tok0:tok0 + TG], s[:], [s.b], [bS["KX"]])
                        for tt in range(4):
                            p, s = ps[it % 2], stg[it % 3]
                            for k in range(KC):
                                P.mm(p[:, 0:256], hT[:, k, tt * 128:(tt + 1) * 128], w[:, k, 256:512], k == 0, k == KC - 1, [w.b, hT.b], [p.b])
                            evac(p, s, 128, 256)
                            P.dma('sp', VX[kind, tok0 + tt * 128:tok0 + (tt + 1) * 128, :], s[:, 0:256], [s.b], [bS["VX"]])
                if own:
                    w = wb[0]
                    P.dma('pool', w[:, :, 0:24], rearr_w(w_in[:, 2560:2584]), [], [w.b])
                    for tt in range(4):
                        p, gs = ps[it % 2], gst[tt % 2]
                        it += 1
                        for k in range(KC):
                            P.mm(p[:, 0:24], hT[:, k, tt * 128:(tt + 1) * 128], w[:, k, 0:24], k == 0, k == KC - 1, [w.b, hT.b], [p.b])
                        P.act(gs[:], p[:, 0:24], AF.Sigmoid, [p.b], [gs.b])
                        t0 = tok0 - 2048 + tt * 128
                        P.dma('sp', GT[t0:t0 + 128, :], gs[:], [gs.b], [bS["GT"]])
                for li in range(2):
                    if li == 0 and not own:
                        continue
                    w = wb[1 - li]
                    cb0 = 2584 + li * 512
                    P.dma('pool', w[:], rearr_w(w_in[:, cb0:cb0 + 512]), [], [w.b])
                    lf, lb, gg, rb_ = latf[li], latb[li], (gq if li == 0 else gkv), rbc[li]
                    for c4 in range(4):
                        p = ps[it % 2]
                        it += 1
                        for k in range(KC):
                            P.mm(p[:], w[:, k, c4 * 128:(c4 + 1) * 128], hT[:, k, :], k == 0, k == KC - 1, [w.b, hT.b], [p.b])
                        P.act(lf[:, c4, :], p[:], AF.Copy, [p.b], [lf.b])
                    P.act(sq[:], lf[:], AF.Square, [lf.b], [sq.b])
                    for c4 in range(4):
                        P.mm(pq[:], ones[:], sq[:, c4, :], c4 == 0, c4 == 3, [ones.b, sq.b], [pq.b])
                    P.dve('tensor_scalar', [pq.b], [rb_.b], out=rb_[:], in0=pq[:], scalar1=1.0 / 512, scalar2=EPS, op0=ALU.mult, op1=ALU.add)
                    P.act(rb_[:], rb_[:], AF.Sqrt, [rb_.b], [rb_.b])
                    P.dve('reciprocal', [rb_.b], [rb_.b], out=rb_[:], in_=rb_[:])
                    for c4 in range(4):
                        P.dve('tensor_scalar', [lf.b, gg.b], [lb.b], out=lb[:, c4, :], in0=lf[:, c4, :], scalar1=gg[:, c4:c4 + 1], scalar2=None, op0=ALU.mult)
                    if li == 1:
                        for tt in range(4):
                            p = ps[it % 2]
                            it += 1
                            for c4 in range(4):
                                P.mm(p[:, 0:8], sq[:, c4, tt * 128:(tt + 1) * 128], ones[:, 0:8], c4 == 0, c4 == 3, [sq.b, ones.b], [p.b])
                            P.dve('tensor_scalar', [p.b], [rtm.b], out=rtm[:, tt:tt + 1], in0=p[:, 0:1], scalar1=1.0 / 512, scalar2=EPS, op0=ALU.mult, op1=ALU.add)
                        P.act(rtm[:], rtm[:], AF.Sqrt, [rtm.b], [rtm.b])
                        P.dve('reciprocal', [rtm.b], [rtm.b], out=rtm[:], in_=rtm[:])

                def rope(pa, pb, mul, s):
                    P.dve('tensor_tensor', [pa.b, CC.b], [rA.b], out=rA[:], in0=pa[0:64, :], in1=CC[:], op=ALU.mult)
                    P.dve('tensor_tensor', [pb.b, SS.b], [rB.b], out=rB[:], in0=pb[0:64, :], in1=SS[:], op=ALU.mult)
                    if mul is None:
                        P.dve('tensor_tensor', [rA.b, rB.b], [s.b], out=s[0:64, :], in0=rA[:], in1=rB[:], op=ALU.add)
                    else:
                        P.dve('tensor_tensor', [rA.b, rB.b], [rA.b], out=rA[:], in0=rA[:], in1=rB[:], op=ALU.add)
                        P.dve('tensor_tensor', [rA.b, mul.b], [s.b], out=s[0:64, :], in0=rA[:], in1=mul[0:64, :], op=ALU.mult)
                for k in range(KC):
                    P.mm(pr[0][0:64, :], wpe[:, k, 0:64], hT[:, k, :], k == 0, k == KC - 1, [wpe.b, hT.b], [pr[0].b])
                for k in range(KC):
                    P.mm(pr[1][0:64, :], wpe[:, k, 64:128], hT[:, k, :], k == 0, k == KC - 1, [wpe.b, hT.b], [pr[1].b])
                s = stg[it % 3]
                it += 1
                rope(pr[0], pr[1], None, s)
                P.dma('sp', KR[:, tok0:tok0 + TG], s[0:64, :], [s.b], [bS["KR"]])
                if own:
                    lb = latb[0]
                    for h in range(8):
                        p, s = ps[it % 2], stg[it % 3]
                        it += 1
                        for c4 in range(4):
                            P.mm(p[:], wuq[:, c4, h * 192:h * 192 + 128], lb[:, c4, :], c4 == 0, c4 == 3, [wuq.b, lb.b], [p.b])
                        P.dve('tensor_tensor', [p.b, rbc[0].b], [s.b], out=s[:], in0=p[:], in1=rbc[0][:], op=ALU.mult)
                        P.dma('sp', QN[h, :, tok0 - 2048:tok0 - 2048 + TG], s[:], [s.b], [bS["QN"]])
                        for c4 in range(4):
                            P.mm(pr[0][0:64, :], wuq[:, c4, h * 192 + 128:h * 192 + 192], lb[:, c4, :], c4 == 0, c4 == 3, [wuq.b, lb.b], [pr[0].b])
                        for c4 in range(4):
                            P.mm(pr[1][0:64, :], wuqs[:, c4, h, :], lb[:, c4, :], c4 == 0, c4 == 3, [wuqs.b, lb.b], [pr[1].b])
                        s = stg[it % 3]
                        it += 1
                        rope(pr[0], pr[1], rbc[0], s)
                        P.dma('sp', QR[h, :, tok0 - 2048:tok0 - 2048 + TG], s[0:64, :], [s.b], [bS["QR"]])
                lb = latb[1]
                for h in range(8):
                    p, s = ps[it % 2], stg[it % 3]
                    it += 1
                    for c4 in range(4):
                        P.mm(p[:], wkn[:, c4, h, :], lb[:, c4, :], c4 == 0, c4 == 3, [wkn.b, lb.b], [p.b])
                    P.dve('tensor_tensor', [p.b, rbc[1].b], [s.b], out=s[:], in0=p[:], in1=rbc[1][:], op=ALU.mult)
                    P.dma('sp', KN[h, :, tok0:tok0 + TG], s[:], [s.b], [bS["KN"]])
                for tt in range(4):
                    for hb2 in range(2):
                        p, s = ps[it % 2], stg[it % 3]
                        it += 1
                        for c4 in range(4):
                            P.mm(p[:], lb[:, c4, tt * 128:(tt + 1) * 128], wv[:, c4, hb2 * 4:(hb2 + 1) * 4, :].rearrange("p h d -> p (h d)"), c4 == 0, c4 == 3, [wv.b, lb.b], [p.b])
                        P.act(s[:], p[:], AF.Copy, [p.b, rtm.b], [s.b], scale=rtm[:, tt:tt + 1])
                        P.dma('sp', VM[tok0 + tt * 128:tok0 + (tt + 1) * 128, hb2 * 512:(hb2 + 1) * 512], s[:], [s.b], [bS["VM"]])
        build_l0_rest(nc, C, P, locals())
        P.finish()
    return nc


def build_l0_rest(nc, C, P, L):
    KX, VX, GT, QTn, QN, QR, KN, KR, VM = (L[k] for k in ("KX", "VX", "GT", "QTn", "QN", "QR", "KN", "KR", "VM"))
    KC_, VC_, OALL, X1A, YF, bS = (L[k] for k in ("KC_", "VC_", "OALL", "X1A", "YF", "bS"))
    ident, identf, ctxv, mods, xl = L["ident"], L["identf"], L["ctxv"], L["mods"], L["xl"]
    upto = L["upto"]
    if upto < 2:
        return
    with C.phase():
        kT = C.sb([128, 4096], BF16, "kT")
        w1 = C.sb([128, 32, 128], BF16, "w1")
        w2 = C.sb([128, 128], BF16, "w2")
        posq = C.sb([32, 128], F32, "posq")
        posb = C.sb([128, 32, 8], BF16, "posb")
        bia = C.sb([128, 1], F32, "bia")
        u = C.sb([128, 256], F32, "u")
        t1 = C.sb([128, 256], F32, "t1")
        hid = C.sb([128, 256], BF16, "hid")
        so = [C.sb([128, 256], BF16, "so") for _ in range(2)]
        ph = C.ps([128, 256], F32, "ph")
        pb = C.ps([128, 64], F32, "pb")
        po = [C.ps([128, 256], F32, "po") for _ in range(2)]
        for kind in range(2):
            P.dma('pool', w1[:], L["w1_kv"][kind].rearrange("(l d) e -> d l e", d=128), [], [w1.b])
            P.dma('pool', w2[:], L["w2_kv"][kind], [], [w2.b])
            P.dma('sp', posq[:], L["pos_kv"][kind], [], [posq.b])
            P.tr(pb[:, 0:32], posq[:, :], identf[0:32, 0:32], [posq.b, identf.b], [pb.b])
            P.dve('tensor_copy', [pb.b], [posb.b], out=posb[:], in_=pb[:, 0:32].unsqueeze(2).to_broadcast([128, 32, 8]))
            for l in range(32):
                P.mm(pb[:, 32:40], w1[:, l, :], posb[:, l, :], l == 0, l == 31, [w1.b, posb.b], [pb.b])
            P.act(bia[:], pb[:, 32:33], AF.Copy, [pb.b], [bia.b])
            for g in range(2):
                P.dma('sp', kT[:], KX[kind, g], [bS["KX"]], [kT.b])
                for l in range(32):
                    P.mm(ph[:, 0:255], w1[:, l, :], kT[:, l:l + 16 * 254 + 1:16], l == 0, l == 31, [w1.b, kT.b], [ph.b])
                P.dve('memset', [], [u.b], u[:], 0.0)
                P.dve('tensor_scalar', [ph.b, bia.b], [u.b], out=u[:, 0:255], in0=ph[:, 0:255], scalar1=bia[:, 0:1], scalar2=None, op0=ALU.add)
                P.dve('tensor_tensor', [u.b], [t1.b], out=t1[:], in0=u[:], in1=u[:], op=ALU.mult)
                P.dve('tensor_scalar', [t1.b], [t1.b], out=t1[:], in0=t1[:], scalar1=0.044715, scalar2=1.0, op0=ALU.mult, op1=ALU.add)
                P.dve('tensor_tensor', [t1.b, u.b], [t1.b], out=t1[:], in0=t1[:], in1=u[:], op=ALU.mult)
                P.act(t1[:], t1[:], AF.Tanh, [t1.b], [t1.b], scale=0.7978845608028654)
                P.dve('tensor_scalar', [t1.b], [t1.b], out=t1[:], in0=t1[:], scalar1=1.0, scalar2=0.5, op0=ALU.add, op1=ALU.mult)
                P.dve('tensor_tensor', [t1.b, u.b], [hid.b], out=hid[:], in0=t1[:], in1=u[:], op=ALU.mult)
                if kind == 0:
                    p, s_ = po[0], so[0]
                    P.mm(p[:], w2[:], hid[:], True, True, [w2.b, hid.b], [p.b])
                    P.act(s_[:], p[:], AF.Copy, [p.b], [s_.b])
                    P.dma('sp', KC_[g], s_[:], [s_.b], [bS["KC"]])
                else:
                    for nt in range(2):
                        p, s_ = po[nt], so[nt]
                        P.mm(p[:, 0:128], hid[:, nt * 128:(nt + 1) * 128], w2[:], True, True, [w2.b, hid.b], [p.b])
                        P.act(s_[:, 0:128], p[:, 0:128], AF.Copy, [p.b], [s_.b])
                        P.dma('sp', VC_[g, nt * 128:(nt + 1) * 128, :], s_[:, 0:128], [s_.b], [bS["VC"]])

    if upto < 3:
        return
    with C.phase():
        ar = AttnRes(C)
        kT = C.sb([128, 4096], BF16, "kT")
        kR = C.sb([64, 4096], BF16, "kR")
        qT = C.sb([128, 2048], BF16, "qT")
        qR = C.sb([64, 2048], BF16, "qR")
        V1 = C.sb([128, 32, 129], BF16, "V1")
        kc = C.sb([128, 256], BF16, "kc")
        V1c = C.sb([128, 2, 193], BF16, "V1c")
        amf = C.sb([128, 2, 64], F32, "amf")
        Fb = C.sb([128, 4864], F32, "Fb")
        Fm = C.sb([128, 1024], F32, "Fm")
        Em = C.sb([64, 4096], BF16, "Em")
        gt = C.sb([128, 16, 24], F32, "gt")
        acc = [C.sb([128, 16, 128], F32, "acc") for _ in range(4)]
        imp = C.sb([128, 16, 64], F32, "imp")
        negT = C.sb([64, 2048], BF16, "negT")
        rec = [C.sb([128, 2], F32, "rec") for _ in range(4)]
        vm_t = C.sb([128, 64], F32, "vm_t")
        am_t = C.sb([128, 64], F32, "am_t")
        sc_t = C.sb([128, 64], F32, "sc_t")
        sc2 = C.sb([128, 64], F32, "sc2")
        m8 = C.sb([128, 16], F32, "m8")
        ngb = C.sb([128, 64], BF16, "ngb")
        ob = [C.sb([128, 128], BF16, "ob") for _ in range(2)]
        ptn = C.ps([64, 128], BF16, "ptn")
        P.dma('pool', Em[:], L["Emat"], [], [Em.b])
        P.dma('sp', Fm[:], L["Fmla"], [], [Fm.b])
        P.dma('sp', gt[:], GT.rearrange("(t p) c -> p t c", p=128), [bS["GT"]], [gt.b])
        P.dma('sp', amf[:], L["Amat"].rearrange("(t p) c -> p t c", p=128), [], [amf.b])
        R_all = [kT.b, qT.b, V1.b, Fb.b, kc.b, V1c.b, negT.b, Em.b, kR.b, qR.b, Fm.b]
        ri = [0]
        oi = [0]

        def load_v(src_cols):
            P.dve('memset', [], [V1.b], V1[:, :, 128:129], 1.0)
            P.dma('sp', V1[:, :, 0:128], src_cols.rearrange("(kt p) d -> p kt d", p=128), [bS["VX"], bS["VM"]], [V1.b])
            P.dve('tensor_scalar', [V1.b, ctxv.b], [V1.b], out=V1[:, 0:16, :], in0=V1[:, 0:16, :], scalar1=ctxv[:, 0:1], scalar2=None, op0=ALU.mult)

        def branch_cb(a, gcol, first):
            def cb(qc, j, po):
                qt = qc * 4 + j
                r = rec[ri[0] % 4]
                ri[0] += 1
                P.dve('tensor_scalar', [po.b], [r.b], out=r[:, 0:1], in0=po[:, 128:129], scalar1=1e-30, scalar2=None, op0=ALU.max)
                P.dve('reciprocal', [r.b], [r.b], out=r[:, 0:1], in_=r[:, 0:1])
                if gcol is not None:
                    P.dve('tensor_tensor', [r.b, gt.b], [r.b], out=r[:, 1:2], in0=r[:, 0:1], in1=gt[:, qt, gcol:gcol + 1], op=ALU.mult)
                    cf = r[:, 1:2]
                else:
                    cf = r[:, 0:1]
                if first:
                    P.dve('tensor_scalar', [po.b, r.b], [a.b], out=a[:, qt, :], in0=po[:, 0:128], scalar1=cf, scalar2=None, op0=ALU.mult)
                else:
                    P.dve('scalar_tensor_tensor', [po.b, r.b, a.b], [a.b], out=a[:, qt, :], in0=po[:, 0:128], scalar=cf, in1=a[:, qt, :], op0=ALU.mult, op1=ALU.add)
                return r
            return cb

        def store_head(a, col0):
            for qt in range(16):
                o = ob[oi[0] % 2]
                oi[0] += 1
                P.act(o[:], a[:, qt, :], AF.Copy, [a.b], [o.b])
                P.dma('sp', OALL[qt * 128:(qt + 1) * 128, col0:col0 + 128], o[:], [o.b], [bS["OALL"]])

        for g in range(2):
            P.dma('sp', kc[:], KC_[g], [bS["KC"]], [kc.b])
            P.dve('memset', [], [V1c.b], V1c[:, :, 128:129], 1.0)
            P.dma('sp', V1c[:, :, 0:128], VC_[g].rearrange("(t p) d -> p t d", p=128), [bS["VC"]], [V1c.b])
            P.dve('tensor_copy', [amf.b], [V1c.b], out=V1c[:, :, 129:193], in_=amf[:])
            P.dve('tensor_scalar', [V1c.b, ctxv.b], [V1c.b], out=V1c[:, 0, :], in0=V1c[:, 0, :], scalar1=ctxv[:, 0:1], scalar2=None, op0=ALU.mult)
            for r4 in range(4):
                h = g * 4 + r4
                a = acc[r4]
                P.dma('sp', qT[:], QTn[h], [bS["QTn"]], [qT.b])
                P.dma('sp', Fb[:, 0:4096].rearrange("p (t q) -> p t q", t=2), L["Fcmp"][h].rearrange("t p q -> p t q"), [], [Fb.b])
                base_cb = branch_cb(a, h * 3 + 0, True)

                def cb(qc, j, po, base_cb=base_cb, r4=r4):
                    r = base_cb(qc, j, po)
                    qt = qc * 4 + j
                    if r4 == 0:
                        P.dve('tensor_scalar', [po.b, r.b], [imp.b], out=imp[:, qt, :], in0=po[:, 129:193], scalar1=r[:, 0:1], scalar2=None, op0=ALU.mult)
                    else:
                        P.dve('scalar_tensor_tensor', [po.b, r.b, imp.b], [imp.b], out=imp[:, qt, :], in0=po[:, 129:193], scalar=r[:, 0:1], in1=imp[:, qt, :], op0=ALU.mult, op1=ALU.add)
                for qc in range(4):
                    kts = [dict(lhsT=[kc[:, nt * 128:(nt + 1) * 128]], v=V1c[:, nt, :], bias=Fb[:, nt * 2048 + qc * 512:nt * 2048 + (qc + 1) * 512]) for nt in range(2)]
                    emit_attn(P, ar, 512, 128 ** -0.5, [qT[:, qc * 512:(qc + 1) * 512]], kts, R_all, cb, qc, VW=193)
            for qt in range(16):
                P.dma('sp', vm_t[:], L["vmul"][qt * 128:(qt + 1) * 128, :], [], [vm_t.b])
                P.dma('sp', am_t[:], L["addm"][qt * 128:(qt + 1) * 128, :], [], [am_t.b])
                P.dve('tensor_tensor', [imp.b, vm_t.b], [sc_t.b], out=sc_t[:], in0=imp[:, qt, :], in1=vm_t[:], op=ALU.mult)
                P.dve('tensor_tensor', [sc_t.b, am_t.b], [sc_t.b], out=sc_t[:], in0=sc_t[:], in1=am_t[:], op=ALU.add)
                P.dve('max', [sc_t.b], [m8.b], out=m8[:, 0:8], in_=sc_t[:])
                P.dve('match_replace', [sc_t.b, m8.b], [sc2.b], out=sc2[:], in_to_replace=m8[:, 0:8], in_values=sc_t[:], imm_value=-2.0)
                P.dve('max', [sc2.b], [m8.b], out=m8[:, 8:16], in_=sc2[:])
                P.dve('tensor_scalar', [sc_t.b, m8.b], [sc2.b], out=sc2[:], in0=sc_t[:], scalar1=m8[:, 15:16], scalar2=None, op0=ALU.is_ge)
                P.dve('scalar_tensor_tensor', [sc_t.b, sc2.b], [sc2.b], out=sc2[:], in0=sc_t[:], scalar=-0.5, in1=sc2[:], op0=ALU.is_gt, op1=ALU.mult)
                P.dve('tensor_scalar', [sc2.b], [ngb.b], out=ngb[:], in0=sc2[:], scalar1=-1.0, scalar2=BIGM, op0=ALU.add, op1=ALU.mult)
                P.tr(ptn[:, :], ngb[:, :], ident[:], [ngb.b, ident.b], [ptn.b])
                P.act(negT[:, qt * 128:(qt + 1) * 128], ptn[:, :], AF.Copy, [ptn.b], [negT.b])
            for r4 in range(4):
                h = g * 4 + r4
                a = acc[r4]
                P.dma('sp', qT[:], QTn[h], [bS["QTn"]], [qT.b])
                for br in range(2):
                    P.dma('sp', kT[:], KX[2 + br, g], [bS["KX"]], [kT.b])
                    load_v(VX[br][:, g * 128:(g + 1) * 128])
                    if br == 0:
                        P.dma('sp', Fb[:], L["Fslc"][h], [], [Fb.b])
                    else:
                        P.dma('sp', Fb[:, 0:1408], L["Fwin"][h], [], [Fb.b])
                    cb = branch_cb(a, h * 3 + 1 + br, False)
                    for qc in range(4):
                        q0 = 2048 + qc * 512
                        lo = 0 if br == 0 else (q0 - 512) // 128
                        kts = []
                        for kt in range(lo, (q0 + 512) // 128):
                            o = (q0 - kt * 128) // 128
                            d = dict(lhsT=[kT[:, kt * 128:(kt + 1) * 128]], v=V1[:, kt, :], bias=Fb[:, 128 * (o + 3):128 * (o + 3) + 512])
                            if br == 0:
                                d['extra'] = [(Em[:, kt * 128:(kt + 1) * 128], negT[:, qc * 512:(qc + 1) * 512])]
                            kts.append(d)
                        emit_attn(P, ar, 512, 128 ** -0.5, [qT[:, qc * 512:(qc + 1) * 512]], kts, R_all, cb, qc)
                store_head(a, h * 128)
        P.dma('sp', kR[:], KR, [bS["KR"]], [kR.b])
        for h in range(8):
            a = acc[h % 4]
            P.dma('sp', kT[:], KN[h], [bS["KN"]], [kT.b])
            P.dma('sp', qT[:], QN[h], [bS["QN"]], [qT.b])
            P.dma('sp', qR[:], QR[h], [bS["QR"]], [qR.b])
            load_v(VM[:, h * 128:(h + 1) * 128])
            cb = branch_cb(a, None, True)
            for qc in range(4):
                q0 = 2048 + qc * 512
                kts = []
                for kt in range(0, (q0 + 512) // 128):
                    o = (q0 - kt * 128) // 128
                    kts.append(dict(lhsT=[kT[:, kt * 128:(kt + 1) * 128], kR[:, kt * 128:(kt + 1) * 128]], v=V1[:, kt, :],
                                    bias=(Fm[:, 128 * (o + 3):128 * (o + 3) + 512] if o <= 0 else None)))
                emit_attn(P, ar, 512, 192 ** -0.5, [qT[:, qc * 512:(qc + 1) * 512], qR[:, qc * 512:(qc + 1) * 512]], kts, R_all, cb, qc)
            store_head(a, 1024 + h * 128)

    if upto < 4:
        return
    with C.phase():
        wo = C.sb([128, KC, D], BF16, "wo")
        P.dma('pool', wo[:], L["w_out"].rearrange("(kc p) n -> p kc n", p=128), [], [wo.b])
        ot = [C.sb([128, D], BF16, "ot") for _ in range(2)]
        oT = C.sb([128, KC, TG], BF16, "oT")
        xr = [C.sb([128, D], F32, "xr") for _ in range(2)]
        tmp = [C.sb([128, 512], F32, "tmp") for _ in range(2)]
        ptr = [C.ps([128, 1024], BF16, "ptr") for _ in range(2)]
        py = [C.ps([128, 512], F32, "py") for _ in range(2)]
        it = 0
        for gi in range(4):
            for tt in range(4):
                t0 = gi * TG + tt * 128
                o = ot[tt % 2]
                P.dma('sp', o[:], OALL[t0:t0 + 128, :], [bS["OALL"]], [o.b])
                for half in range(2):
                    pt = ptr[half]
                    for c in range(8):
                        cc = half * 8 + c
                        P.tr(pt[:, c * 128:(c + 1) * 128], o[:, cc * 128:(cc + 1) * 128], ident[:], [o.b, ident.b], [pt.b])
                    P.act(oT[:, half * 8:(half + 1) * 8, tt * 128:(tt + 1) * 128], pt[:].rearrange("p (c t) -> p c t", c=8), AF.Copy, [pt.b], [oT.b])
            for tt in range(4):
                t0 = gi * TG + tt * 128
                x = xr[tt % 2]
                P.dma('sp', x[:], xl[2048 + t0:2048 + t0 + 128, :], [], [x.b])
                for nb in range(4):
                    p, tm = py[it % 2], tmp[it % 2]
                    it += 1
                    sl = slice(nb * 512, (nb + 1) * 512)
                    for kc_ in range(KC):
                        P.mm(p[:], oT[:, kc_, tt * 128:(tt + 1) * 128], wo[:, kc_, sl], kc_ == 0, kc_ == KC - 1, [oT.b, wo.b], [p.b])
                    P.dve('tensor_tensor', [p.b, mods[2].b], [tm.b], out=tm[:], in0=p[:], in1=mods[2][:, sl], op=ALU.mult)
                    P.dve('tensor_tensor', [tm.b, x.b], [x.b], out=x[:, sl], in0=x[:, sl], in1=tm[:], op=ALU.add)
                P.dma('sp', X1A[t0:t0 + 128, :], x[:], [x.b], [bS["X1A"]])

    if upto < 5:
        return
    with C.phase():
        xb0 = C.sb([128, D], F32, "xb")
        xb_ = [xb0, xb0]
        hb0 = C.sb([128, D], BF16, "hb")
        st_ = C.sb([128, 4], F32, "st")
        pst_ = [C.ps([128, 1024], BF16, "pst") for _ in range(2)]
        nb_ = (xb_, [hb0, hb0], hb0, st_, pst_)

        def loader(g, hT):
            emit_norm_T(C, P, nb_, X1A[g * TG:(g + 1) * TG, :], mods[4], mods[3], ident, hT, xdeps=[bS["X1A"]])
        emit_swiglu(C, P, loader, 4, L["wg"], L["wu"], L["wd"], 44, None, YF, bS["YF"], nwd=2)

    with C.phase():
        xa = [C.sb([128, D], F32, "xa") for _ in range(2)]
        yy = [C.sb([128, D], F32, "yy") for _ in range(2)]
        for t in range(16):
            a, y = xa[t % 2], yy[t % 2]
            P.dma('sp', a[:], X1A[t * 128:(t + 1) * 128, :], [bS["X1A"]], [a.b])
            P.dma('sp', y[:], YF[t * 128:(t + 1) * 128, :], [bS["YF"]], [y.b])
            P.dve('tensor_tensor', [y.b, mods[5].b], [y.b], out=y[:], in0=y[:], in1=mods[5][:], op=ALU.mult)
            P.dve('tensor_tensor', [y.b, a.b], [a.b], out=a[:], in0=a[:], in1=y[:], op=ALU.add)
            P.dma('sp', L["x1"][t * 128:(t + 1) * 128, :], a[:], [a.b], [])


def l0_tables(rel_bias, s):
    rb = np.asarray(rel_bias, np.float32)
    t = {}
    t["Fwin"] = toeplitz_table(rb, range(8), 1408, 384, 1, 511)
    t["Fslc"] = toeplitz_table(rb, range(8), 4864, 384, 1, 10 ** 9)
    k = np.arange(128)[:, None]
    j = np.arange(1024)[None, :]
    t["Fmla"] = np.where(j - k - 384 >= 0, np.float32(0), np.float32(NEG)).astype(np.float32)
    n = (np.arange(2)[:, None, None] * 128 + np.arange(128)[None, :, None])
    tl = 2048 + np.arange(2048)[None, None, :]
    rel = tl - (16 * n + 31)
    ok = (rel >= 0) & (n <= 254)
    bk = t5_bucket_np(rel)
    t["Fcmp"] = np.stack([np.where(ok, rb[bk, h], np.float32(NEG)) for h in range(8)], 0).astype(np.float32)
    q = np.arange(2048)[:, None]
    jb = np.arange(64)[None, :]
    blk_t = (2048 + q) // 64
    first = 0 if s == 1 else 32
    valid = (jb <= blk_t) & (jb >= first)
    forced = valid & ((jb == first) | (jb == blk_t) | (jb == blk_t - 1))
    t["vmul"] = (valid & ~forced).astype(np.float32)
    fval = np.where(jb == first, np.float32(3e9), np.where(jb == blk_t, np.float32(2e9), np.float32(1e9)))
    t["addm"] = np.where(forced, fval, np.where(valid, np.float32(0), np.float32(-1))).astype(np.float32)
    A = np.zeros((256, 64), np.float32)
    for jj in range(64):
        for m in range(4):
            for n2 in range(2):
                i = 4 * jj + m - n2
                if 0 <= i < 256:
                    A[i, jj] += 1
    t["Amat"] = A
    t["Emat"] = (np.arange(4096)[None, :] // 64 == np.arange(64)[:, None]).astype(np.float32)
    inv = (np.float32(10000.0) ** (-np.arange(0, 64, 2, dtype=np.float32) / np.float32(64))).astype(np.float32)
    t["invf"] = np.stack([np.concatenate([inv, inv]), np.concatenate([-np.ones(32, np.float32), np.ones(32, np.float32)])], 1).astype(np.float32)
    t["ctxv"] = np.full((128, 1), float(s), np.float32)
    t["ident"] = np.eye(128, dtype=np.float32)
    return t


def l0_inputs(inp, b, s, shared, mods):
    x = inp['x'][b]
    pos = np.asarray(inp['positions'][b]).astype(np.int32)
    if s == 1:
        xl, pl = x, pos
    else:
        xl = np.concatenate([np.zeros((2048, D), np.float32), x[:2048]], 0)
        pl = np.concatenate([np.zeros(2048, np.int32), pos[:2048]])
    d = dict(shared[s])
    d.update({"xl": np.ascontiguousarray(xl), "pos": np.ascontiguousarray(pl[None]), "modrow": np.ascontiguousarray(mods[0, b][None])})
    return d


def l0_shared(inp):
    f = lambda a: np.ascontiguousarray(np.asarray(a, np.float32))
    base = {"gn_m": f(inp['mix_norm_g'][0][None]), "gn_f": f(inp['ffn_norm_g'][0][None]),
            "w_in": f(inp['ab_w_in'][0]), "w_out": f(inp['ab_w_out'][0]),
            "pos_k": f(inp['nsa_cmp_pos_k'][0]), "pos_v": f(inp['nsa_cmp_pos_v'][0]), "w1_k": f(inp['nsa_cmp_w1_k'][0]), "w1_v": f(inp['nsa_cmp_w1_v'][0]),
            "w2_k": f(inp['nsa_cmp_w2_k'][0]), "w2_v": f(inp['nsa_cmp_w2_v'][0]),
            "qng": f(inp['mla_q_norm_g'][0].reshape(4, 128).T), "kvng": f(inp['mla_kv_norm_g'][0].reshape(4, 128).T),
            "w_uq": f(inp['mla_w_uq'][0]), "w_ukv": f(inp['mla_w_ukv'][0]),
            "wg": f(inp['ffn_w_gate'][0]), "wu": f(inp['ffn_w_up'][0]), "wd": f(inp['ffn_w_down'][0])}
    return [{**base, **l0_tables(inp['rel_bias'], s)} for s in range(2)]


def build_final():
    nc = bass.Bass("TRN2", target_bir_lowering=False)
    x1m = nc.dram_tensor("x1m", [2048, D], F32, kind="ExternalInput").ap()
    parts = nc.dram_tensor("parts", [8, 2048, D], F32, kind="ExternalInput").ap()
    gf = nc.dram_tensor("gf", [1, D], F32, kind="ExternalInput").ap()
    fg = nc.dram_tensor("fg", [1, D], F32, kind="ExternalInput").ap()
    out = nc.dram_tensor("out", [2048, D], F32, kind="ExternalOutput").ap()
    P = Prog(nc)
    C = Ctx(nc, P)
    with C.phase():
        gfb = C.sb([128, D], F32, "gfb")
        fgb = C.sb([128, D], F32, "fgb")
        xb = [C.sb([128, D], F32, "xb") for _ in range(2)]
        acc = [C.sb([128, D], F32, "acc") for _ in range(2)]
        pb = [C.sb([128, D], F32, "pb") for _ in range(3)]
        junk = C.sb([128, D], BF16, "junk")
        st = [C.sb([128, 4], F32, "st") for _ in range(2)]
        P.dma('sp', gfb[:], gf.partition_broadcast(128), [], [gfb.b])
        P.dma('sp', fgb[:], fg.partition_broadcast(128), [], [fgb.b])
        k = 0
        for t in range(16):
            x, a, s_ = xb[t % 2], acc[t % 2], st[t % 2]
            rows = slice(t * 128, (t + 1) * 128)
            P.dma('sp', x[:], x1m[rows, :], [], [x.b])
            P.dma('sp', a[:], parts[0, rows, :], [], [a.b])
            for e in range(1, 8):
                p = pb[k % 3]
                k += 1
                P.dma('sp', p[:], parts[e, rows, :], [], [p.b])
                P.dve('tensor_tensor', [a.b, p.b], [a.b], out=a[:], in0=a[:], in1=p[:], op=ALU.add)
            P.dve('tensor_tensor', [a.b, gfb.b], [a.b], out=a[:], in0=a[:], in1=gfb[:], op=ALU.mult)
            P.dve('tensor_tensor', [a.b, x.b], [x.b], out=x[:], in0=x[:], in1=a[:], op=ALU.add)
            P.act(junk[:], x[:], AF.Square, [x.b], [junk.b, s_.b], accum_out=s_[:, 0:1])
            P.dve('tensor_scalar', [s_.b], [s_.b], out=s_[:, 1:2], in0=s_[:, 0:1], scalar1=1.0 / D, scalar2=EPS, op0=ALU.mult, op1=ALU.add)
            P.act(s_[:, 3:4], s_[:, 1:2], AF.Sqrt, [s_.b], [s_.b])
            P.dve('reciprocal', [s_.b], [s_.b], out=s_[:, 2:3], in_=s_[:, 3:4])
            P.dve('scalar_tensor_tensor', [x.b, s_.b, fgb.b], [x.b], out=x[:], in0=x[:], scalar=s_[:, 2:3], in1=fgb[:], op0=ALU.mult, op1=ALU.mult)
            P.dma('sp', out[rows, :], x[:], [x.b], [])
    P.finish()
    return nc


def _launch(nc, ims):
    return run_bass_kernel_spmd(nc, ims, core_ids=list(range(8))).results


def kernel(**inp):
    f32 = lambda a: np.ascontiguousarray(np.asarray(a, np.float32))
    inp = {k: np.asarray(v) for k, v in inp.items()}
    mods = run_mods(inp)
    shared = l0_shared(inp)
    res = _launch(build_l0(), [l0_inputs(inp, i // 2, i % 2, shared, mods) for i in range(8)])
    x1 = np.empty((4, 4096, D), np.float32)
    for i in range(8):
        x1[i // 2, (i % 2) * 2048:(i % 2 + 1) * 2048] = res[i]["x1"]
    del shared, res
    base = {"gn_m": f32(inp['mix_norm_g'][1][None]),
            "gn_f": f32(inp['ffn_norm_g'][1][None]), "c_w_in": f32(inp['c_w_in'][0]), "c_w_out": f32(inp['c_w_out'][0]),
            "wrT": f32(np.asarray(inp['moe_w_router'][0]).T).reshape(1, -1), "Fd": make_Fd(f32(inp['rel_bias'])),
            "ident": np.eye(128, dtype=np.float32)}
    ims = []
    for i in range(8):
        b, s = i // 2, i % 2
        xl = x1[b] if s == 1 else np.concatenate([np.zeros((2048, D), np.float32), x1[b, :2048]], 0)
        ims.append({**base, "xl": np.ascontiguousarray(xl), "modrow": np.ascontiguousarray(mods[1, b][None]),
                    "ctxv": np.full((128, 1), float(s), np.float32)})
    res = _launch(build_l1mix(), ims)
    x1m = [np.asarray(res[i]["x1m"]) for i in range(8)]
    gfs = [np.asarray(res[i]["gf"]) for i in range(8)]
    hT_all = np.ascontiguousarray(np.concatenate([np.asarray(res[i]["hTf"]) for i in range(8)], axis=1))
    gates = np.concatenate([np.asarray(res[i]["gates"]) for i in range(8)], axis=0)
    del ims, base, res, x1
    ims = [{"hT": hT_all, "gate": np.ascontiguousarray(gates[:, e].reshape(128, 128).T),
            "wg": f32(inp['moe_w_gate'][0, e]), "wu": f32(inp['moe_w_up'][0, e]), "wd": f32(inp['moe_w_down'][0, e])} for e in range(8)]
    res = _launch(build_moe(), ims)
    ys = [np.asarray(res[e]["y"]) for e in range(8)]
    del ims, res
    fg = f32(inp['final_norm_g'][None])
    ims = [{"x1m": x1m[i], "parts": np.ascontiguousarray(np.stack([ys[e][i * 2048:(i + 1) * 2048] for e in range(8)], 0)),
            "gf": gfs[i], "fg": fg} for i in range(8)]
    res = _launch(build_final(), ims)
    out = np.empty((4, 4096, D), np.float32)
    for i in range(8):
        out[i // 2, (i % 2) * 2048:(i % 2 + 1) * 2048] = res[i]["out"]
    return out
```
